# Optimizing a Trainium2 kernel written in Bass

```python
import math, functools
import jax, jax.numpy as jnp
from jax import lax
import numpy as np

D_MODEL = 1024
BATCH = 4
SEQ = 8192
DEPTH = 2

GRID_W = 64
CTX_LEN = 256
EPS = 1e-6

DN_HEADS = 4
DN_DK = 128
DN_DV = 128
DN_CHUNK = 64
DN_CONV_W = 3
QK_W = DN_HEADS * DN_DK
V_W = DN_HEADS * DN_DV
QKV_W = 2 * QK_W + V_W
POOL_WINDOWS = (2, 4, 8, 16)
POOL_GROUP = D_MODEL // 8
POOL_WIDTH = len(POOL_WINDOWS) * POOL_GROUP
MIX_WIDTH = V_W + POOL_WIDTH
EVEN_IN = QKV_W + V_W + 4 * DN_HEADS + POOL_WIDTH
SC_CONV_W = 3
N_EXPERTS = 16
D_EXPERT = D_MODEL // 2
EC_CAPACITY_FACTOR = 2

N_EVEN = (DEPTH + 1) // 2
N_ODD = DEPTH // 2

kernel_name = "hybrid_dit_deltanet_pool_shortconv_ecmoe"


def rms_norm(x, g):
    xf = x.astype(jnp.float32)
    y = xf * lax.rsqrt(jnp.mean(xf * xf, axis=-1, keepdims=True) + EPS)
    return (y * g.astype(jnp.float32)).astype(x.dtype)


def modulate(h, shift, scale):
    return h * (1 + scale[:, None]) + shift[:, None]


def dw_conv(x, w):
    k = w.shape[0]
    pad_l = (k - 1) // 2
    return lax.conv_general_dilated(
        x, w[:, None, :].astype(x.dtype), window_strides=(1,), padding=[(pad_l, k - 1 - pad_l)],
        dimension_numbers=('NWC', 'WIO', 'NWC'), feature_group_count=x.shape[-1])


def l2norm(t):
    return t * lax.rsqrt(jnp.sum(t * t, axis=-1, keepdims=True) + EPS)


def to_heads(t, d):
    b, L, _ = t.shape
    return t.reshape(b, L, DN_HEADS, d).transpose(0, 2, 1, 3)


def gated_delta_chunked(q, k, v, beta, log_decay, s0):
    b, h, L, dk = k.shape
    dv = v.shape[-1]
    c = DN_CHUNK
    n = L // c
    k = k.reshape(b, h, n, c, dk)
    v = v.reshape(b, h, n, c, dv)
    beta = beta.reshape(b, h, n, c)
    gam = jnp.cumsum(log_decay.reshape(b, h, n, c), axis=-1)
    incl = jnp.tril(jnp.ones((c, c), dtype=bool))
    strict = jnp.tril(jnp.ones((c, c), dtype=bool), k=-1)
    diff = gam[..., :, None] - gam[..., None, :]
    decay = jnp.where(incl, jnp.exp(jnp.where(incl, diff, 0.0)), 0.0)
    kk = jnp.einsum('bhnid,bhnjd->bhnij', k, k)
    t_mat = jnp.eye(c, dtype=jnp.float32) + jnp.where(strict, kk * decay, 0.0) * beta[..., :, None]
    solve = functools.partial(lax.linalg.triangular_solve, left_side=True, lower=True, unit_diagonal=True)
    u = solve(t_mat, v * beta[..., None])
    wk = solve(t_mat, k * (beta * jnp.exp(gam))[..., None])
    k_dec = k * jnp.exp(gam[..., -1:] - gam)[..., None]
    chunk_decay = jnp.exp(gam[..., -1])[..., None, None]
    to_scan = lambda t: jnp.moveaxis(t, 2, 0)

    def advance(s, u_c, wk_c, kd_c, cd_c):
        w = u_c - jnp.einsum('bhck,bhkv->bhcv', wk_c, s)
        return w, cd_c * s + jnp.einsum('bhck,bhcv->bhkv', kd_c, w)

    if q is None:
        def step_state(s, inp):
            _, s_new = advance(s, *inp)
            return s_new, None
        s_fin, _ = lax.scan(step_state, s0, tuple(map(to_scan, (u, wk, k_dec, chunk_decay))))
        return None, s_fin

    q = q.reshape(b, h, n, c, dk)
    qk = jnp.einsum('bhnid,bhnjd->bhnij', q, k) * decay
    q_dec = q * jnp.exp(gam)[..., None]

    def step(s, inp):
        u_c, wk_c, kd_c, cd_c, qk_c, qd_c = inp
        w, s_new = advance(s, u_c, wk_c, kd_c, cd_c)
        o = jnp.einsum('bhck,bhkv->bhcv', qd_c, s) + jnp.einsum('bhij,bhjv->bhiv', qk_c, w)
        return s_new, o

    s_fin, o = lax.scan(step, s0, tuple(map(to_scan, (u, wk, k_dec, chunk_decay, qk, q_dec))))
    return jnp.moveaxis(o, 0, 2).reshape(b, h, L, dv), s_fin


def even_project(h, w_in, conv_w, a_log, dt_bias):
    proj = h @ w_in
    qkv, z, gates, pool_in = jnp.split(proj, [QKV_W, QKV_W + V_W, QKV_W + V_W + 4 * DN_HEADS], axis=-1)
    qkv = jax.nn.silu(dw_conv(qkv, conv_w)).astype(jnp.float32)
    q, k, v = jnp.split(qkv, [QK_W, 2 * QK_W], axis=-1)
    q = l2norm(to_heads(q, DN_DK)) * DN_DK ** -0.5
    k = l2norm(to_heads(k, DN_DK))
    v = to_heads(v, DN_DV)
    b, L, _ = gates.shape
    gates = gates.astype(jnp.float32).reshape(b, L, 4, DN_HEADS).transpose(2, 0, 3, 1)
    beta = jax.nn.sigmoid(gates[:2])
    a_log = a_log.astype(jnp.float32)[:, None, :, None]
    dt_bias = dt_bias.astype(jnp.float32)[:, None, :, None]
    log_decay = -jnp.exp(a_log) * jax.nn.softplus(gates[2:] + dt_bias)
    return q, k, v, z, beta, log_decay, pool_in


def centred_mean(u, n_seg, seg_len, w):
    b, L, ch = u.shape
    us = u.astype(jnp.float32).reshape(b, n_seg, seg_len, ch)
    cs = jnp.concatenate([jnp.zeros_like(us[:, :, :1]), jnp.cumsum(us, axis=2)], axis=2)
    t = jnp.arange(seg_len)
    lo = jnp.clip(t - w // 2, 0, seg_len)
    hi = jnp.clip(t + w - w // 2, 0, seg_len)
    cnt = (hi - lo).astype(jnp.float32)[:, None]
    return ((cs[:, :, hi] - cs[:, :, lo]) / cnt).reshape(b, L, ch).astype(u.dtype)


def even_output(o, z, pool_in, o_norm, pool_w, pool_scale, w_out, n_seg, seg_len):
    b, _, L, _ = o.shape
    o = o.transpose(0, 2, 1, 3)
    zh = z.reshape(b, L, DN_HEADS, DN_DV).astype(jnp.float32)
    o = (rms_norm(o, o_norm) * jax.nn.silu(zh)).reshape(b, L, V_W).astype(z.dtype)
    groups = jnp.stack([centred_mean(pg, n_seg, seg_len, w) - pg
                        for pg, w in zip(jnp.split(pool_in, len(POOL_WINDOWS), axis=-1), POOL_WINDOWS)], axis=2)
    pooled = jnp.einsum('blgc,gcd->blgd', groups, pool_w).reshape(b, L, POOL_WIDTH) * pool_scale
    return jnp.concatenate([o, pooled], axis=-1) @ w_out


def even_mixer(hl, hc, w_in, conv_w, a_log, dt_bias, o_norm, pool_w, pool_scale, w_out, rows, ctx_out):
    ql, kl, vl, zl, betal, gl, pl = even_project(hl, w_in, conv_w, a_log, dt_bias)
    qc, kc, vc, zc, betac, gc, pc = even_project(hc, w_in, conv_w, a_log, dt_bias)
    flip = lambda t: jnp.flip(t, axis=2)
    s0 = jnp.zeros((hl.shape[0], DN_HEADS, DN_DK, DN_DV), jnp.float32)
    oc_f, sc_f = gated_delta_chunked(qc if ctx_out else None, kc, vc, betac[0], gc[0], s0)
    oc_b, sc_b = gated_delta_chunked(flip(qc) if ctx_out else None, flip(kc), flip(vc), flip(betac[1]), flip(gc[1]), s0)
    ol_f, _ = gated_delta_chunked(ql, kl, vl, betal[0], gl[0], sc_f)
    ol_b, _ = gated_delta_chunked(flip(ql), flip(kl), flip(vl), flip(betal[1]), flip(gl[1]), sc_b)
    y_lat = even_output(ol_f + flip(ol_b), zl, pl, o_norm, pool_w, pool_scale, w_out, rows, GRID_W)
    if not ctx_out:
        return y_lat, None
    y_ctx = even_output(oc_f + flip(oc_b), zc, pc, o_norm, pool_w, pool_scale, w_out, 1, hc.shape[1])
    return y_lat, y_ctx


def short_conv_mixer(h, w_in, conv_w, w_out):
    gb, gc, xin = jnp.split(h @ w_in, 3, axis=-1)
    return (gb * dw_conv(gc * xin, conv_w)) @ w_out


def expert_choice_ffn(h, router, w_gate, w_up, w_down):
    b, n, _ = h.shape
    cap = EC_CAPACITY_FACTOR * n // N_EXPERTS
    aff = jax.nn.softmax(jnp.einsum('bnd,de->bne', h, router).astype(jnp.float32), axis=-1)
    gate, idx = lax.top_k(aff.transpose(0, 2, 1), cap)
    bidx = jnp.arange(b)[:, None, None]
    xg = h[bidx, idx]
    hid = jax.nn.silu(jnp.einsum('becd,edf->becf', xg, w_gate)) * jnp.einsum('becd,edf->becf', xg, w_up)
    y = jnp.einsum('becf,efd->becd', hid, w_down) * gate[..., None].astype(h.dtype)
    return jnp.zeros_like(h).at[bidx, idx].add(y)


def setup_inputs(seed: int = 0) -> dict:
    key = jax.random.key(seed)
    ks = jax.random.split(key, 24)
    f32 = jnp.float32
    D = D_MODEL
    nrm = lambda k, shape, s: jax.random.normal(k, shape, f32) * s
    dt = jnp.exp(jax.random.uniform(ks[10], (N_EVEN, 2, DN_HEADS), f32, math.log(1e-3), math.log(1e-1)))
    return {
        "x": nrm(ks[0], (BATCH, SEQ, D), 1.0),
        "c": nrm(ks[1], (BATCH, D), 1.0),
        "ctx": nrm(ks[2], (BATCH, CTX_LEN, D), 1.0),
        "c_ctx": nrm(ks[3], (D,), 1.0),
        "ada_w": nrm(ks[4], (DEPTH, D, 6 * D), 0.5 * D ** -0.5),
        "ada_b": nrm(ks[5], (DEPTH, 6 * D), 0.02),
        "norm_mix": 1.0 + nrm(ks[6], (DEPTH, D), 0.02),
        "norm_ffn": 1.0 + nrm(ks[7], (DEPTH, D), 0.02),
        "norm_final": 1.0 + nrm(ks[8], (D,), 0.02),
        "ev_w_in": nrm(ks[9], (N_EVEN, D, EVEN_IN), D ** -0.5),
        "dn_conv": nrm(ks[11], (N_EVEN, DN_CONV_W, QKV_W), DN_CONV_W ** -0.5),
        "dn_a_log": jnp.log(jax.random.uniform(ks[12], (N_EVEN, 2, DN_HEADS), f32, 1.0, 16.0)),
        "dn_dt_bias": dt + jnp.log(-jnp.expm1(-dt)),
        "dn_norm": 1.0 + nrm(ks[13], (N_EVEN, DN_DV), 0.02),
        "pool_w": nrm(ks[14], (N_EVEN, len(POOL_WINDOWS), POOL_GROUP, POOL_GROUP), POOL_GROUP ** -0.5),
        "pool_scale": 1.0 + nrm(ks[15], (N_EVEN, POOL_WIDTH), 0.1),
        "ev_w_out": nrm(ks[16], (N_EVEN, MIX_WIDTH, D), MIX_WIDTH ** -0.5),
        "sc_w_in": nrm(ks[17], (N_ODD, D, 3 * D), D ** -0.5),
        "sc_conv": nrm(ks[18], (N_ODD, SC_CONV_W, D), SC_CONV_W ** -0.5),
        "sc_w_out": nrm(ks[19], (N_ODD, D, D), D ** -0.5),
        "router": nrm(ks[20], (DEPTH, D, N_EXPERTS), D ** -0.5),
        "w_gate": nrm(ks[21], (DEPTH, N_EXPERTS, D, D_EXPERT), D ** -0.5),
        "w_up": nrm(ks[22], (DEPTH, N_EXPERTS, D, D_EXPERT), D ** -0.5),
        "w_down": nrm(ks[23], (DEPTH, N_EXPERTS, D_EXPERT, D), D_EXPERT ** -0.5),
    }


def reference(x, c, ctx, c_ctx, ada_w, ada_b, norm_mix, norm_ffn, norm_final,
              ev_w_in, dn_conv, dn_a_log, dn_dt_bias, dn_norm, pool_w, pool_scale, ev_w_out,
              sc_w_in, sc_conv, sc_w_out, router, w_gate, w_up, w_down):
    rows = x.shape[1] // GRID_W
    xl, xc = x, ctx
    for i in range(DEPTH):
        j = i // 2
        ctx_read = i % 2 == 0
        ctx_live = any(l % 2 == 0 for l in range(i + 1, DEPTH))
        sh1_l, sc1_l, g1_l, sh2_l, sc2_l, g2_l = jnp.split(jax.nn.silu(c) @ ada_w[i] + ada_b[i], 6, axis=-1)
        hl = modulate(rms_norm(xl, norm_mix[i]), sh1_l, sc1_l)
        yc = None
        if ctx_read or ctx_live:
            sh1_c, sc1_c, g1_c, sh2_c, sc2_c, g2_c = jnp.split((jax.nn.silu(c_ctx) @ ada_w[i] + ada_b[i])[None], 6, axis=-1)
            hc = modulate(rms_norm(xc, norm_mix[i]), sh1_c, sc1_c)
        if ctx_read:
            yl, yc = even_mixer(hl, hc, ev_w_in[j], dn_conv[j], dn_a_log[j], dn_dt_bias[j], dn_norm[j],
                                pool_w[j], pool_scale[j], ev_w_out[j], rows, ctx_live)
        else:
            yl = short_conv_mixer(hl, sc_w_in[j], sc_conv[j], sc_w_out[j])
            if ctx_live:
                yc = short_conv_mixer(hc, sc_w_in[j], sc_conv[j], sc_w_out[j])
        xl = xl + g1_l[:, None] * yl
        hl2 = modulate(rms_norm(xl, norm_ffn[i]), sh2_l, sc2_l)
        xl = xl + g2_l[:, None] * expert_choice_ffn(hl2, router[i], w_gate[i], w_up[i], w_down[i])
        if ctx_live:
            xc = xc + g1_c[:, None] * yc
            hc2 = modulate(rms_norm(xc, norm_ffn[i]), sh2_c, sc2_c)
            xc = xc + g2_c[:, None] * expert_choice_ffn(hc2, router[i], w_gate[i], w_up[i], w_down[i])
    return rms_norm(xl, norm_final)
```

```python
import numpy as np
import ml_dtypes
from contextlib import ExitStack
import concourse.bass as bass
import concourse.mybir as mybir
from concourse.bass_utils import run_bass_kernel_spmd

F32 = mybir.dt.float32
BF16 = mybir.dt.bfloat16
I32 = mybir.dt.int32
ALU = mybir.AluOpType
AF = mybir.ActivationFunctionType
AX = mybir.AxisListType
EPS = 1e-6
D = 1024
CTX = 256
NE = 16


class Buf:
    __slots__ = ("w", "r")

    def __init__(self):
        self.w = None
        self.r = []


class Tile:
    def __init__(self, t):
        self.t = t
        self.b = Buf()

    def __getitem__(self, k):
        return self.t[k]


class Ring:
    def __init__(self, tiles):
        self.tiles = tiles
        self.i = 0

    def next(self):
        t = self.tiles[self.i % len(self.tiles)]
        self.i += 1
        return t


class Prog:
    ENG = ("pe", "act", "dve", "pool", "sp")
    NDMA = 48
    NHW = 32

    def __init__(self, nc):
        self.nc = nc
        self.es = ExitStack()
        self.q = {e: [] for e in self.ENG}
        self.cnt = {e: 0 for e in self.ENG}
        self.seen = {e: {} for e in self.ENG}
        self.sem = {e: self.es.enter_context(nc.semaphore("pg_" + e)) for e in self.ENG}
        self.dsem = [self.es.enter_context(nc.semaphore("pg_d%d" % i)) for i in range(self.NDMA)]
        self.dcnt = [0] * self.NDMA
        self.dnext = 0
        self.dnext_sw = 0
        self.ninst = 0
        self.uid = 0

    def sb(self, shape, dt, name=None):
        self.uid += 1
        return Tile(self.es.enter_context(self.nc.sbuf_tensor("%s_%d" % (name or "sb", self.uid), list(shape), dt)))

    def ps(self, shape, dt, name=None):
        self.uid += 1
        return Tile(self.es.enter_context(self.nc.psum_tensor("%s_%d" % (name or "ps", self.uid), list(shape), dt)))

    def ring(self, n, shape, dt, psum=False, name=None):
        return Ring([(self.ps if psum else self.sb)(shape, dt, name) for _ in range(n)])

    def _semof(self, tl):
        return self.sem[tl] if isinstance(tl, str) else self.dsem[tl]

    def _deps(self, eng, reads, writes):
        deps = {}

        def add(tok):
            if tok is None:
                return
            tl, v = tok
            if tl == "pe" and eng == "pe":
                return
            if deps.get(tl, 0) < v:
                deps[tl] = v
        for b in reads:
            add(b.b.w)
        for b in writes:
            add(b.b.w)
            for t in b.b.r:
                add(t)
        waits = []
        sn = self.seen[eng]
        for tl, v in deps.items():
            if sn.get(tl, 0) < v:
                sn[tl] = v
                waits.append((tl, v))
        return waits

    def _mark(self, tok, reads, writes):
        for b in reads:
            b.b.r.append(tok)
            if len(b.b.r) > 64:
                b.b.r = b.b.r[-48:]
        for b in writes:
            b.b.w = tok
            b.b.r = []

    def op(self, eng, fn, R=(), W=()):
        waits = self._deps(eng, R, W)
        self.cnt[eng] += 1
        tok = (eng, self.cnt[eng])
        self.q[eng].append((waits, fn, (eng, 1)))
        self._mark(tok, R, W)
        self.ninst += 1
        return tok

    def dma(self, eng, out, in_, R=(), W=(), **kw):
        return self.dmaf(eng, lambda e: e.dma_start(out=out, in_=in_, **kw), R, W)

    def dmaf(self, eng, fn, R=(), W=()):
        waits = self._deps(eng, R, W)
        if eng == "pool":
            k = self.NHW + self.dnext_sw
            self.dnext_sw = (self.dnext_sw + 1) % (self.NDMA - self.NHW)
        else:
            k = self.dnext
            self.dnext = (self.dnext + 1) % self.NHW
        if self.dcnt[k] > 0 and self.seen[eng].get(k, 0) < self.dcnt[k]:
            self.seen[eng][k] = self.dcnt[k]
            waits.append((k, self.dcnt[k]))
        self.dcnt[k] += 16
        tok = (k, self.dcnt[k])
        self.q[eng].append((waits, fn, (k, 16)))
        self._mark(tok, R, W)
        self.ninst += 1
        return tok

    def mm(self, out, lhsT, rhs, start=True, stop=True, R=(), W=()):
        return self.op("pe", lambda e: e.matmul(out, lhsT=lhsT, rhs=rhs, start=start, stop=stop), R, W)

    def tr(self, out, in_, ident, R=(), W=()):
        return self.op("pe", lambda e: e.transpose(out, in_, ident), R, W)

    def act(self, out, in_, func, bias=None, scale=1.0, accum=None, R=(), W=()):
        def f(e):
            kw = {}
            if bias is not None:
                kw["bias"] = bias
            if accum is not None:
                kw["accum_out"] = accum
            return e.activation(out=out, in_=in_, func=func, scale=scale, **kw)
        return self.op("act", f, R, W)

    def ts(self, eng, out, in0, s1, s2, op0, op1=None, R=(), W=()):
        def f(e):
            if op1 is None:
                return e.tensor_scalar(out=out, in0=in0, scalar1=s1, scalar2=None, op0=op0)
            return e.tensor_scalar(out=out, in0=in0, scalar1=s1, scalar2=s2, op0=op0, op1=op1)
        return self.op(eng, f, R, W)

    def tt(self, eng, out, in0, in1, op, R=(), W=()):
        return self.op(eng, lambda e: e.tensor_tensor(out=out, in0=in0, in1=in1, op=op), R, W)

    def stt(self, eng, out, in0, scalar, in1, op0, op1, R=(), W=()):
        return self.op(eng, lambda e: e.scalar_tensor_tensor(out=out, in0=in0, scalar=scalar, in1=in1, op0=op0, op1=op1), R, W)

    def cp(self, eng, out, in_, R=(), W=()):
        if eng == "act":
            return self.op("act", lambda e: e.copy(out=out, in_=in_), R, W)
        return self.op(eng, lambda e: e.tensor_copy(out=out, in_=in_), R, W)

    def rsqrt(self, ap, T):
        self.op("act", lambda e: e.activation(out=ap, in_=ap, func=AF.Sqrt), [T], [T])
        self.op("dve", lambda e: e.reciprocal(out=ap, in_=ap), [T], [T])

    def memset(self, eng, ap, val, W=()):
        return self.op(eng, lambda e: e.memset(ap, val), (), W)

    def barrier(self):
        for e in self.ENG:
            waits = []
            sn = self.seen[e]
            for f in self.ENG:
                if f != e and sn.get(f, 0) < self.cnt[f]:
                    sn[f] = self.cnt[f]
                    waits.append((f, self.cnt[f]))
            for k in range(self.NDMA):
                if self.dcnt[k] > 0 and sn.get(k, 0) < self.dcnt[k]:
                    sn[k] = self.dcnt[k]
                    waits.append((k, self.dcnt[k]))
            if waits:
                self.q[e].append((waits, None, None))

    def scope(self):
        prog = self

        class _S:
            def __enter__(s2):
                s2.outer = prog.es
                prog.es = ExitStack()
                return prog

            def __exit__(s2, *a):
                if a[0] is None:
                    prog.flush()
                prog.es.close()
                prog.es = s2.outer
                return False
        return _S()

    def flush(self):
        nc = self.nc
        self.barrier()
        with nc.Block() as block:
            def run(engname):
                def body(eng):
                    for waits, fn, inc in self.q[engname]:
                        for tl, v in waits:
                            eng.wait_ge(self._semof(tl), v)
                        if fn is not None:
                            fn(eng).then_inc(self._semof(inc[0]), inc[1])
                return body
            block.tensor(run("pe"))
            block.scalar(run("act"))
            block.vector(run("dve"))
            block.gpsimd(run("pool"))
            block.sync(run("sp"))
        self.q = {e: [] for e in self.ENG}

    def finish(self):
        self.es.close()


def interleave(gens):
    gens = list(gens)
    while gens:
        nxt = []
        for g in gens:
            try:
                next(g)
                nxt.append(g)
            except StopIteration:
                pass
        gens = nxt


def make_consts():
    cf = {}
    i = np.arange(128)
    cf["ones"] = np.ones((128, 128), np.float32)
    cf["ucf"] = (i[:, None] <= i[None, :]).astype(np.float32)
    cf["ucb"] = (i[:, None] >= i[None, :]).astype(np.float32)
    cf["msf"] = (i[None, :] > i[:, None]).astype(np.float32)
    cf["msb"] = (i[None, :] < i[:, None]).astype(np.float32)
    cf["mif"] = (i[None, :] >= i[:, None]).astype(np.float32)
    cf["mib"] = (i[None, :] <= i[:, None]).astype(np.float32)
    cf["iota"] = np.broadcast_to(i[None, :].astype(np.float32), (128, 128)).copy()
    cf["identf"] = np.eye(128, dtype=np.float32)
    misc = np.zeros((128, 128), np.float32)
    misc[:, 0] = i
    misc[:, 8:16] = np.arange(8)[None, :]
    cf["misc"] = misc
    inv = np.zeros((128, 4 * 128), np.float32)
    pm = np.zeros((4, 128, 128), np.float32)
    for g, w in enumerate((2, 4, 8, 16)):
        for t in range(128):
            seg = t // 64
            tl = t % 64
            lo = min(max(tl - w // 2, 0), 64)
            hi = min(max(tl + w - w // 2, 0), 64)
            cnt = hi - lo
            pm[g, seg * 64 + lo: seg * 64 + hi, t] += 1.0
            pm[g, t, t] -= cnt
            inv[:, g * 128 + t] = 1.0 / cnt
    cf["invcnt"] = inv
    lvl_names = []
    for lv in range(7):
        b = 1 << lv
        jj, ii = i[:, None], i[None, :]
        mf = ((jj // (2 * b)) == (ii // (2 * b))) & ((jj % (2 * b)) < b) & ((ii % (2 * b)) >= b)
        cf["nmf%d" % lv] = -(mf.astype(np.float32))
        cf["nmb%d" % lv] = -(mf.T.astype(np.float32))
        lvl_names += ["nmf%d" % lv, "nmb%d" % lv]
    names_f = ["ones", "ucf", "ucb", "msf", "msb", "mif", "mib", "iota", "identf", "misc"] + lvl_names
    arr_f = np.concatenate([cf[n] for n in names_f] + [inv], axis=1).astype(np.float32)
    off_f = {n: k * 128 for k, n in enumerate(names_f)}
    off_f["invcnt"] = len(names_f) * 128
    cb = [np.eye(128, dtype=np.float32), np.ones((128, 128), np.float32),
          (i[:, None] < i[None, :]).astype(np.float32)] + [pm[g] for g in range(4)]
    arr_b = np.concatenate(cb, axis=1).astype(ml_dtypes.bfloat16)
    off_b = {"ident": 0, "ones": 128, "lstrict": 256, "pm": 384}
    return arr_f, off_f, arr_b, off_b


def build(L, debug_outs=(), stop=None):
    assert L % 256 == 0
    NT = L // 128
    LT = CTX + L
    NCH = LT // 128
    CAP = 2 * L // NE
    NM = CAP // 128
    assert CAP % 128 == 0
    arr_f, OF, arr_b, OB = make_consts()
    nc = bass.Bass("TRN2", target_bir_lowering=False)

    def din(name, shape, dt=F32):
        return nc.dram_tensor(name, list(shape), dt, kind="ExternalInput").ap()

    def dscr(name, shape, dt=F32):
        kind = "ExternalOutput" if name in debug_outs else "Internal"
        return nc.dram_tensor(name, list(shape), dt, kind=kind).ap()

    I = dict(
        x=din("x", [L, D]), ctx=din("ctx", [CTX, D]), cvec=din("cvec", [128, 16]),
        ada_w=din("ada_w", [2, D, 6 * D]), ada_b=din("ada_b", [2, 6 * D]),
        norm_mix=din("norm_mix", [2, D]), norm_ffn=din("norm_ffn", [2, D]), norm_final=din("norm_final", [D]),
        ev_w_in=din("ev_w_in", [D, 2576]), dn_conv=din("dn_conv", [128, 36]),
        dn_a_log=din("dn_a_log", [8]), dn_dt_bias=din("dn_dt_bias", [8]), dn_norm=din("dn_norm", [128]),
        pool_w=din("pool_w", [4, 128, 128]), pool_scale=din("pool_scale", [128, 4]),
        ev_w_out=din("ev_w_out", [D, D]),
        sc_w_in=din("sc_w_in", [D, 3 * D]), sc_conv=din("sc_conv", [128, 24]), sc_w_out=din("sc_w_out", [D, D]),
        router=din("router", [2, D, NE]), w_gate=din("w_gate", [2, NE, D, 512]),
        w_up=din("w_up", [2, NE, D, 512]), w_down=din("w_down", [2, NE, 512, D]),
        cf=din("cf", list(arr_f.shape)), cb=din("cb", list(arr_b.shape), BF16),
    )
    out = nc.dram_tensor("out", [L, D], F32, kind="ExternalOutput").ap()
    S = dict(
        mod=dscr("mod", [2, 2, 6 * D]),
        hT=dscr("hT", [8, 128, LT], BF16),
        qT=dscr("qT", [4, 128, LT], BF16), kT=dscr("kT", [4, 128, LT], BF16),
        ktok=dscr("ktok", [LT, 512], BF16), vtok=dscr("vtok", [LT, 512], BF16),
        gates=dscr("gates", [LT, 16]), zs=dscr("zs", [L, 512], BF16),
        mixT=dscr("mixT", [8, 128, L], BF16),
        of=dscr("of", [L, 512]), ob=dscr("ob", [L, 512]),
        xl=dscr("xl", [L, D]), h2=dscr("h2", [L, D], BF16),
    )
    p = Prog(nc)
    cfT = p.sb([128, arr_f.shape[1]], F32, "cf")
    cbT = p.sb([128, arr_b.shape[1]], BF16, "cb")
    p.dma("sp", cfT[:], I["cf"], W=[cfT])
    p.dma("sp", cbT[:], I["cb"], W=[cbT])

    def CF(n, w=128):
        return cfT[:, OF[n]:OF[n] + w]

    def CB(n, w=128, o=0):
        return cbT[:, OB[n] + o:OB[n] + o + w]

    def colvec(dst, src1d, eng="sp"):
        p.dma(eng, dst[:], src1d.rearrange("(k p) -> p k", p=128), W=[dst], allow_slow_non_contiguous=True)

    def rowvec(dst, src1d, eng="sp"):
        p.dma(eng, dst[:], src1d.partition_broadcast(128), W=[dst])

    with p.scope():
        cv = p.sb([128, 16], F32)
        sc = p.sb([128, 16], F32)
        p.dma("sp", cv[:], I["cvec"], W=[cv])
        p.act(sc[:], cv[:], AF.Silu, R=[cv], W=[sc])
        awr = p.ring(2, [128, 8, 512], F32)
        psr = p.ring(2, [128, 512], F32, psum=True)
        for i in range(2):
            bias = p.sb([2, 6 * D], F32)
            modrow = p.sb([2, 6 * D], F32)
            for r in range(2):
                p.dma("act", bias[r:r + 1, :], I["ada_b"][i:i + 1, :], W=[bias])
            for n in range(12):
                a = awr.next()
                p.dma("sp", a[:], I["ada_w"][i][:, n * 512:(n + 1) * 512].rearrange("(k p) c -> p k c", p=128), W=[a])
                ps = psr.next()
                for k in range(8):
                    p.mm(ps[0:2, :], sc[:, k::8], a[:, k, :], start=(k == 0), stop=(k == 7), R=[sc, a], W=[ps])
                p.tt("dve", modrow[0:2, n * 512:(n + 1) * 512], ps[0:2, :], bias[0:2, n * 512:(n + 1) * 512], ALU.add,
                     R=[ps, bias], W=[modrow])
            p.dma("sp", S["mod"][i], modrow[0:2, :], R=[modrow])

    def modv(i, r, j):
        return S["mod"][i, r, j * D:(j + 1) * D]

    def phase_hT(srcs, i, normvec):
        with p.scope():
            xr = p.ring(3, [128, D], F32)
            junk = p.sb([128, D], BF16)
            ssr = p.ring(3, [128, 2], F32)
            xnr = p.ring(2, [128, D], BF16)
            pTr = p.ring(2, [128, D], BF16, psum=True)
            tmpr = p.ring(2, [128, 8, 128], F32)
            hsr = p.ring(2, [128, 8, 256], BF16)
            for (src, ntok, col0, r) in srcs:
                gcol = p.sb([128, 8], F32)
                shc = p.sb([128, 8], F32)
                scc = p.sb([128, 8], F32)
                A = p.sb([128, 8], F32)
                colvec(gcol, normvec)
                colvec(shc, modv(i, r, 0))
                colvec(scc, modv(i, r, 1))
                p.stt("dve", A[:], scc[:], 1.0, gcol[:], ALU.add, ALU.mult, R=[scc, gcol], W=[A])
                def hA(t0):
                    xt = xr.next()
                    p.dma("sp", xt[:], src[t0:t0 + 128, :], W=[xt])
                    ss = ssr.next()
                    p.memset("pool", ss[:], 0.0, W=[ss])
                    p.act(junk[:], xt[:], AF.Square, accum=ss[:, 0:1], R=[xt], W=[junk, ss])
                    p.ts("dve", ss[:, 1:2], ss[:, 0:1], 1.0 / D, EPS, ALU.mult, ALU.add, R=[ss], W=[ss])
                    p.rsqrt(ss[:, 1:2], ss)
                    xn = xnr.next()
                    p.act(xn[:], xt[:], AF.Copy, scale=ss[:, 1:2], R=[xt, ss], W=[xn])
                    return xn

                def hB(xn, hs, t2):
                    pT = pTr.next()
                    for k in range(8):
                        p.tr(pT[:, k * 128:(k + 1) * 128], xn[:, k * 128:(k + 1) * 128], CB("ident"), R=[xn, cbT], W=[pT])
                    tmp = tmpr.next()
                    p.tt("dve", tmp[:], pT[:].rearrange("p (k t) -> p k t", k=8),
                         A[:, :].unsqueeze(2).to_broadcast([128, 8, 128]), ALU.mult, R=[pT, A], W=[tmp])
                    p.tt("pool", hs[:, :, t2 * 128:(t2 + 1) * 128], tmp[:],
                         shc[:, :].unsqueeze(2).to_broadcast([128, 8, 128]), ALU.add, R=[tmp, shc], W=[hs])

                ntile = ntok // 128
                pend = None
                hs = None
                for j in range(ntile + 1):
                    xn = hA(j * 128) if j < ntile else None
                    if pend is not None:
                        jj, pxn = pend
                        if jj % 2 == 0:
                            hs = hsr.next()
                        hB(pxn, hs, jj % 2)
                        if jj % 2 == 1:
                            c0 = col0 + (jj // 2) * 256
                            p.dma("pool", S["hT"][:, :, c0:c0 + 256].rearrange("k p t -> p k t"), hs[:], R=[hs])
                    pend = (j, xn)

    def load_window(hw, col0, ntok, blk):
        a = col0 + blk * 256
        lo = a - 1 if blk > 0 else a
        hi = a + 257 if (blk + 1) * 256 < ntok else a + 256
        d0 = 0 if blk > 0 else 1
        if blk == 0:
            p.memset("pool", hw[:, :, 0:1], 0.0, W=[hw])
        if (blk + 1) * 256 >= ntok:
            p.memset("pool", hw[:, :, 257:258], 0.0, W=[hw])
        p.dma("sp", hw[:, :, d0:d0 + (hi - lo)], S["hT"][:, :, lo:hi].rearrange("k p t -> p k t"), W=[hw])

    def conv3(ps, wcol, c, acc, psT, accT):
        p.act(acc[:], ps[:, 1:257], AF.Copy, scale=wcol[:, c, 1:2], R=[psT, wcol], W=[accT])
        p.stt("dve", acc[:], ps[:, 0:256], wcol[:, c, 0:1], acc[:], ALU.mult, ALU.add, R=[psT, wcol, accT], W=[accT])
        p.stt("dve", acc[:], ps[:, 2:258], wcol[:, c, 2:3], acc[:], ALU.mult, ALU.add, R=[psT, wcol, accT], W=[accT])

    def phase_even_proj():
        with p.scope():
            w = p.sb([128, 8, 2576], BF16, "wev")
            for k in range(8):
                p.dma("pool", w[:, k, :], I["ev_w_in"][k * 128:(k + 1) * 128, :], W=[w])
            cw = p.sb([128, 12, 3], F32)
            p.dma("sp", cw[:], I["dn_conv"].rearrange("p (c t) -> p c t", t=3), W=[cw])
            pw = p.sb([128, 4, 128], BF16)
            p.dma("pool", pw[:], I["pool_w"].rearrange("g c d -> c g d"), W=[pw])
            pscale = p.sb([128, 4], F32)
            p.dma("sp", pscale[:], I["pool_scale"], W=[pscale])
            dtb = p.sb([128, 8], F32)
            nega = p.sb([128, 8], F32)
            rowvec(dtb, I["dn_dt_bias"])
            rowvec(nega, I["dn_a_log"])
            p.act(nega[:], nega[:], AF.Exp, R=[nega], W=[nega])
            p.ts("dve", nega[:], nega[:], -1.0, None, ALU.mult, R=[nega], W=[nega])
            hwr = p.ring(2, [128, 8, 258], BF16)
            psW = p.ring(3, [128, 512], F32, psum=True)
            psT = p.ring(2, [128, 1024], BF16, psum=True)
            psS = p.ring(2, [128, 512], F32, psum=True)
            accr = p.ring(3, [128, 256], F32)
            sr = p.ring(3, [128, 256], F32)
            sqr = p.ring(2, [128, 256], BF16)
            rinvr = p.ring(2, [128, 256], F32)
            qnr = p.ring(3, [128, 256], BF16)
            tokr = p.ring(3, [128, 2, 128], BF16)
            zr = p.ring(2, [128, 512], BF16)
            ur = p.ring(2, [128, 512], BF16)
            g4r = p.ring(2, [128, 512], BF16)
            m4r = p.ring(2, [128, 4, 128], BF16)
            gtr = p.ring(2, [128, 48], F32)
            for (col0, ntok, lat) in ((0, CTX, False), (CTX, L, True)):
                for blk in range(ntok // 256):
                    hw = hwr.next()
                    load_window(hw, col0, ntok, blk)
                    c0 = col0 + blk * 256
                    def stageA(c):
                        ps = psW.next()
                        for k in range(8):
                            p.mm(ps[:, 0:258], w[:, k, c * 128:(c + 1) * 128], hw[:, k, :], start=(k == 0), stop=(k == 7),
                                 R=[w, hw], W=[ps])
                        acc = accr.next()
                        conv3(ps, cw, c, acc, ps, acc)
                        st = dict(c=c)
                        if c < 8:
                            s_ = sr.next()
                            p.act(s_[:], acc[:], AF.Silu, R=[acc], W=[s_])
                            sq = sqr.next()
                            p.tt("dve", sq[:], s_[:], s_[:], ALU.mult, R=[s_], W=[sq])
                            st.update(s=s_, sq=sq)
                        else:
                            qn = qnr.next()
                            p.act(qn[:], acc[:], AF.Silu, R=[acc], W=[qn])
                            st.update(qn=qn)
                        return st

                    def stageB(st):
                        c = st["c"]
                        h = c % 4
                        if c < 8:
                            s_ = st["s"]; sq = st["sq"]
                            pss = psS.next()
                            p.mm(pss[:, 0:256], CB("ones"), sq[:], R=[sq, cbT], W=[pss])
                            rinv = rinvr.next()
                            p.ts("dve", rinv[:], pss[:, 0:256], EPS, None, ALU.add, R=[pss], W=[rinv])
                            p.rsqrt(rinv[:], rinv)
                            qn = qnr.next()
                            p.stt("dve", qn[:], s_[:], (128.0 ** -0.5) if c < 4 else 1.0, rinv[:], ALU.mult, ALU.mult,
                                  R=[s_, rinv], W=[qn])
                            dst = S["qT"] if c < 4 else S["kT"]
                            p.dma("pool", dst[h, :, c0:c0 + 256], qn[:], R=[qn])
                            src_tok = qn if c >= 4 else None
                            dtok = S["ktok"]
                        else:
                            src_tok = st["qn"]
                            dtok = S["vtok"]
                        if src_tok is not None:
                            pt = psT.next()
                            for t2 in range(2):
                                p.tr(pt[:, t2 * 128:(t2 + 1) * 128], src_tok[:, t2 * 128:(t2 + 1) * 128], CB("ident"),
                                     R=[src_tok, cbT], W=[pt])
                            tk = tokr.next()
                            p.cp("act", tk[:], pt[:, 0:256].rearrange("p (a b) -> p a b", a=2), R=[pt], W=[tk])
                            p.dma("pool", dtok[c0:c0 + 256, h * 128:(h + 1) * 128].rearrange("(a p) d -> p a d", p=128), tk[:], R=[tk])

                    pend = None
                    for c in range(12):
                        st = stageA(c)
                        if pend is not None:
                            stageB(pend)
                        pend = st
                    stageB(pend)
                    for t2 in range(2):
                        t0 = c0 + t2 * 128
                        lhs = lambda k: hw[:, k, 1 + t2 * 128:1 + (t2 + 1) * 128]
                        psg = psS.next()
                        for k in range(8):
                            p.mm(psg[:, 0:16], lhs(k), w[:, k, 2048:2064], start=(k == 0), stop=(k == 7), R=[w, hw], W=[psg])
                        if lat:
                            psz = psW.next()
                            for k in range(8):
                                p.mm(psz[:], lhs(k), w[:, k, 1536:2048], start=(k == 0), stop=(k == 7), R=[w, hw], W=[psz])
                            psp = psW.next()
                            for k in range(8):
                                p.mm(psp[:], lhs(k), w[:, k, 2064:2576], start=(k == 0), stop=(k == 7), R=[w, hw], W=[psp])
                        ps = psg
                        g = gtr.next()
                        p.act(g[:, 0:8], ps[:, 0:8], AF.Sigmoid, R=[ps], W=[g])
                        p.tt("dve", g[:, 16:24], ps[:, 8:16], dtb[:], ALU.add, R=[ps, dtb], W=[g])
                        p.ts("dve", g[:, 24:32], g[:, 16:24], 30.0, None, ALU.min, R=[g], W=[g])
                        p.act(g[:, 24:32], g[:, 24:32], AF.Exp, R=[g], W=[g])
                        p.act(g[:, 24:32], g[:, 24:32], AF.Ln, bias=1.0, R=[g], W=[g])
                        p.ts("dve", g[:, 32:40], g[:, 16:24], -30.0, 0.0, ALU.add, ALU.max, R=[g], W=[g])
                        p.tt("dve", g[:, 24:32], g[:, 24:32], g[:, 32:40], ALU.add, R=[g], W=[g])
                        p.tt("dve", g[:, 8:16], g[:, 24:32], nega[:], ALU.mult, R=[g, nega], W=[g])
                        p.dma("pool", S["gates"][t0:t0 + 128, :], g[:, 0:16], R=[g])
                        if not lat:
                            continue
                        tl = t0 - CTX
                        z = zr.next()
                        p.act(z[:], psz[:], AF.Silu, R=[psz], W=[z])
                        p.dma("pool", S["zs"][tl:tl + 128, :], z[:], R=[z])
                        u = ur.next()
                        p.cp("act", u[:], psp[:], R=[psp], W=[u])
                        ps1 = psS.next()
                        for gi in range(4):
                            p.mm(ps1[:, gi * 128:(gi + 1) * 128], u[:, gi * 128:(gi + 1) * 128], CB("pm", 128, gi * 128), R=[u, cbT], W=[ps1])
                        gT4 = g4r.next()
                        p.tt("dve", gT4[:], ps1[:], cfT[:, OF["invcnt"]:OF["invcnt"] + 512], ALU.mult, R=[ps1, cfT], W=[gT4])
                        ps2 = psS.next()
                        for gi in range(4):
                            p.mm(ps2[:, gi * 128:(gi + 1) * 128], pw[:, gi, :], gT4[:, gi * 128:(gi + 1) * 128], R=[pw, gT4], W=[ps2])
                        mo4 = m4r.next()
                        p.tt("dve", mo4[:], ps2[:].rearrange("p (g t) -> p g t", g=4),
                             pscale[:, :].unsqueeze(2).to_broadcast([128, 4, 128]), ALU.mult, R=[ps2, pscale], W=[mo4])
                        p.dma("pool", S["mixT"][4:8, :, tl:tl + 128].rearrange("k p t -> p k t"), mo4[:], R=[mo4])

    def phase_delta():
        with p.scope():
            psPd = {0: p.ring(2, [128, 4, 128], F32, psum=True, name="psP0"),
                    1: p.ring(2, [128, 4, 128], F32, psum=True, name="psP1")}
            psSc = {0: p.ring(2, [128, 4, 128], F32, psum=True, name="psS0"),
                    1: p.ring(2, [128, 4, 128], F32, psum=True, name="psS1")}
            NS = 3
            names_bf = ["qT", "kT", "ktok", "vtok", "P", "qkd", "qd", "kd", "B", "BT"]
            ins = {}
            for d in range(2):
                ins[d] = dict(
                    g=p.ring(NS, [128, 16], F32), sm=p.ring(NS, [128, 32], F32),
                    X=p.ring(2, [128, 4, 128], F32), E=p.ring(2, [128, 4, 128], F32),
                    Es=p.ring(2, [128, 4, 128], F32), Ei=p.ring(2, [128, 4, 128], F32),
                    lu=p.ring(2, [128, 4, 128], F32), eg=p.ring(2, [128, 4, 128], F32),
                    nR=p.ring(2, [128, 4, 128], BF16), w=p.ring(2, [128, 4, 128], BF16),
                    osb=p.ring(2, [128, 4, 128], F32),
                    S32=p.sb([128, 4, 128], F32), Sbf=p.ring(2, [128, 4, 128], BF16),
                )
                ins[d]["B32"] = p.ring(2, [128, 4, 128], F32)
                ins[d]["GT"] = p.ring(2, [128, 4, 128], BF16)
                ins[d]["Xb"] = p.ring(2, [128, 4, 128], BF16)
                ins[d]["tmp"] = p.ring(4, [128, 4, 128], BF16)
                for nm in names_bf:
                    ins[d][nm] = p.ring(NS if nm not in ("B", "BT", "B2", "B2T") else 2, [128, 4, 128], BF16)
            order = {0: list(range(NCH)), 1: [1, 0] + list(range(NCH - 1, 1, -1))}
            prepped = {0: {}, 1: {}}

            def prep(d, it):
                R_ = ins[d]
                psP = psPd[d]
                c = order[d][it]
                t0 = c * 128
                ucum = CF("ucf") if d == 0 else CF("ucb")
                ms = CF("msf") if d == 0 else CF("msb")
                mi = CF("mif") if d == 0 else CF("mib")
                lastcol = 127 if d == 0 else 0
                qT = R_["qT"].next(); kT = R_["kT"].next(); ktok = R_["ktok"].next(); vtok = R_["vtok"].next()
                g = R_["g"].next(); sm = R_["sm"].next()
                p.dma("act", qT[:], S["qT"][:, :, t0:t0 + 128].rearrange("h p t -> p h t"), W=[qT])
                p.dma("act", kT[:], S["kT"][:, :, t0:t0 + 128].rearrange("h p t -> p h t"), W=[kT])
                p.dma("act", ktok[:], S["ktok"][t0:t0 + 128, :].rearrange("p (h d) -> p h d", h=4), W=[ktok])
                p.dma("act", vtok[:], S["vtok"][t0:t0 + 128, :].rearrange("p (h d) -> p h d", h=4), W=[vtok])
                p.dma("act", g[:], S["gates"][t0:t0 + 128, :], W=[g])
                ld = g[:, 8 + 4 * d:12 + 4 * d]
                beta = g[:, 4 * d:4 * d + 4]
                yield
                ps = psP.next()
                p.mm(ps[:, 0, 0:4], ucum, ld, R=[cfT, g], W=[ps])
                p.cp("dve", sm[:, 0:4], ps[:, 0, 0:4], R=[ps], W=[sm])
                lu = R_["lu"].next()
                p.tt("dve", lu[:], ucum.unsqueeze(1).to_broadcast([128, 4, 128]),
                     ld.unsqueeze(2).to_broadcast([128, 4, 128]), ALU.mult, R=[cfT, g], W=[lu])
                pg = psP.next()
                for h in range(4):
                    p.mm(pg[:, h, :], CF("ones"), lu[:, h, :], R=[cfT, lu], W=[pg])
                yield
                p.cp("dve", sm[:, 4:8], pg[:, :, lastcol], R=[pg], W=[sm])
                X = R_["X"].next()
                p.tt("dve", X[:], pg[:], sm[:, 0:4].unsqueeze(2).to_broadcast([128, 4, 128]), ALU.subtract, R=[pg, sm], W=[X])
                E = R_["E"].next()
                p.act(X[:], X[:], AF.Relu, scale=-1.0, R=[X], W=[X])
                p.act(E[:], X[:], AF.Exp, scale=-1.0, R=[X], W=[E])
                eg = R_["eg"].next()
                p.act(eg[:], pg[:], AF.Exp, R=[pg], W=[eg])
                p.tt("dve", sm[:, 8:12], sm[:, 4:8], sm[:, 0:4], ALU.subtract, R=[sm], W=[sm])
                p.act(sm[:, 8:12], sm[:, 8:12], AF.Exp, R=[sm], W=[sm])
                p.act(sm[:, 12:16], sm[:, 0:4], AF.Exp, R=[sm], W=[sm])
                p.ts("dve", sm[:, 16:20], beta, -1.0, None, ALU.mult, R=[g], W=[sm])
                p.act(sm[:, 20:24], sm[:, 4:8], AF.Exp, R=[sm], W=[sm])
                yield
                Es = R_["Es"].next(); Ei = R_["Ei"].next()
                p.tt("pool", Es[:], E[:], ms.unsqueeze(1).to_broadcast([128, 4, 128]), ALU.mult, R=[E, cfT], W=[Es])
                p.tt("pool", Ei[:], E[:], mi.unsqueeze(1).to_broadcast([128, 4, 128]), ALU.mult, R=[E, cfT], W=[Ei])
                qd = R_["qd"].next()
                p.tt("dve", qd[:], qT[:], eg[:], ALU.mult, R=[qT, eg], W=[qd])
                kd = R_["kd"].next()
                for h in range(4):
                    p.act(kd[:, h, :], ktok[:, h, :], AF.Copy, scale=sm[:, 8 + h:9 + h], R=[ktok, sm], W=[kd])
                pk = psP.next()
                for h in range(4):
                    p.mm(pk[:, h, :], kT[:, h, :], kT[:, h, :], R=[kT], W=[pk])
                pq = psP.next()
                for h in range(4):
                    p.mm(pq[:, h, :], kT[:, h, :], qT[:, h, :], R=[kT, qT], W=[pq])
                yield
                B = R_["B"].next(); BT = R_["BT"].next(); B32 = R_["B32"].next()
                for h in range(4):
                    p.stt("dve", B32[:, h, :], pk[:, h, :], beta[:, h:h + 1], Es[:, h, :], ALU.mult, ALU.mult, R=[pk, Es, g], W=[B32])
                p.cp("act", B[:], B32[:], R=[B32], W=[B])
                qkd = R_["qkd"].next()
                p.tt("dve", qkd[:], pq[:], Ei[:], ALU.mult, R=[pq, Ei], W=[qkd])
                pb = psP.next()
                for h in range(4):
                    p.tr(pb[:, h, :], B32[:, h, :], CF("identf"), R=[B32, cfT], W=[pb])
                yield
                p.cp("act", BT[:], pb[:], R=[pb], W=[BT])
                nmn = "nmf%d" if d == 0 else "nmb%d"
                nmt = "nmb%d" if d == 0 else "nmf%d"
                bc4 = lambda ap: ap.unsqueeze(1).to_broadcast([128, 4, 128])
                G = R_["P"].next(); GT = R_["GT"].next()
                tmp = R_["tmp"].next(); tmpT = R_["tmp"].next()
                p.tt("dve", tmp[:], B[:], bc4(CF(nmn % 0)), ALU.mult, R=[B, cfT], W=[tmp])
                p.tt("dve", G[:], tmp[:], bc4(CF("identf")), ALU.add, R=[tmp, cfT], W=[G])
                p.tt("pool", tmpT[:], BT[:], bc4(CF(nmt % 0)), ALU.mult, R=[BT, cfT], W=[tmpT])
                p.tt("pool", GT[:], tmpT[:], bc4(CF("identf")), ALU.add, R=[tmpT, cfT], W=[GT])
                yield
                for lev in range(1, 7):
                    last = (lev == 6)
                    px = psP.next()
                    for h in range(4):
                        p.mm(px[:, h, :], BT[:, h, :], G[:, h, :], R=[BT, G], W=[px])
                    yield
                    Xb = R_["Xb"].next()
                    p.cp("act", Xb[:], px[:], R=[px], W=[Xb])
                    py = psP.next()
                    for h in range(4):
                        p.mm(py[:, h, :], GT[:, h, :], Xb[:, h, :], R=[GT, Xb], W=[py])
                    if not last:
                        pyt = psP.next()
                        for h in range(4):
                            p.mm(pyt[:, h, :], Xb[:, h, :], GT[:, h, :], R=[GT, Xb], W=[pyt])
                    yield
                    tmp = R_["tmp"].next()
                    p.tt("dve", tmp[:], py[:], bc4(CF(nmn % lev)), ALU.mult, R=[py, cfT], W=[tmp])
                    p.tt("dve", G[:], G[:], tmp[:], ALU.add, R=[G, tmp], W=[G])
                    if not last:
                        tmpT = R_["tmp"].next()
                        p.tt("dve", tmpT[:], pyt[:], bc4(CF(nmt % lev)), ALU.mult, R=[pyt, cfT], W=[tmpT])
                        p.tt("pool", GT[:], GT[:], tmpT[:], ALU.add, R=[GT, tmpT], W=[GT])
                    yield
                P = G
                prepped[d][it] = dict(c=c, kT=kT, vtok=vtok, sm=sm, P=P, qkd=qkd, qd=qd, kd=kd)

            state = {}

            def scan(d, it):
                R_ = ins[d]
                pr = prepped[d].pop(it)
                c = pr["c"]; sm = pr["sm"]
                S32 = R_["S32"]
                Sbf = state[d]
                pks = psSc[d].next()
                for h in range(4):
                    p.mm(pks[:, h, :], pr["kT"][:, h, :], Sbf[:, h, :], R=[pr["kT"], Sbf], W=[pks])
                yield
                nR = R_["nR"].next()
                for h in range(4):
                    p.stt("dve", nR[:, h, :], pks[:, h, :], sm[:, 12 + h:13 + h], pr["vtok"][:, h, :], ALU.mult, ALU.subtract,
                          R=[pks, sm, pr["vtok"]], W=[nR])
                pw_ = psSc[d].next()
                for h in range(4):
                    p.mm(pw_[:, h, :], pr["P"][:, h, :], nR[:, h, :], R=[pr["P"], nR], W=[pw_])
                yield
                w = R_["w"].next()
                for h in range(4):
                    p.act(w[:, h, :], pw_[:, h, :], AF.Copy, scale=sm[:, 16 + h:17 + h], R=[pw_, sm], W=[w])
                if c >= 2:
                    po = psSc[d].next()
                    for h in range(4):
                        p.mm(po[:, h, :], pr["qd"][:, h, :], Sbf[:, h, :], start=True, stop=False, R=[pr["qd"], Sbf], W=[po])
                        p.mm(po[:, h, :], pr["qkd"][:, h, :], w[:, h, :], start=False, stop=True, R=[pr["qkd"], w], W=[po])
                pds = psSc[d].next()
                for h in range(4):
                    p.mm(pds[:, h, :], pr["kd"][:, h, :], w[:, h, :], R=[pr["kd"], w], W=[pds])
                yield
                for h in range(4):
                    p.stt("dve", S32[:, h, :], S32[:, h, :], sm[:, 20 + h:21 + h], pds[:, h, :], ALU.mult, ALU.add,
                          R=[S32, sm, pds], W=[S32])
                nS = R_["Sbf"].next()
                p.cp("act", nS[:], S32[:], R=[S32], W=[nS])
                state[d] = nS
                if c >= 2:
                    osb = R_["osb"].next()
                    p.cp("act", osb[:], po[:], R=[po], W=[osb])
                    tl = (c - 2) * 128
                    dst = S["of"] if d == 0 else S["ob"]
                    p.dma("sp", dst[tl:tl + 128, :].rearrange("p (h d) -> p h d", h=4), osb[:], R=[osb])
                yield

            for d in range(2):
                p.memset("pool", ins[d]["S32"][:], 0.0, W=[ins[d]["S32"]])
                s0 = ins[d]["Sbf"].next()
                p.memset("pool", s0[:], 0.0, W=[s0])
                state[d] = s0
            interleave([prep(0, 0), prep(1, 0)])
            import os
            nit = int(os.environ.get("DELTA_ITERS", NCH))
            for it in range(nit):
                gens = [scan(0, it), scan(1, it)]
                if it + 1 < NCH:
                    gens += [prep(0, it + 1), prep(1, it + 1)]
                interleave(gens)

    def phase_even_out():
        with p.scope():
            wo = p.sb([128, 8, D], BF16, "wo")
            for k in range(8):
                p.dma("pool", wo[:, k, :], I["ev_w_out"][k * 128:(k + 1) * 128, :], W=[wo])
            onorm = p.sb([128, 128], F32)
            rowvec(onorm, I["dn_norm"])
            g1 = p.sb([128, D], F32)
            rowvec(g1, modv(0, 0, 2))
            ofr = p.ring(2, [128, 4, 128], F32); obr = p.ring(2, [128, 4, 128], F32)
            zr = p.ring(2, [128, 4, 128], BF16); xr = p.ring(3, [128, D], F32)
            plr = p.ring(3, [128, 4, 128], BF16)
            junk = p.sb([128, 128], F32)
            ssr = p.ring(2, [128, 8], F32)
            omr = p.ring(3, [128, 4, 128], BF16)
            mxr = p.ring(2, [128, 4, 128], BF16)
            psB = p.ring(2, [128, 8, 128], BF16, psum=True)
            psY = p.ring(4, [128, 512], F32, psum=True)
            x1r = p.ring(2, [128, D], F32)
            def eA(j):
                t0 = j * 128
                of = ofr.next(); ob = obr.next(); z = zr.next(); xt = xr.next(); pl = plr.next()
                p.dma("sp", of[:], S["of"][t0:t0 + 128, :].rearrange("p (h d) -> p h d", h=4), W=[of])
                p.dma("sp", ob[:], S["ob"][t0:t0 + 128, :].rearrange("p (h d) -> p h d", h=4), W=[ob])
                p.dma("sp", z[:], S["zs"][t0:t0 + 128, :].rearrange("p (h d) -> p h d", h=4), W=[z])
                p.dma("sp", xt[:], I["x"][t0:t0 + 128, :], W=[xt])
                p.dma("sp", pl[:], S["mixT"][4:8, :, t0:t0 + 128].rearrange("k p t -> p k t"), W=[pl])
                p.tt("pool", of[:], of[:], ob[:], ALU.add, R=[of, ob], W=[of])
                ss = ssr.next()
                p.memset("pool", ss[:], 0.0, W=[ss])
                for h in range(4):
                    p.act(junk[:], of[:, h, :], AF.Square, accum=ss[:, h:h + 1], R=[of], W=[junk, ss])
                p.ts("dve", ss[:, 4:8], ss[:, 0:4], 1.0 / 128, EPS, ALU.mult, ALU.add, R=[ss], W=[ss])
                p.rsqrt(ss[:, 4:8], ss)
                p.tt("dve", of[:], of[:], ss[:, 4:8].unsqueeze(2).to_broadcast([128, 4, 128]), ALU.mult, R=[of, ss], W=[of])
                p.tt("pool", of[:], of[:], onorm[:, :].unsqueeze(1).to_broadcast([128, 4, 128]), ALU.mult, R=[of, onorm], W=[of])
                om = omr.next()
                p.tt("dve", om[:], of[:], z[:], ALU.mult, R=[of, z], W=[om])
                return (t0, om, pl, xt)

            def eB(st):
                t0, om, pl, xt = st
                pb = psB.next()
                for h in range(4):
                    p.tr(pb[:, h, :], om[:, h, :], CB("ident"), R=[om, cbT], W=[pb])
                mx = mxr.next()
                p.cp("act", mx[:], pb[:, 0:4, :], R=[pb], W=[mx])
                x1 = x1r.next()
                for nh in range(2):
                    py = psY.next()
                    for k in range(8):
                        lhs = mx[:, k, :] if k < 4 else pl[:, k - 4, :]
                        p.mm(py[:], lhs, wo[:, k, nh * 512:(nh + 1) * 512], start=(k == 0), stop=(k == 7), R=[mx, pl, wo], W=[py])
                    p.tt("dve", x1[:, nh * 512:(nh + 1) * 512], py[:], g1[:, nh * 512:(nh + 1) * 512], ALU.mult, R=[py, g1], W=[x1])
                p.tt("pool", x1[:], x1[:], xt[:], ALU.add, R=[x1, xt], W=[x1])
                p.dma("pool", S["xl"][t0:t0 + 128, :], x1[:], R=[x1])

            pend = None
            for j in range(NT):
                st = eA(j)
                if pend is not None:
                    eB(pend)
                pend = st
            eB(pend)

    def phase_odd():
        with p.scope():
            w = p.sb([128, 8, 3 * D], BF16, "wsc")
            for k in range(8):
                p.dma("pool", w[:, k, :], I["sc_w_in"][k * 128:(k + 1) * 128, :], W=[w])
            wo = p.sb([128, 8, D], BF16, "wsco")
            for k in range(8):
                p.dma("pool", wo[:, k, :], I["sc_w_out"][k * 128:(k + 1) * 128, :], W=[wo])
            cw = p.sb([128, 8, 3], F32)
            p.dma("sp", cw[:], I["sc_conv"].rearrange("p (c t) -> p c t", t=3), W=[cw])
            g1 = p.sb([128, D], F32)
            rowvec(g1, modv(1, 0, 2))
            hwr = p.ring(2, [128, 8, 258], BF16)
            psW = p.ring(5, [128, 512], F32, psum=True)
            psY = p.ring(3, [128, 512], F32, psum=True)
            gcr = p.ring(2, [128, 258], F32)
            ur = p.ring(2, [128, 258], F32)
            accr = p.ring(2, [128, 256], F32)
            mixr = p.ring(2, [128, 8, 256], BF16)
            xr = p.ring(2, [128, D], F32)
            x1r = p.ring(2, [128, D], F32)
            for blk in range(L // 256):
                hw = hwr.next()
                load_window(hw, CTX, L, blk)
                mix = mixr.next()
                for c in range(8):
                    pss = []
                    for part in range(3):
                        ps = psW.next()
                        col = part * D + c * 128
                        for k in range(8):
                            p.mm(ps[:, 0:258], w[:, k, col:col + 128], hw[:, k, :], start=(k == 0), stop=(k == 7), R=[w, hw], W=[ps])
                        pss.append(ps)
                    gc = gcr.next()
                    p.cp("act", gc[:], pss[1][:, 0:258], R=[pss[1]], W=[gc])
                    u = ur.next()
                    p.tt("dve", u[:], gc[:], pss[2][:, 0:258], ALU.mult, R=[gc, pss[2]], W=[u])
                    acc = accr.next()
                    p.ts("pool", acc[:], u[:, 1:257], cw[:, c, 1:2], None, ALU.mult, R=[u, cw], W=[acc])
                    p.stt("dve", acc[:], u[:, 0:256], cw[:, c, 0:1], acc[:], ALU.mult, ALU.add, R=[u, cw, acc], W=[acc])
                    p.stt("dve", acc[:], u[:, 2:258], cw[:, c, 2:3], acc[:], ALU.mult, ALU.add, R=[u, cw, acc], W=[acc])
                    p.tt("dve", mix[:, c, :], acc[:], pss[0][:, 1:257], ALU.mult, R=[acc, pss[0]], W=[mix])
                for t2 in range(2):
                    t0 = blk * 256 + t2 * 128
                    xt = xr.next()
                    p.dma("act", xt[:], S["xl"][t0:t0 + 128, :], W=[xt])
                    x1 = x1r.next()
                    for nh in range(2):
                        py = psY.next()
                        for k in range(8):
                            p.mm(py[:], mix[:, k, t2 * 128:(t2 + 1) * 128], wo[:, k, nh * 512:(nh + 1) * 512],
                                 start=(k == 0), stop=(k == 7), R=[mix, wo], W=[py])
                        p.tt("dve", x1[:, nh * 512:(nh + 1) * 512], py[:], g1[:, nh * 512:(nh + 1) * 512], ALU.mult, R=[py, g1], W=[x1])
                    p.tt("pool", x1[:], x1[:], xt[:], ALU.add, R=[x1, xt], W=[x1])
                    p.dma("pool", S["xl"][t0:t0 + 128, :], x1[:], R=[x1, xt])

    def phase_moe(i):
        with p.scope():
            affall = p.sb([128, NT, NE], F32, "affall")
            idx = p.sb([128, NE, NM], I32, "idx")
            gate = p.sb([128, NE, NM], F32, "gate")
            g2 = p.sb([128, D], F32, "g2")
            rowvec(g2, modv(i, 0, 5))
            with p.scope():
                Ar = p.sb([128, D], F32); Br = p.sb([128, D], F32); gr_ = p.sb([128, D], F32)
                rowvec(gr_, I["norm_ffn"][i])
                rowvec(Br, modv(i, 0, 3))
                rowvec(Ar, modv(i, 0, 4))
                p.stt("dve", Ar[:], Ar[:], 1.0, gr_[:], ALU.add, ALU.mult, R=[Ar, gr_], W=[Ar])
                r32 = p.sb([128, 8, NE], F32); rhi = p.sb([128, 8, NE], BF16); rlo = p.sb([128, 8, NE], BF16)
                p.dma("sp", r32[:], I["router"][i].rearrange("(k p) e -> p k e", p=128), W=[r32])
                p.cp("dve", rhi[:], r32[:], R=[r32], W=[rhi])
                p.tt("dve", r32[:], r32[:], rhi[:], ALU.subtract, R=[r32, rhi], W=[r32])
                p.cp("dve", rlo[:], r32[:], R=[r32], W=[rlo])
                xr = p.ring(2, [128, D], F32); junk = p.sb([128, D], BF16)
                ssr = p.ring(2, [128, 8], F32)
                hr = p.ring(2, [128, D], F32); hir = p.ring(2, [128, D], BF16); lor = p.ring(2, [128, D], BF16)
                psT = p.ring(4, [128, D], BF16, psum=True)
                hiTr = p.ring(2, [128, D], BF16); loTr = p.ring(2, [128, D], BF16)
                psL = p.ring(2, [128, 512], F32, psum=True)
                er = p.ring(2, [128, NE], F32)
                def mA(j):
                    t0 = j * 128
                    xt = xr.next()
                    p.dma("sp", xt[:], S["xl"][t0:t0 + 128, :], W=[xt])
                    ss = ssr.next()
                    p.memset("pool", ss[:], 0.0, W=[ss])
                    p.act(junk[:], xt[:], AF.Square, accum=ss[:, 0:1], R=[xt], W=[junk, ss])
                    p.ts("dve", ss[:, 1:2], ss[:, 0:1], 1.0 / D, EPS, ALU.mult, ALU.add, R=[ss], W=[ss])
                    p.rsqrt(ss[:, 1:2], ss)
                    h = hr.next()
                    p.stt("dve", h[:], xt[:], ss[:, 1:2], Ar[:], ALU.mult, ALU.mult, R=[xt, ss, Ar], W=[h])
                    p.tt("dve", h[:], h[:], Br[:], ALU.add, R=[h, Br], W=[h])
                    hi = hir.next(); lo = lor.next()
                    p.cp("act", hi[:], h[:], R=[h], W=[hi])
                    p.dma("pool", S["h2"][t0:t0 + 128, :], hi[:], R=[hi])
                    p.tt("dve", lo[:], h[:], hi[:], ALU.subtract, R=[h, hi], W=[lo])
                    return (j, ss, hi, lo)

                def mB(st):
                    j, ss, hi, lo = st
                    pTh = psT.next(); pTl = psT.next()
                    for k in range(8):
                        p.tr(pTh[:, k * 128:(k + 1) * 128], hi[:, k * 128:(k + 1) * 128], CB("ident"), R=[hi, cbT], W=[pTh])
                    for k in range(8):
                        p.tr(pTl[:, k * 128:(k + 1) * 128], lo[:, k * 128:(k + 1) * 128], CB("ident"), R=[lo, cbT], W=[pTl])
                    hiT = hiTr.next(); loT = loTr.next()
                    p.cp("act", hiT[:], pTh[:], R=[pTh], W=[hiT])
                    p.cp("dve", loT[:], pTl[:], R=[pTl], W=[loT])
                    pl = psL.next()
                    n = 0
                    for (a, b) in ((hiT, rhi), (loT, rhi), (hiT, rlo)):
                        for k in range(8):
                            p.mm(pl[:, 0:NE], a[:, k * 128:(k + 1) * 128], b[:, k, :], start=(n == 0), stop=(n == 23), R=[a, b], W=[pl])
                            n += 1
                    p.op("dve", lambda e, o=ss[:, 2:3], a=pl[:, 0:NE]: e.tensor_reduce(out=o, in_=a, axis=AX.X, op=ALU.max), R=[pl], W=[ss])
                    p.ts("dve", ss[:, 3:4], ss[:, 2:3], -1.0, None, ALU.mult, R=[ss], W=[ss])
                    ex = er.next()
                    p.act(ex[:], pl[:, 0:NE], AF.Exp, bias=ss[:, 3:4], accum=ss[:, 4:5], R=[pl, ss], W=[ex, ss])
                    p.op("dve", lambda e, o=ss[:, 5:6], a=ss[:, 4:5]: e.reciprocal(out=o, in_=a), R=[ss], W=[ss])
                    p.ts("dve", affall[:, j, :], ex[:], ss[:, 5:6], None, ALU.mult, R=[ex, ss], W=[affall])

                pend = None
                for j in range(NT):
                    st = mA(j)
                    if pend is not None:
                        mB(pend)
                    pend = st
                mB(pend)
            with p.scope():
                NB_ = NT * NE
                lo = p.sb([128, NE], F32); hi = p.sb([128, NE], F32); mid = p.sb([128, NE], F32)
                sel = p.sb([128, NE], F32); d1 = p.sb([128, NE], F32)
                cmp_ = p.sb([128, NT, NE], BF16); cntp = p.sb([128, NE], F32)
                psc = p.ring(2, [128, 512], F32, psum=True)
                p.memset("dve", lo[:], 0.0, W=[lo])
                p.memset("dve", hi[:], 2.0, W=[hi])
                for itn in range(32):
                    p.tt("dve", mid[:], lo[:], hi[:], ALU.add, R=[lo, hi], W=[mid])
                    p.ts("dve", mid[:], mid[:], 0.5, None, ALU.mult, R=[mid], W=[mid])
                    p.tt("dve", cmp_[:], affall[:], mid[:, :].unsqueeze(1).to_broadcast([128, NT, NE]), ALU.is_ge, R=[affall, mid], W=[cmp_])
                    p.op("dve", lambda e: e.tensor_reduce(out=cntp[:], in_=cmp_[:].rearrange("p j e -> p e j"), axis=AX.X, op=ALU.add),
                         R=[cmp_], W=[cntp])
                    pc = psc.next()
                    p.mm(pc[:, 0:NE], CF("ones"), cntp[:], R=[cfT, cntp], W=[pc])
                    p.ts("dve", sel[:], pc[:, 0:NE], float(CAP) - 0.5, None, ALU.is_ge, R=[pc], W=[sel])
                    p.tt("dve", d1[:], mid[:], lo[:], ALU.subtract, R=[mid, lo], W=[d1])
                    p.tt("dve", d1[:], d1[:], sel[:], ALU.mult, R=[d1, sel], W=[d1])
                    p.tt("dve", lo[:], lo[:], d1[:], ALU.add, R=[lo, d1], W=[lo])
                    p.tt("dve", d1[:], hi[:], mid[:], ALU.subtract, R=[hi, mid], W=[d1])
                    p.tt("dve", d1[:], d1[:], sel[:], ALU.mult, R=[d1, sel], W=[d1])
                    p.tt("dve", hi[:], mid[:], d1[:], ALU.add, R=[mid, d1], W=[hi])
                selm = p.sb([128, NT, NE], F32); ca = p.sb([128, NT, NE], F32); cb_ = p.sb([128, NT, NE], F32)
                p.tt("dve", selm[:], affall[:], lo[:, :].unsqueeze(1).to_broadcast([128, NT, NE]), ALU.is_ge, R=[affall, lo], W=[selm])
                p.cp("dve", ca[:], selm[:], R=[selm], W=[ca])
                cur, oth = ca, cb_
                s = 1
                while s < NT:
                    p.cp("pool", oth[:, 0:s, :], cur[:, 0:s, :], R=[cur], W=[oth])
                    p.tt("dve", oth[:, s:, :], cur[:, s:, :], cur[:, 0:NT - s, :], ALU.add, R=[cur], W=[oth])
                    cur, oth = oth, cur
                    s *= 2
                totb = p.sb([128, NE], BF16)
                p.cp("dve", totb[:], cur[:, NT - 1, :], R=[cur], W=[totb])
                pp_ = psc.next()
                p.mm(pp_[:, 0:NE], CB("lstrict"), totb[:], R=[cbT, totb], W=[pp_])
                pref = p.sb([128, NE], F32)
                p.cp("dve", pref[:], pp_[:, 0:NE], R=[pp_], W=[pref])
                pos = oth
                p.tt("dve", pos[:], cur[:], selm[:], ALU.subtract, R=[cur, selm], W=[pos])
                p.tt("dve", pos[:], pos[:], pref[:, :].unsqueeze(1).to_broadcast([128, NT, NE]), ALU.add, R=[pos, pref], W=[pos])
                posi = p.sb([128, NT, NE], I32); pdi = p.sb([128, NT, NE], I32); pmi = p.sb([128, NT, NE], I32)
                pdf = p.sb([128, NT, NE], F32); pmf = p.sb([128, NT, NE], F32)
                p.cp("dve", posi[:], pos[:], R=[pos], W=[posi])
                p.op("dve", lambda e: e.tensor_single_scalar(out=pdi[:], in_=posi[:], scalar=7, op=ALU.arith_shift_right), R=[posi], W=[pdi])
                p.op("dve", lambda e: e.tensor_single_scalar(out=pmi[:], in_=posi[:], scalar=127, op=ALU.bitwise_and), R=[posi], W=[pmi])
                p.cp("dve", pdf[:], pdi[:], R=[pdi], W=[pdf])
                p.cp("dve", pmf[:], pmi[:], R=[pmi], W=[pmf])
                p.stt("dve", pmf[:], pmf[:], 1.0, selm[:], ALU.add, ALU.mult, R=[pmf, selm], W=[pmf])
                p.ts("dve", pmf[:], pmf[:], -1.0, None, ALU.add, R=[pmf], W=[pmf])
                val4 = p.sb([128, NT, NE, 4], F32)
                ahi = p.sb([128, NT, NE], BF16)
                p.cp("pool", val4[:, :, :, 0], cfT[:, OF["misc"]:OF["misc"] + 1].unsqueeze(2).to_broadcast([128, NT, NE]), R=[cfT], W=[val4])
                jrow = cfT[:, OF["iota"]:OF["iota"] + NT]
                p.cp("pool", val4[:, :, :, 1], jrow.unsqueeze(2).to_broadcast([128, NT, NE]), R=[cfT], W=[val4])
                p.cp("dve", ahi[:], affall[:], R=[affall], W=[ahi])
                p.cp("dve", val4[:, :, :, 2], ahi[:], R=[ahi], W=[val4])
                p.tt("dve", val4[:, :, :, 3], affall[:], ahi[:], ALU.subtract, R=[affall, ahi], W=[val4])
                iota8 = cfT[:, OF["misc"] + 8:OF["misc"] + 16]
                JC = 8
                Mh = p.sb([128, JC, NE, 8], F32)
                Rr = p.ring(2, [128, NT, 128], BF16)
                pacc = psc.next()
                rhs_all = [p.sb([128, JC, NE, 8, 4], BF16) for _ in range((NT + JC - 1) // JC)]
                for jc in range(0, NT, JC):
                    n = min(JC, NT - jc)
                    p.tt("dve", Mh[:, 0:n], pdf[:, jc:jc + n, :].unsqueeze(3).to_broadcast([128, n, NE, 8]),
                         iota8.unsqueeze(1).unsqueeze(1).to_broadcast([128, n, NE, 8]), ALU.is_equal, R=[pdf, cfT], W=[Mh])
                    ra = rhs_all[jc // JC]
                    p.tt("pool", ra[:, 0:n], Mh[:, 0:n].unsqueeze(4).to_broadcast([128, n, NE, 8, 4]),
                         val4[:, jc:jc + n].unsqueeze(3).to_broadcast([128, n, NE, 8, 4]), ALU.mult, R=[Mh, val4], W=[ra])
                for e_ in range(NE):
                    Rm = Rr.next()
                    p.tt("dve", Rm[:], CF("iota").unsqueeze(1).to_broadcast([128, NT, 128]),
                         pmf[:, :, e_:e_ + 1].to_broadcast([128, NT, 128]), ALU.is_equal, R=[cfT, pmf], W=[Rm])
                    for j in range(NT):
                        ra = rhs_all[j // JC]
                        p.mm(pacc[:, e_ * 32:(e_ + 1) * 32], Rm[:, j, :], ra[:, j % JC, e_].rearrange("p m g -> p (m g)"),
                             start=(j == 0), stop=(j == NT - 1), R=[Rm, ra], W=[pacc])
                accs = p.sb([128, 512], F32)
                p.cp("dve", accs[:], pacc[:, 0:512], R=[pacc], W=[accs])
                accv = accs[:, 0:512].rearrange("p (e m g) -> p e m g", e=NE, m=8)
                lf = p.sb([128, NE, NM], F32)
                p.stt("dve", lf[:], accv[:, :, 0:NM, 1], 128.0, accv[:, :, 0:NM, 0], ALU.mult, ALU.add, R=[accs], W=[lf])
                p.cp("dve", idx[:], lf[:], R=[lf], W=[idx])
                p.tt("dve", gate[:], accv[:, :, 0:NM, 2], accv[:, :, 0:NM, 3], ALU.add, R=[accs], W=[gate])
            with p.scope():
                wgr = p.ring(2, [128, 8, 512], BF16, name="wg"); wur = p.ring(2, [128, 8, 512], BF16, name="wu")
                wdr = p.ring(2, [128, 4, D], BF16, name="wd")
                xgr = p.ring(2 * NM + 2, [128, D], BF16)
                xgTr = p.ring(2, [128, 8, CAP], BF16, name="xgT")
                hidr = p.ring(2, [128, 4, CAP], BF16, name="hid")
                psT = p.ring(2, [128, D], BF16, psum=True)
                psG = p.ring(4, [128, 512], F32, psum=True)
                psY = p.ring(2, [128, 512], F32, psum=True)
                sgr = p.ring(2, [128, 512], F32)
                yr = p.ring(2, [128, D], F32)
                xacc = Tile(None)
                SH = min(512, CAP)
                def issue_loads(e_):
                    wg = wgr.next(); wu = wur.next(); wd = wdr.next()
                    p.dma("pool", wg[:], I["w_gate"][i, e_].rearrange("(k p) f -> p k f", p=128), W=[wg])
                    p.dma("pool", wu[:], I["w_up"][i, e_].rearrange("(k p) f -> p k f", p=128), W=[wu])
                    p.dma("pool", wd[:], I["w_down"][i, e_].rearrange("(k p) f -> p k f", p=128), W=[wd])
                    xgs = []
                    for m in range(NM):
                        xg = xgr.next()
                        p.dmaf("pool", lambda e, o=xg[:], ix=idx[:, e_, m:m + 1]: e.indirect_dma_start(
                            out=o, out_offset=None, in_=S["h2"][:, :], in_offset=bass.IndirectOffsetOnAxis(ap=ix, axis=0)),
                            R=[idx], W=[xg])
                        xgs.append(xg)
                    return wg, wu, wd, xgs

                nxt = issue_loads(0)
                for e_ in range(NE):
                    wg, wu, wd, xgs = nxt
                    if e_ + 1 < NE:
                        nxt = issue_loads(e_ + 1)
                    xgT = xgTr.next()
                    for m in range(NM):
                        xg = xgs[m]
                        pt = psT.next()
                        for k in range(8):
                            p.tr(pt[:, k * 128:(k + 1) * 128], xg[:, k * 128:(k + 1) * 128], CB("ident"), R=[xg, cbT], W=[pt])
                        p.cp("act" if m % 2 == 0 else "dve", xgT[:, :, m * 128:(m + 1) * 128], pt[:].rearrange("p (k t) -> p k t", k=8), R=[pt], W=[xgT])
                    hid = hidr.next()
                    for fc in range(4):
                        for sh in range(CAP // SH):
                            pg = psG.next(); pu = psG.next()
                            for k in range(8):
                                p.mm(pg[:, 0:SH], wg[:, k, fc * 128:(fc + 1) * 128], xgT[:, k, sh * SH:(sh + 1) * SH],
                                     start=(k == 0), stop=(k == 7), R=[wg, xgT], W=[pg])
                            for k in range(8):
                                p.mm(pu[:, 0:SH], wu[:, k, fc * 128:(fc + 1) * 128], xgT[:, k, sh * SH:(sh + 1) * SH],
                                     start=(k == 0), stop=(k == 7), R=[wu, xgT], W=[pu])
                            sg = sgr.next()
                            p.act(sg[:, 0:SH], pg[:, 0:SH], AF.Silu, R=[pg], W=[sg])
                            p.tt("dve", hid[:, fc, sh * SH:(sh + 1) * SH], sg[:, 0:SH], pu[:, 0:SH], ALU.mult, R=[sg, pu], W=[hid])
                    for m in range(NM):
                        y = yr.next()
                        for dh in range(2):
                            py = psY.next()
                            for fk in range(4):
                                p.mm(py[:], hid[:, fk, m * 128:(m + 1) * 128], wd[:, fk, dh * 512:(dh + 1) * 512],
                                     start=(fk == 0), stop=(fk == 3), R=[hid, wd], W=[py])
                            p.stt("dve", y[:, dh * 512:(dh + 1) * 512], py[:], gate[:, e_, m:m + 1], g2[:, dh * 512:(dh + 1) * 512],
                                  ALU.mult, ALU.mult, R=[py, gate, g2], W=[y])
                        p.dmaf("pool", lambda e, s_=y[:], ix=idx[:, e_, m:m + 1]: e.indirect_dma_start(
                            out=S["xl"][:, :], out_offset=bass.IndirectOffsetOnAxis(ap=ix, axis=0), in_=s_, in_offset=None,
                            compute_op=ALU.add), R=[y, idx], W=[xacc])

    def phase_final():
        with p.scope():
            gfin = p.sb([128, D], F32)
            rowvec(gfin, I["norm_final"])
            xr = p.ring(3, [128, D], F32); junk = p.sb([128, D], BF16); ssr = p.ring(3, [128, 2], F32)
            orr = p.ring(3, [128, D], F32)
            for j in range(NT):
                t0 = j * 128
                xt = xr.next()
                p.dma("sp", xt[:], S["xl"][t0:t0 + 128, :], W=[xt])
                ss = ssr.next()
                p.memset("pool", ss[:], 0.0, W=[ss])
                p.act(junk[:], xt[:], AF.Square, accum=ss[:, 0:1], R=[xt], W=[junk, ss])
                p.ts("dve", ss[:, 1:2], ss[:, 0:1], 1.0 / D, EPS, ALU.mult, ALU.add, R=[ss], W=[ss])
                p.rsqrt(ss[:, 1:2], ss)
                o = orr.next()
                p.stt("dve", o[:], xt[:], ss[:, 1:2], gfin[:], ALU.mult, ALU.mult, R=[xt, ss, gfin], W=[o])
                p.dma("act", out[t0:t0 + 128, :], o[:], R=[o])

    phases = [
        ("hT0", lambda: phase_hT([(I["ctx"], CTX, 0, 1), (I["x"], L, CTX, 0)], 0, I["norm_mix"][0])),
        ("proj", phase_even_proj), ("delta", phase_delta), ("evout", phase_even_out),
        ("moe0", lambda: phase_moe(0)),
        ("hT1", lambda: phase_hT([(S["xl"], L, CTX, 0)], 1, I["norm_mix"][1])),
        ("odd", phase_odd), ("moe1", lambda: phase_moe(1)), ("final", phase_final),
    ]
    for nm, fn in phases:
        fn()
        if stop == nm:
            break
    p.flush()
    p.finish()
    print("bass instructions:", p.ninst, {e: p.cnt[e] for e in p.ENG})
    return nc


def prep_inputs(b, inputs, L):
    arr_f, _, arr_b, _ = make_consts()
    f = lambda a: np.ascontiguousarray(np.asarray(a, dtype=np.float32))
    c = f(inputs["c"])[b]
    cc = f(inputs["c_ctx"])
    cvec = np.concatenate([c.reshape(8, 128).T, cc.reshape(8, 128).T], axis=1)
    m = dict(
        x=f(inputs["x"])[b][:L], ctx=f(inputs["ctx"])[b], cvec=f(cvec),
        ada_w=f(inputs["ada_w"]), ada_b=f(inputs["ada_b"]),
        norm_mix=f(inputs["norm_mix"]), norm_ffn=f(inputs["norm_ffn"]), norm_final=f(inputs["norm_final"]),
        ev_w_in=f(inputs["ev_w_in"])[0],
        dn_conv=f(f(inputs["dn_conv"])[0].reshape(3, 12, 128).transpose(2, 1, 0).reshape(128, 36)),
        dn_a_log=f(inputs["dn_a_log"])[0].reshape(8), dn_dt_bias=f(inputs["dn_dt_bias"])[0].reshape(8),
        dn_norm=f(inputs["dn_norm"])[0], pool_w=f(inputs["pool_w"])[0],
        pool_scale=f(f(inputs["pool_scale"])[0].reshape(4, 128).T),
        ev_w_out=f(inputs["ev_w_out"])[0],
        sc_w_in=f(inputs["sc_w_in"])[0],
        sc_conv=f(f(inputs["sc_conv"])[0].reshape(3, 8, 128).transpose(2, 1, 0).reshape(128, 24)),
        sc_w_out=f(inputs["sc_w_out"])[0],
        router=f(inputs["router"]), w_gate=f(inputs["w_gate"]), w_up=f(inputs["w_up"]), w_down=f(inputs["w_down"]),
        cf=arr_f, cb=arr_b,
    )
    return m


_NC_CACHE = {}


def kernel(**inputs):
    x = np.asarray(inputs["x"])
    B, L, _ = x.shape
    if L not in _NC_CACHE:
        _NC_CACHE[L] = build(L)
    nc = _NC_CACHE[L]
    maps = [prep_inputs(b, inputs, L) for b in range(B)]
    n = 8
    in_maps = [maps[c % B] for c in range(n)]
    res = run_bass_kernel_spmd(nc, in_maps, core_ids=list(range(n)))
    outs = [np.asarray(res.results[b]["out"], dtype=np.float32) for b in range(B)]
    return np.stack(outs, axis=0)
```

```python
import numpy as np
import ml_dtypes
from contextlib import ExitStack
import concourse.bass as bass
import concourse.mybir as mybir
from concourse.bass_utils import run_bass_kernel_spmd

F32 = mybir.dt.float32
BF16 = mybir.dt.bfloat16
I32 = mybir.dt.int32
ALU = mybir.AluOpType
AF = mybir.ActivationFunctionType
AX = mybir.AxisListType
EPS = 1e-6
D = 1024
CTX = 256
NE = 16


class Buf:
    __slots__ = ("w", "r")

    def __init__(self):
        self.w = None
        self.r = []


class Tile:
    def __init__(self, t):
        self.t = t
        self.b = Buf()

    def __getitem__(self, k):
        return self.t[k]


class Ring:
    def __init__(self, tiles):
        self.tiles = tiles
        self.i = 0

    def next(self):
        t = self.tiles[self.i % len(self.tiles)]
        self.i += 1
        return t


class Prog:
    ENG = ("pe", "act", "dve", "pool", "sp")
    NDMA = 48
    NHW = 32

    def __init__(self, nc):
        self.nc = nc
        self.es = ExitStack()
        self.q = {e: [] for e in self.ENG}
        self.cnt = {e: 0 for e in self.ENG}
        self.seen = {e: {} for e in self.ENG}
        self.sem = {e: self.es.enter_context(nc.semaphore("pg_" + e)) for e in self.ENG}
        self.dsem = [self.es.enter_context(nc.semaphore("pg_d%d" % i)) for i in range(self.NDMA)]
        self.dcnt = [0] * self.NDMA
        self.dnext = 0
        self.dnext_sw = 0
        self.ninst = 0
        self.uid = 0

    def sb(self, shape, dt, name=None):
        self.uid += 1
        return Tile(self.es.enter_context(self.nc.sbuf_tensor("%s_%d" % (name or "sb", self.uid), list(shape), dt)))

    def ps(self, shape, dt, name=None):
        self.uid += 1
        return Tile(self.es.enter_context(self.nc.psum_tensor("%s_%d" % (name or "ps", self.uid), list(shape), dt)))

    def ring(self, n, shape, dt, psum=False, name=None):
        return Ring([(self.ps if psum else self.sb)(shape, dt, name) for _ in range(n)])

    def _semof(self, tl):
        return self.sem[tl] if isinstance(tl, str) else self.dsem[tl]

    def _deps(self, eng, reads, writes):
        deps = {}

        def add(tok):
            if tok is None:
                return
            tl, v = tok
            if tl == "pe" and eng == "pe":
                return
            if deps.get(tl, 0) < v:
                deps[tl] = v
        for b in reads:
            add(b.b.w)
        for b in writes:
            add(b.b.w)
            for t in b.b.r:
                add(t)
        waits = []
        sn = self.seen[eng]
        for tl, v in deps.items():
            if sn.get(tl, 0) < v:
                sn[tl] = v
                waits.append((tl, v))
        return waits

    def _mark(self, tok, reads, writes):
        for b in reads:
            b.b.r.append(tok)
            if len(b.b.r) > 64:
                b.b.r = b.b.r[-48:]
        for b in writes:
            b.b.w = tok
            b.b.r = []

    def op(self, eng, fn, R=(), W=()):
        waits = self._deps(eng, R, W)
        self.cnt[eng] += 1
        tok = (eng, self.cnt[eng])
        self.q[eng].append((waits, fn, (eng, 1)))
        self._mark(tok, R, W)
        self.ninst += 1
        return tok

    def dma(self, eng, out, in_, R=(), W=(), **kw):
        return self.dmaf(eng, lambda e: e.dma_start(out=out, in_=in_, **kw), R, W)

    def dmaf(self, eng, fn, R=(), W=()):
        waits = self._deps(eng, R, W)
        if eng == "pool":
            k = self.NHW + self.dnext_sw
            self.dnext_sw = (self.dnext_sw + 1) % (self.NDMA - self.NHW)
        else:
            k = self.dnext
            self.dnext = (self.dnext + 1) % self.NHW
        if self.dcnt[k] > 0 and self.seen[eng].get(k, 0) < self.dcnt[k]:
            self.seen[eng][k] = self.dcnt[k]
            waits.append((k, self.dcnt[k]))
        self.dcnt[k] += 16
        tok = (k, self.dcnt[k])
        self.q[eng].append((waits, fn, (k, 16)))
        self._mark(tok, R, W)
        self.ninst += 1
        return tok

    def mm(self, out, lhsT, rhs, start=True, stop=True, R=(), W=()):
        return self.op("pe", lambda e: e.matmul(out, lhsT=lhsT, rhs=rhs, start=start, stop=stop), R, W)

    def tr(self, out, in_, ident, R=(), W=()):
        return self.op("pe", lambda e: e.transpose(out, in_, ident), R, W)

    def act(self, out, in_, func, bias=None, scale=1.0, accum=None, R=(), W=()):
        def f(e):
            kw = {}
            if bias is not None:
                kw["bias"] = bias
            if accum is not None:
                kw["accum_out"] = accum
            return e.activation(out=out, in_=in_, func=func, scale=scale, **kw)
        return self.op("act", f, R, W)

    def ts(self, eng, out, in0, s1, s2, op0, op1=None, R=(), W=()):
        def f(e):
            if op1 is None:
                return e.tensor_scalar(out=out, in0=in0, scalar1=s1, scalar2=None, op0=op0)
            return e.tensor_scalar(out=out, in0=in0, scalar1=s1, scalar2=s2, op0=op0, op1=op1)
        return self.op(eng, f, R, W)

    def tt(self, eng, out, in0, in1, op, R=(), W=()):
        return self.op(eng, lambda e: e.tensor_tensor(out=out, in0=in0, in1=in1, op=op), R, W)

    def stt(self, eng, out, in0, scalar, in1, op0, op1, R=(), W=()):
        return self.op(eng, lambda e: e.scalar_tensor_tensor(out=out, in0=in0, scalar=scalar, in1=in1, op0=op0, op1=op1), R, W)

    def cp(self, eng, out, in_, R=(), W=()):
        if eng == "act":
            return self.op("act", lambda e: e.copy(out=out, in_=in_), R, W)
        return self.op(eng, lambda e: e.tensor_copy(out=out, in_=in_), R, W)

    def rsqrt(self, ap, T):
        self.op("act", lambda e: e.activation(out=ap, in_=ap, func=AF.Sqrt), [T], [T])
        self.op("dve", lambda e: e.reciprocal(out=ap, in_=ap), [T], [T])

    def memset(self, eng, ap, val, W=()):
        return self.op(eng, lambda e: e.memset(ap, val), (), W)

    def barrier(self):
        for e in self.ENG:
            waits = []
            sn = self.seen[e]
            for f in self.ENG:
                if f != e and sn.get(f, 0) < self.cnt[f]:
                    sn[f] = self.cnt[f]
                    waits.append((f, self.cnt[f]))
            for k in range(self.NDMA):
                if self.dcnt[k] > 0 and sn.get(k, 0) < self.dcnt[k]:
                    sn[k] = self.dcnt[k]
                    waits.append((k, self.dcnt[k]))
            if waits:
                self.q[e].append((waits, None, None))

    def scope(self):
        prog = self

        class _S:
            def __enter__(s2):
                s2.outer = prog.es
                prog.es = ExitStack()
                return prog

            def __exit__(s2, *a):
                if a[0] is None:
                    prog.flush()
                prog.es.close()
                prog.es = s2.outer
                return False
        return _S()

    def flush(self):
        nc = self.nc
        self.barrier()
        with nc.Block() as block:
            def run(engname):
                def body(eng):
                    for waits, fn, inc in self.q[engname]:
                        for tl, v in waits:
                            eng.wait_ge(self._semof(tl), v)
                        if fn is not None:
                            fn(eng).then_inc(self._semof(inc[0]), inc[1])
                return body
            block.tensor(run("pe"))
            block.scalar(run("act"))
            block.vector(run("dve"))
            block.gpsimd(run("pool"))
            block.sync(run("sp"))
        self.q = {e: [] for e in self.ENG}

    def finish(self):
        self.es.close()


def interleave(gens):
    gens = list(gens)
    while gens:
        nxt = []
        for g in gens:
            try:
                next(g)
                nxt.append(g)
            except StopIteration:
                pass
        gens = nxt


def make_consts():
    cf = {}
    i = np.arange(128)
    cf["ones"] = np.ones((128, 128), np.float32)
    cf["ucf"] = (i[:, None] <= i[None, :]).astype(np.float32)
    cf["ucb"] = (i[:, None] >= i[None, :]).astype(np.float32)
    cf["msf"] = (i[None, :] > i[:, None]).astype(np.float32)
    cf["msb"] = (i[None, :] < i[:, None]).astype(np.float32)
    cf["mif"] = (i[None, :] >= i[:, None]).astype(np.float32)
    cf["mib"] = (i[None, :] <= i[:, None]).astype(np.float32)
    cf["iota"] = np.broadcast_to(i[None, :].astype(np.float32), (128, 128)).copy()
    cf["identf"] = np.eye(128, dtype=np.float32)
    misc = np.zeros((128, 128), np.float32)
    misc[:, 0] = i
    misc[:, 8:16] = np.arange(8)[None, :]
    cf["misc"] = misc
    inv = np.zeros((128, 4 * 128), np.float32)
    pm = np.zeros((4, 128, 128), np.float32)
    for g, w in enumerate((2, 4, 8, 16)):
        for t in range(128):
            seg = t // 64
            tl = t % 64
            lo = min(max(tl - w // 2, 0), 64)
            hi = min(max(tl + w - w // 2, 0), 64)
            cnt = hi - lo
            pm[g, seg * 64 + lo: seg * 64 + hi, t] += 1.0
            pm[g, t, t] -= cnt
            inv[:, g * 128 + t] = 1.0 / cnt
    cf["invcnt"] = inv
    lvl_names = []
    for lv in range(7):
        b = 1 << lv
        jj, ii = i[:, None], i[None, :]
        mf = ((jj // (2 * b)) == (ii // (2 * b))) & ((jj % (2 * b)) < b) & ((ii % (2 * b)) >= b)
        cf["nmf%d" % lv] = -(mf.astype(np.float32))
        cf["nmb%d" % lv] = -(mf.T.astype(np.float32))
        lvl_names += ["nmf%d" % lv, "nmb%d" % lv]
    names_f = ["ones", "ucf", "ucb", "msf", "msb", "mif", "mib", "iota", "identf", "misc"] + lvl_names
    arr_f = np.concatenate([cf[n] for n in names_f] + [inv], axis=1).astype(np.float32)
    off_f = {n: k * 128 for k, n in enumerate(names_f)}
    off_f["invcnt"] = len(names_f) * 128
    cb = [np.eye(128, dtype=np.float32), np.ones((128, 128), np.float32),
          (i[:, None] < i[None, :]).astype(np.float32)] + [pm[g] for g in range(4)]
    arr_b = np.concatenate(cb, axis=1).astype(ml_dtypes.bfloat16)
    off_b = {"ident": 0, "ones": 128, "lstrict": 256, "pm": 384}
    return arr_f, off_f, arr_b, off_b


def build(L, debug_outs=(), stop=None):
    assert L % 256 == 0
    NT = L // 128
    LT = CTX + L
    NCH = LT // 128
    CAP = 2 * L // NE
    NM = CAP // 128
    assert CAP % 128 == 0
    arr_f, OF, arr_b, OB = make_consts()
    nc = bass.Bass("TRN2", target_bir_lowering=False)

    def din(name, shape, dt=F32):
        return nc.dram_tensor(name, list(shape), dt, kind="ExternalInput").ap()

    def dscr(name, shape, dt=F32):
        kind = "ExternalOutput" if name in debug_outs else "Internal"
        return nc.dram_tensor(name, list(shape), dt, kind=kind).ap()

    I = dict(
        x=din("x", [L, D]), ctx=din("ctx", [CTX, D]), cvec=din("cvec", [128, 16]),
        ada_w=din("ada_w", [2, D, 6 * D]), ada_b=din("ada_b", [2, 6 * D]),
        norm_mix=din("norm_mix", [2, D]), norm_ffn=din("norm_ffn", [2, D]), norm_final=din("norm_final", [D]),
        ev_w_in=din("ev_w_in", [D, 2576]), dn_conv=din("dn_conv", [128, 36]),
        dn_a_log=din("dn_a_log", [8]), dn_dt_bias=din("dn_dt_bias", [8]), dn_norm=din("dn_norm", [128]),
        pool_w=din("pool_w", [4, 128, 128]), pool_scale=din("pool_scale", [128, 4]),
        ev_w_out=din("ev_w_out", [D, D]),
        sc_w_in=din("sc_w_in", [D, 3 * D]), sc_conv=din("sc_conv", [128, 24]), sc_w_out=din("sc_w_out", [D, D]),
        router=din("router", [2, D, NE]), w_gate=din("w_gate", [2, NE, D, 512]),
        w_up=din("w_up", [2, NE, D, 512]), w_down=din("w_down", [2, NE, 512, D]),
        cf=din("cf", list(arr_f.shape)), cb=din("cb", list(arr_b.shape), BF16),
    )
    out = nc.dram_tensor("out", [L, D], F32, kind="ExternalOutput").ap()
    S = dict(
        mod=dscr("mod", [2, 2, 6 * D]),
        hT=dscr("hT", [8, 128, LT], BF16),
        qT=dscr("qT", [4, 128, LT], BF16), kT=dscr("kT", [4, 128, LT], BF16),
        ktok=dscr("ktok", [LT, 512], BF16), vtok=dscr("vtok", [LT, 512], BF16),
        gates=dscr("gates", [LT, 16]), zs=dscr("zs", [L, 512], BF16),
        mixT=dscr("mixT", [8, 128, L], BF16),
        of=dscr("of", [L, 512]), ob=dscr("ob", [L, 512]),
        xl=dscr("xl", [L, D]), h2=dscr("h2", [L, D], BF16),
    )
    p = Prog(nc)
    cfT = p.sb([128, arr_f.shape[1]], F32, "cf")
    cbT = p.sb([128, arr_b.shape[1]], BF16, "cb")
    p.dma("sp", cfT[:], I["cf"], W=[cfT])
    p.dma("sp", cbT[:], I["cb"], W=[cbT])

    def CF(n, w=128):
        return cfT[:, OF[n]:OF[n] + w]

    def CB(n, w=128, o=0):
        return cbT[:, OB[n] + o:OB[n] + o + w]

    def colvec(dst, src1d, eng="sp"):
        p.dma(eng, dst[:], src1d.rearrange("(k p) -> p k", p=128), W=[dst], allow_slow_non_contiguous=True)

    def rowvec(dst, src1d, eng="sp"):
        p.dma(eng, dst[:], src1d.partition_broadcast(128), W=[dst])

    with p.scope():
        cv = p.sb([128, 16], F32)
        sc = p.sb([128, 16], F32)
        p.dma("sp", cv[:], I["cvec"], W=[cv])
        p.act(sc[:], cv[:], AF.Silu, R=[cv], W=[sc])
        awr = p.ring(2, [128, 8, 512], F32)
        psr = p.ring(2, [128, 512], F32, psum=True)
        for i in range(2):
            bias = p.sb([2, 6 * D], F32)
            modrow = p.sb([2, 6 * D], F32)
            for r in range(2):
                p.dma("act", bias[r:r + 1, :], I["ada_b"][i:i + 1, :], W=[bias])
            for n in range(12):
                a = awr.next()
                p.dma("sp", a[:], I["ada_w"][i][:, n * 512:(n + 1) * 512].rearrange("(k p) c -> p k c", p=128), W=[a])
                ps = psr.next()
                for k in range(8):
                    p.mm(ps[0:2, :], sc[:, k::8], a[:, k, :], start=(k == 0), stop=(k == 7), R=[sc, a], W=[ps])
                p.tt("dve", modrow[0:2, n * 512:(n + 1) * 512], ps[0:2, :], bias[0:2, n * 512:(n + 1) * 512], ALU.add,
                     R=[ps, bias], W=[modrow])
            p.dma("sp", S["mod"][i], modrow[0:2, :], R=[modrow])

    def modv(i, r, j):
        return S["mod"][i, r, j * D:(j + 1) * D]

    def phase_hT(srcs, i, normvec):
        with p.scope():
            xr = p.ring(3, [128, D], F32)
            junk = p.sb([128, D], BF16)
            ssr = p.ring(3, [128, 2], F32)
            xnr = p.ring(2, [128, D], BF16)
            pTr = p.ring(2, [128, D], BF16, psum=True)
            tmpr = p.ring(2, [128, 8, 128], F32)
            hsr = p.ring(2, [128, 8, 256], BF16)
            for (src, ntok, col0, r) in srcs:
                gcol = p.sb([128, 8], F32)
                shc = p.sb([128, 8], F32)
                scc = p.sb([128, 8], F32)
                A = p.sb([128, 8], F32)
                colvec(gcol, normvec)
                colvec(shc, modv(i, r, 0))
                colvec(scc, modv(i, r, 1))
                p.stt("dve", A[:], scc[:], 1.0, gcol[:], ALU.add, ALU.mult, R=[scc, gcol], W=[A])
                def hA(t0):
                    xt = xr.next()
                    p.dma("sp", xt[:], src[t0:t0 + 128, :], W=[xt])
                    ss = ssr.next()
                    p.memset("pool", ss[:], 0.0, W=[ss])
                    p.act(junk[:], xt[:], AF.Square, accum=ss[:, 0:1], R=[xt], W=[junk, ss])
                    p.ts("dve", ss[:, 1:2], ss[:, 0:1], 1.0 / D, EPS, ALU.mult, ALU.add, R=[ss], W=[ss])
                    p.rsqrt(ss[:, 1:2], ss)
                    xn = xnr.next()
                    p.act(xn[:], xt[:], AF.Copy, scale=ss[:, 1:2], R=[xt, ss], W=[xn])
                    return xn

                def hB(xn, hs, t2):
                    pT = pTr.next()
                    for k in range(8):
                        p.tr(pT[:, k * 128:(k + 1) * 128], xn[:, k * 128:(k + 1) * 128], CB("ident"), R=[xn, cbT], W=[pT])
                    tmp = tmpr.next()
                    p.tt("dve", tmp[:], pT[:].rearrange("p (k t) -> p k t", k=8),
                         A[:, :].unsqueeze(2).to_broadcast([128, 8, 128]), ALU.mult, R=[pT, A], W=[tmp])
                    p.tt("pool", hs[:, :, t2 * 128:(t2 + 1) * 128], tmp[:],
                         shc[:, :].unsqueeze(2).to_broadcast([128, 8, 128]), ALU.add, R=[tmp, shc], W=[hs])

                ntile = ntok // 128
                pend = None
                hs = None
                for j in range(ntile + 1):
                    xn = hA(j * 128) if j < ntile else None
                    if pend is not None:
                        jj, pxn = pend
                        if jj % 2 == 0:
                            hs = hsr.next()
                        hB(pxn, hs, jj % 2)
                        if jj % 2 == 1:
                            c0 = col0 + (jj // 2) * 256
                            p.dma("pool", S["hT"][:, :, c0:c0 + 256].rearrange("k p t -> p k t"), hs[:], R=[hs])
                    pend = (j, xn)

    def load_window(hw, col0, ntok, blk):
        a = col0 + blk * 256
        lo = a - 1 if blk > 0 else a
        hi = a + 257 if (blk + 1) * 256 < ntok else a + 256
        d0 = 0 if blk > 0 else 1
        if blk == 0:
            p.memset("pool", hw[:, :, 0:1], 0.0, W=[hw])
        if (blk + 1) * 256 >= ntok:
            p.memset("pool", hw[:, :, 257:258], 0.0, W=[hw])
        p.dma("sp", hw[:, :, d0:d0 + (hi - lo)], S["hT"][:, :, lo:hi].rearrange("k p t -> p k t"), W=[hw])

    def conv3(ps, wcol, c, acc, psT, accT):
        p.act(acc[:], ps[:, 1:257], AF.Copy, scale=wcol[:, c, 1:2], R=[psT, wcol], W=[accT])
        p.stt("dve", acc[:], ps[:, 0:256], wcol[:, c, 0:1], acc[:], ALU.mult, ALU.add, R=[psT, wcol, accT], W=[accT])
        p.stt("dve", acc[:], ps[:, 2:258], wcol[:, c, 2:3], acc[:], ALU.mult, ALU.add, R=[psT, wcol, accT], W=[accT])

    def phase_even_proj():
        with p.scope():
            w = p.sb([128, 8, 2576], BF16, "wev")
            for k in range(8):
                p.dma("pool", w[:, k, :], I["ev_w_in"][k * 128:(k + 1) * 128, :], W=[w])
            cw = p.sb([128, 12, 3], F32)
            p.dma("sp", cw[:], I["dn_conv"].rearrange("p (c t) -> p c t", t=3), W=[cw])
            pw = p.sb([128, 4, 128], BF16)
            p.dma("pool", pw[:], I["pool_w"].rearrange("g c d -> c g d"), W=[pw])
            pscale = p.sb([128, 4], F32)
            p.dma("sp", pscale[:], I["pool_scale"], W=[pscale])
            dtb = p.sb([128, 8], F32)
            nega = p.sb([128, 8], F32)
            rowvec(dtb, I["dn_dt_bias"])
            rowvec(nega, I["dn_a_log"])
            p.act(nega[:], nega[:], AF.Exp, R=[nega], W=[nega])
            p.ts("dve", nega[:], nega[:], -1.0, None, ALU.mult, R=[nega], W=[nega])
            hwr = p.ring(2, [128, 8, 258], BF16)
            psW = p.ring(3, [128, 512], F32, psum=True)
            psT = p.ring(2, [128, 1024], BF16, psum=True)
            psS = p.ring(2, [128, 512], F32, psum=True)
            accr = p.ring(3, [128, 256], F32)
            sr = p.ring(3, [128, 256], F32)
            sqr = p.ring(2, [128, 256], BF16)
            rinvr = p.ring(2, [128, 256], F32)
            qnr = p.ring(3, [128, 256], BF16)
            tokr = p.ring(3, [128, 2, 128], BF16)
            zr = p.ring(2, [128, 512], BF16)
            ur = p.ring(2, [128, 512], BF16)
            g4r = p.ring(2, [128, 512], BF16)
            m4r = p.ring(2, [128, 4, 128], BF16)
            gtr = p.ring(2, [128, 48], F32)
            for (col0, ntok, lat) in ((0, CTX, False), (CTX, L, True)):
                for blk in range(ntok // 256):
                    hw = hwr.next()
                    load_window(hw, col0, ntok, blk)
                    c0 = col0 + blk * 256
                    def stageA(c):
                        ps = psW.next()
                        for k in range(8):
                            p.mm(ps[:, 0:258], w[:, k, c * 128:(c + 1) * 128], hw[:, k, :], start=(k == 0), stop=(k == 7),
                                 R=[w, hw], W=[ps])
                        acc = accr.next()
                        conv3(ps, cw, c, acc, ps, acc)
                        st = dict(c=c)
                        if c < 8:
                            s_ = sr.next()
                            p.act(s_[:], acc[:], AF.Silu, R=[acc], W=[s_])
                            sq = sqr.next()
                            p.tt("pool", sq[:], s_[:], s_[:], ALU.mult, R=[s_], W=[sq])
                            st.update(s=s_, sq=sq)
                        else:
                            qn = qnr.next()
                            p.act(qn[:], acc[:], AF.Silu, R=[acc], W=[qn])
                            st.update(qn=qn)
                        return st

                    def stageB(st):
                        c = st["c"]
                        h = c % 4
                        if c < 8:
                            s_ = st["s"]; sq = st["sq"]
                            pss = psS.next()
                            p.mm(pss[:, 0:256], CB("ones"), sq[:], R=[sq, cbT], W=[pss])
                            rinv = rinvr.next()
                            p.ts("dve", rinv[:], pss[:, 0:256], EPS, None, ALU.add, R=[pss], W=[rinv])
                            p.rsqrt(rinv[:], rinv)
                            qn = qnr.next()
                            p.stt("dve", qn[:], s_[:], (128.0 ** -0.5) if c < 4 else 1.0, rinv[:], ALU.mult, ALU.mult,
                                  R=[s_, rinv], W=[qn])
                            dst = S["qT"] if c < 4 else S["kT"]
                            p.dma("pool", dst[h, :, c0:c0 + 256], qn[:], R=[qn])
                            src_tok = qn if c >= 4 else None
                            dtok = S["ktok"]
                        else:
                            src_tok = st["qn"]
                            dtok = S["vtok"]
                        if src_tok is not None:
                            pt = psT.next()
                            for t2 in range(2):
                                p.tr(pt[:, t2 * 128:(t2 + 1) * 128], src_tok[:, t2 * 128:(t2 + 1) * 128], CB("ident"),
                                     R=[src_tok, cbT], W=[pt])
                            tk = tokr.next()
                            p.cp("act", tk[:], pt[:, 0:256].rearrange("p (a b) -> p a b", a=2), R=[pt], W=[tk])
                            p.dma("pool", dtok[c0:c0 + 256, h * 128:(h + 1) * 128].rearrange("(a p) d -> p a d", p=128), tk[:], R=[tk])

                    pend = None
                    for c in range(12):
                        st = stageA(c)
                        if pend is not None:
                            stageB(pend)
                        pend = st
                    stageB(pend)
                    for t2 in range(2):
                        t0 = c0 + t2 * 128
                        lhs = lambda k: hw[:, k, 1 + t2 * 128:1 + (t2 + 1) * 128]
                        psg = psS.next()
                        for k in range(8):
                            p.mm(psg[:, 0:16], lhs(k), w[:, k, 2048:2064], start=(k == 0), stop=(k == 7), R=[w, hw], W=[psg])
                        if lat:
                            psz = psW.next()
                            for k in range(8):
                                p.mm(psz[:], lhs(k), w[:, k, 1536:2048], start=(k == 0), stop=(k == 7), R=[w, hw], W=[psz])
                            psp = psW.next()
                            for k in range(8):
                                p.mm(psp[:], lhs(k), w[:, k, 2064:2576], start=(k == 0), stop=(k == 7), R=[w, hw], W=[psp])
                        ps = psg
                        g = gtr.next()
                        p.act(g[:, 0:8], ps[:, 0:8], AF.Sigmoid, R=[ps], W=[g])
                        p.tt("dve", g[:, 16:24], ps[:, 8:16], dtb[:], ALU.add, R=[ps, dtb], W=[g])
                        p.ts("dve", g[:, 24:32], g[:, 16:24], 30.0, None, ALU.min, R=[g], W=[g])
                        p.act(g[:, 24:32], g[:, 24:32], AF.Exp, R=[g], W=[g])
                        p.act(g[:, 24:32], g[:, 24:32], AF.Ln, bias=1.0, R=[g], W=[g])
                        p.ts("dve", g[:, 32:40], g[:, 16:24], -30.0, 0.0, ALU.add, ALU.max, R=[g], W=[g])
                        p.tt("dve", g[:, 24:32], g[:, 24:32], g[:, 32:40], ALU.add, R=[g], W=[g])
                        p.tt("dve", g[:, 8:16], g[:, 24:32], nega[:], ALU.mult, R=[g, nega], W=[g])
                        p.dma("pool", S["gates"][t0:t0 + 128, :], g[:, 0:16], R=[g])
                        if not lat:
                            continue
                        tl = t0 - CTX
                        z = zr.next()
                        p.act(z[:], psz[:], AF.Silu, R=[psz], W=[z])
                        p.dma("pool", S["zs"][tl:tl + 128, :], z[:], R=[z])
                        u = ur.next()
                        p.cp("act", u[:], psp[:], R=[psp], W=[u])
                        ps1 = psS.next()
                        for gi in range(4):
                            p.mm(ps1[:, gi * 128:(gi + 1) * 128], u[:, gi * 128:(gi + 1) * 128], CB("pm", 128, gi * 128), R=[u, cbT], W=[ps1])
                        gT4 = g4r.next()
                        p.tt("dve", gT4[:], ps1[:], cfT[:, OF["invcnt"]:OF["invcnt"] + 512], ALU.mult, R=[ps1, cfT], W=[gT4])
                        ps2 = psS.next()
                        for gi in range(4):
                            p.mm(ps2[:, gi * 128:(gi + 1) * 128], pw[:, gi, :], gT4[:, gi * 128:(gi + 1) * 128], R=[pw, gT4], W=[ps2])
                        mo4 = m4r.next()
                        p.tt("dve", mo4[:], ps2[:].rearrange("p (g t) -> p g t", g=4),
                             pscale[:, :].unsqueeze(2).to_broadcast([128, 4, 128]), ALU.mult, R=[ps2, pscale], W=[mo4])
                        p.dma("pool", S["mixT"][4:8, :, tl:tl + 128].rearrange("k p t -> p k t"), mo4[:], R=[mo4])

    def phase_delta():
        with p.scope():
            psPd = {0: p.ring(2, [128, 4, 128], F32, psum=True, name="psP0"),
                    1: p.ring(2, [128, 4, 128], F32, psum=True, name="psP1")}
            psSc = {0: p.ring(2, [128, 4, 128], F32, psum=True, name="psS0"),
                    1: p.ring(2, [128, 4, 128], F32, psum=True, name="psS1")}
            NS = 3
            names_bf = ["qT", "kT", "ktok", "vtok", "P", "qkd", "qd", "kd", "B", "BT"]
            ins = {}
            for d in range(2):
                ins[d] = dict(
                    g=p.ring(NS, [128, 16], F32), sm=p.ring(NS, [128, 32], F32),
                    X=p.ring(2, [128, 4, 128], F32), E=p.ring(2, [128, 4, 128], F32),
                    Es=p.ring(2, [128, 4, 128], F32), Ei=p.ring(2, [128, 4, 128], F32),
                    lu=p.ring(2, [128, 4, 128], F32), eg=p.ring(2, [128, 4, 128], F32),
                    nR=p.ring(2, [128, 4, 128], BF16), w=p.ring(2, [128, 4, 128], BF16),
                    osb=p.ring(2, [128, 4, 128], F32),
                    S32=p.sb([128, 4, 128], F32), Sbf=p.ring(2, [128, 4, 128], BF16),
                )
                ins[d]["B32"] = p.ring(2, [128, 4, 128], F32)
                ins[d]["GT"] = p.ring(2, [128, 4, 128], BF16)
                ins[d]["Xb"] = p.ring(2, [128, 4, 128], BF16)
                ins[d]["tmp"] = p.ring(4, [128, 4, 128], BF16)
                for nm in names_bf:
                    ins[d][nm] = p.ring(NS if nm not in ("B", "BT", "B2", "B2T") else 2, [128, 4, 128], BF16)
            order = {0: list(range(NCH)), 1: [1, 0] + list(range(NCH - 1, 1, -1))}
            prepped = {0: {}, 1: {}}

            def prep(d, it):
                R_ = ins[d]
                psP = psPd[d]
                c = order[d][it]
                t0 = c * 128
                ucum = CF("ucf") if d == 0 else CF("ucb")
                ms = CF("msf") if d == 0 else CF("msb")
                mi = CF("mif") if d == 0 else CF("mib")
                lastcol = 127 if d == 0 else 0
                qT = R_["qT"].next(); kT = R_["kT"].next(); ktok = R_["ktok"].next(); vtok = R_["vtok"].next()
                g = R_["g"].next(); sm = R_["sm"].next()
                p.dma("act", qT[:], S["qT"][:, :, t0:t0 + 128].rearrange("h p t -> p h t"), W=[qT])
                p.dma("act", kT[:], S["kT"][:, :, t0:t0 + 128].rearrange("h p t -> p h t"), W=[kT])
                p.dma("act", ktok[:], S["ktok"][t0:t0 + 128, :].rearrange("p (h d) -> p h d", h=4), W=[ktok])
                p.dma("act", vtok[:], S["vtok"][t0:t0 + 128, :].rearrange("p (h d) -> p h d", h=4), W=[vtok])
                p.dma("act", g[:], S["gates"][t0:t0 + 128, :], W=[g])
                ld = g[:, 8 + 4 * d:12 + 4 * d]
                beta = g[:, 4 * d:4 * d + 4]
                yield
                ps = psP.next()
                p.mm(ps[:, 0, 0:4], ucum, ld, R=[cfT, g], W=[ps])
                p.cp("dve", sm[:, 0:4], ps[:, 0, 0:4], R=[ps], W=[sm])
                lu = R_["lu"].next()
                p.tt("dve", lu[:], ucum.unsqueeze(1).to_broadcast([128, 4, 128]),
                     ld.unsqueeze(2).to_broadcast([128, 4, 128]), ALU.mult, R=[cfT, g], W=[lu])
                pg = psP.next()
                for h in range(4):
                    p.mm(pg[:, h, :], CF("ones"), lu[:, h, :], R=[cfT, lu], W=[pg])
                yield
                p.cp("dve", sm[:, 4:8], pg[:, :, lastcol], R=[pg], W=[sm])
                X = R_["X"].next()
                p.tt("dve", X[:], pg[:], sm[:, 0:4].unsqueeze(2).to_broadcast([128, 4, 128]), ALU.subtract, R=[pg, sm], W=[X])
                E = R_["E"].next()
                p.act(X[:], X[:], AF.Relu, scale=-1.0, R=[X], W=[X])
                p.act(E[:], X[:], AF.Exp, scale=-1.0, R=[X], W=[E])
                eg = R_["eg"].next()
                p.act(eg[:], pg[:], AF.Exp, R=[pg], W=[eg])
                p.tt("dve", sm[:, 8:12], sm[:, 4:8], sm[:, 0:4], ALU.subtract, R=[sm], W=[sm])
                p.act(sm[:, 8:12], sm[:, 8:12], AF.Exp, R=[sm], W=[sm])
                p.act(sm[:, 12:16], sm[:, 0:4], AF.Exp, R=[sm], W=[sm])
                p.ts("dve", sm[:, 16:20], beta, -1.0, None, ALU.mult, R=[g], W=[sm])
                p.act(sm[:, 20:24], sm[:, 4:8], AF.Exp, R=[sm], W=[sm])
                yield
                Es = R_["Es"].next(); Ei = R_["Ei"].next()
                p.tt("pool", Es[:], E[:], ms.unsqueeze(1).to_broadcast([128, 4, 128]), ALU.mult, R=[E, cfT], W=[Es])
                p.tt("pool", Ei[:], E[:], mi.unsqueeze(1).to_broadcast([128, 4, 128]), ALU.mult, R=[E, cfT], W=[Ei])
                qd = R_["qd"].next()
                p.tt("dve", qd[:], qT[:], eg[:], ALU.mult, R=[qT, eg], W=[qd])
                kd = R_["kd"].next()
                for h in range(4):
                    p.act(kd[:, h, :], ktok[:, h, :], AF.Copy, scale=sm[:, 8 + h:9 + h], R=[ktok, sm], W=[kd])
                pk = psP.next()
                for h in range(4):
                    p.mm(pk[:, h, :], kT[:, h, :], kT[:, h, :], R=[kT], W=[pk])
                pq = psP.next()
                for h in range(4):
                    p.mm(pq[:, h, :], kT[:, h, :], qT[:, h, :], R=[kT, qT], W=[pq])
                yield
                B = R_["B"].next(); BT = R_["BT"].next(); B32 = R_["B32"].next()
                for h in range(4):
                    p.stt("dve", B32[:, h, :], pk[:, h, :], beta[:, h:h + 1], Es[:, h, :], ALU.mult, ALU.mult, R=[pk, Es, g], W=[B32])
                p.cp("act", B[:], B32[:], R=[B32], W=[B])
                qkd = R_["qkd"].next()
                p.tt("dve", qkd[:], pq[:], Ei[:], ALU.mult, R=[pq, Ei], W=[qkd])
                pb = psP.next()
                for h in range(4):
                    p.tr(pb[:, h, :], B32[:, h, :], CF("identf"), R=[B32, cfT], W=[pb])
                yield
                p.cp("act", BT[:], pb[:], R=[pb], W=[BT])
                nmn = "nmf%d" if d == 0 else "nmb%d"
                nmt = "nmb%d" if d == 0 else "nmf%d"
                bc4 = lambda ap: ap.unsqueeze(1).to_broadcast([128, 4, 128])
                G = R_["P"].next(); GT = R_["GT"].next()
                tmp = R_["tmp"].next(); tmpT = R_["tmp"].next()
                p.tt("dve", tmp[:], B[:], bc4(CF(nmn % 0)), ALU.mult, R=[B, cfT], W=[tmp])
                p.tt("dve", G[:], tmp[:], bc4(CF("identf")), ALU.add, R=[tmp, cfT], W=[G])
                p.tt("pool", tmpT[:], BT[:], bc4(CF(nmt % 0)), ALU.mult, R=[BT, cfT], W=[tmpT])
                p.tt("pool", GT[:], tmpT[:], bc4(CF("identf")), ALU.add, R=[tmpT, cfT], W=[GT])
                yield
                for lev in range(1, 7):
                    last = (lev == 6)
                    px = psP.next()
                    for h in range(4):
                        p.mm(px[:, h, :], BT[:, h, :], G[:, h, :], R=[BT, G], W=[px])
                    yield
                    Xb = R_["Xb"].next()
                    p.cp("act", Xb[:], px[:], R=[px], W=[Xb])
                    py = psP.next()
                    for h in range(4):
                        p.mm(py[:, h, :], GT[:, h, :], Xb[:, h, :], R=[GT, Xb], W=[py])
                    if not last:
                        pyt = psP.next()
                        for h in range(4):
                            p.mm(pyt[:, h, :], Xb[:, h, :], GT[:, h, :], R=[GT, Xb], W=[pyt])
                    yield
                    tmp = R_["tmp"].next()
                    p.tt("dve", tmp[:], py[:], bc4(CF(nmn % lev)), ALU.mult, R=[py, cfT], W=[tmp])
                    p.tt("dve", G[:], G[:], tmp[:], ALU.add, R=[G, tmp], W=[G])
                    if not last:
                        tmpT = R_["tmp"].next()
                        p.tt("dve", tmpT[:], pyt[:], bc4(CF(nmt % lev)), ALU.mult, R=[pyt, cfT], W=[tmpT])
                        p.tt("pool", GT[:], GT[:], tmpT[:], ALU.add, R=[GT, tmpT], W=[GT])
                    yield
                P = G
                prepped[d][it] = dict(c=c, kT=kT, vtok=vtok, sm=sm, P=P, qkd=qkd, qd=qd, kd=kd)

            state = {}

            def scan(d, it):
                R_ = ins[d]
                pr = prepped[d].pop(it)
                c = pr["c"]; sm = pr["sm"]
                S32 = R_["S32"]
                Sbf = state[d]
                pks = psSc[d].next()
                for h in range(4):
                    p.mm(pks[:, h, :], pr["kT"][:, h, :], Sbf[:, h, :], R=[pr["kT"], Sbf], W=[pks])
                yield
                nR = R_["nR"].next()
                for h in range(4):
                    p.stt("dve", nR[:, h, :], pks[:, h, :], sm[:, 12 + h:13 + h], pr["vtok"][:, h, :], ALU.mult, ALU.subtract,
                          R=[pks, sm, pr["vtok"]], W=[nR])
                pw_ = psSc[d].next()
                for h in range(4):
                    p.mm(pw_[:, h, :], pr["P"][:, h, :], nR[:, h, :], R=[pr["P"], nR], W=[pw_])
                yield
                w = R_["w"].next()
                for h in range(4):
                    p.act(w[:, h, :], pw_[:, h, :], AF.Copy, scale=sm[:, 16 + h:17 + h], R=[pw_, sm], W=[w])
                if c >= 2:
                    po = psSc[d].next()
                    for h in range(4):
                        p.mm(po[:, h, :], pr["qd"][:, h, :], Sbf[:, h, :], start=True, stop=False, R=[pr["qd"], Sbf], W=[po])
                        p.mm(po[:, h, :], pr["qkd"][:, h, :], w[:, h, :], start=False, stop=True, R=[pr["qkd"], w], W=[po])
                pds = psSc[d].next()
                for h in range(4):
                    p.mm(pds[:, h, :], pr["kd"][:, h, :], w[:, h, :], R=[pr["kd"], w], W=[pds])
                yield
                for h in range(4):
                    p.stt("dve", S32[:, h, :], S32[:, h, :], sm[:, 20 + h:21 + h], pds[:, h, :], ALU.mult, ALU.add,
                          R=[S32, sm, pds], W=[S32])
                nS = R_["Sbf"].next()
                p.cp("act", nS[:], S32[:], R=[S32], W=[nS])
                state[d] = nS
                if c >= 2:
                    osb = R_["osb"].next()
                    p.cp("act", osb[:], po[:], R=[po], W=[osb])
                    tl = (c - 2) * 128
                    dst = S["of"] if d == 0 else S["ob"]
                    p.dma("sp", dst[tl:tl + 128, :].rearrange("p (h d) -> p h d", h=4), osb[:], R=[osb])
                yield

            for d in range(2):
                p.memset("pool", ins[d]["S32"][:], 0.0, W=[ins[d]["S32"]])
                s0 = ins[d]["Sbf"].next()
                p.memset("pool", s0[:], 0.0, W=[s0])
                state[d] = s0
            interleave([prep(0, 0), prep(1, 0)])
            import os
            nit = int(os.environ.get("DELTA_ITERS", NCH))
            for it in range(nit):
                gens = [scan(0, it), scan(1, it)]
                if it + 1 < NCH:
                    gens += [prep(0, it + 1), prep(1, it + 1)]
                interleave(gens)

    def phase_even_out():
        with p.scope():
            wo = p.sb([128, 8, D], BF16, "wo")
            for k in range(8):
                p.dma("pool", wo[:, k, :], I["ev_w_out"][k * 128:(k + 1) * 128, :], W=[wo])
            onorm = p.sb([128, 128], F32)
            rowvec(onorm, I["dn_norm"])
            g1 = p.sb([128, D], F32)
            rowvec(g1, modv(0, 0, 2))
            ofr = p.ring(2, [128, 4, 128], F32); obr = p.ring(2, [128, 4, 128], F32)
            zr = p.ring(2, [128, 4, 128], BF16); xr = p.ring(3, [128, D], F32)
            plr = p.ring(3, [128, 4, 128], BF16)
            junk = p.sb([128, 128], F32)
            ssr = p.ring(2, [128, 8], F32)
            omr = p.ring(3, [128, 4, 128], BF16)
            mxr = p.ring(2, [128, 4, 128], BF16)
            psB = p.ring(2, [128, 8, 128], BF16, psum=True)
            psY = p.ring(4, [128, 512], F32, psum=True)
            x1r = p.ring(2, [128, D], F32)
            def eA(j):
                t0 = j * 128
                of = ofr.next(); ob = obr.next(); z = zr.next(); xt = xr.next(); pl = plr.next()
                p.dma("sp", of[:], S["of"][t0:t0 + 128, :].rearrange("p (h d) -> p h d", h=4), W=[of])
                p.dma("sp", ob[:], S["ob"][t0:t0 + 128, :].rearrange("p (h d) -> p h d", h=4), W=[ob])
                p.dma("sp", z[:], S["zs"][t0:t0 + 128, :].rearrange("p (h d) -> p h d", h=4), W=[z])
                p.dma("sp", xt[:], I["x"][t0:t0 + 128, :], W=[xt])
                p.dma("sp", pl[:], S["mixT"][4:8, :, t0:t0 + 128].rearrange("k p t -> p k t"), W=[pl])
                p.tt("pool", of[:], of[:], ob[:], ALU.add, R=[of, ob], W=[of])
                ss = ssr.next()
                p.memset("pool", ss[:], 0.0, W=[ss])
                for h in range(4):
                    p.act(junk[:], of[:, h, :], AF.Square, accum=ss[:, h:h + 1], R=[of], W=[junk, ss])
                p.ts("dve", ss[:, 4:8], ss[:, 0:4], 1.0 / 128, EPS, ALU.mult, ALU.add, R=[ss], W=[ss])
                p.rsqrt(ss[:, 4:8], ss)
                p.tt("dve", of[:], of[:], ss[:, 4:8].unsqueeze(2).to_broadcast([128, 4, 128]), ALU.mult, R=[of, ss], W=[of])
                p.tt("pool", of[:], of[:], onorm[:, :].unsqueeze(1).to_broadcast([128, 4, 128]), ALU.mult, R=[of, onorm], W=[of])
                om = omr.next()
                p.tt("dve", om[:], of[:], z[:], ALU.mult, R=[of, z], W=[om])
                return (t0, om, pl, xt)

            def eB(st):
                t0, om, pl, xt = st
                pb = psB.next()
                for h in range(4):
                    p.tr(pb[:, h, :], om[:, h, :], CB("ident"), R=[om, cbT], W=[pb])
                mx = mxr.next()
                p.cp("act", mx[:], pb[:, 0:4, :], R=[pb], W=[mx])
                x1 = x1r.next()
                for nh in range(2):
                    py = psY.next()
                    for k in range(8):
                        lhs = mx[:, k, :] if k < 4 else pl[:, k - 4, :]
                        p.mm(py[:], lhs, wo[:, k, nh * 512:(nh + 1) * 512], start=(k == 0), stop=(k == 7), R=[mx, pl, wo], W=[py])
                    p.tt("dve", x1[:, nh * 512:(nh + 1) * 512], py[:], g1[:, nh * 512:(nh + 1) * 512], ALU.mult, R=[py, g1], W=[x1])
                p.tt("pool", x1[:], x1[:], xt[:], ALU.add, R=[x1, xt], W=[x1])
                p.dma("pool", S["xl"][t0:t0 + 128, :], x1[:], R=[x1])

            pend = None
            for j in range(NT):
                st = eA(j)
                if pend is not None:
                    eB(pend)
                pend = st
            eB(pend)

    def phase_odd():
        with p.scope():
            w = p.sb([128, 8, 3 * D], BF16, "wsc")
            for k in range(8):
                p.dma("pool", w[:, k, :], I["sc_w_in"][k * 128:(k + 1) * 128, :], W=[w])
            wo = p.sb([128, 8, D], BF16, "wsco")
            for k in range(8):
                p.dma("pool", wo[:, k, :], I["sc_w_out"][k * 128:(k + 1) * 128, :], W=[wo])
            cw = p.sb([128, 8, 3], F32)
            p.dma("sp", cw[:], I["sc_conv"].rearrange("p (c t) -> p c t", t=3), W=[cw])
            g1 = p.sb([128, D], F32)
            rowvec(g1, modv(1, 0, 2))
            hwr = p.ring(2, [128, 8, 258], BF16)
            psW = p.ring(5, [128, 512], F32, psum=True)
            psY = p.ring(3, [128, 512], F32, psum=True)
            gcr = p.ring(2, [128, 258], F32)
            ur = p.ring(2, [128, 258], F32)
            accr = p.ring(2, [128, 256], F32)
            mixr = p.ring(2, [128, 8, 256], BF16)
            xr = p.ring(2, [128, D], F32)
            x1r = p.ring(2, [128, D], F32)
            for blk in range(L // 256):
                hw = hwr.next()
                load_window(hw, CTX, L, blk)
                mix = mixr.next()
                for c in range(8):
                    pss = []
                    for part in range(3):
                        ps = psW.next()
                        col = part * D + c * 128
                        for k in range(8):
                            p.mm(ps[:, 0:258], w[:, k, col:col + 128], hw[:, k, :], start=(k == 0), stop=(k == 7), R=[w, hw], W=[ps])
                        pss.append(ps)
                    gc = gcr.next()
                    p.cp("act", gc[:], pss[1][:, 0:258], R=[pss[1]], W=[gc])
                    u = ur.next()
                    p.tt("dve", u[:], gc[:], pss[2][:, 0:258], ALU.mult, R=[gc, pss[2]], W=[u])
                    acc = accr.next()
                    p.ts("pool", acc[:], u[:, 1:257], cw[:, c, 1:2], None, ALU.mult, R=[u, cw], W=[acc])
                    p.stt("dve", acc[:], u[:, 0:256], cw[:, c, 0:1], acc[:], ALU.mult, ALU.add, R=[u, cw, acc], W=[acc])
                    p.stt("dve", acc[:], u[:, 2:258], cw[:, c, 2:3], acc[:], ALU.mult, ALU.add, R=[u, cw, acc], W=[acc])
                    p.tt("dve", mix[:, c, :], acc[:], pss[0][:, 1:257], ALU.mult, R=[acc, pss[0]], W=[mix])
                for t2 in range(2):
                    t0 = blk * 256 + t2 * 128
                    xt = xr.next()
                    p.dma("act", xt[:], S["xl"][t0:t0 + 128, :], W=[xt])
                    x1 = x1r.next()
                    for nh in range(2):
                        py = psY.next()
                        for k in range(8):
                            p.mm(py[:], mix[:, k, t2 * 128:(t2 + 1) * 128], wo[:, k, nh * 512:(nh + 1) * 512],
                                 start=(k == 0), stop=(k == 7), R=[mix, wo], W=[py])
                        p.tt("dve", x1[:, nh * 512:(nh + 1) * 512], py[:], g1[:, nh * 512:(nh + 1) * 512], ALU.mult, R=[py, g1], W=[x1])
                    p.tt("pool", x1[:], x1[:], xt[:], ALU.add, R=[x1, xt], W=[x1])
                    p.dma("pool", S["xl"][t0:t0 + 128, :], x1[:], R=[x1, xt])

    def phase_moe(i):
        with p.scope():
            affall = p.sb([128, NT, NE], F32, "affall")
            idx = p.sb([128, NE, NM], I32, "idx")
            gate = p.sb([128, NE, NM], F32, "gate")
            g2 = p.sb([128, D], F32, "g2")
            rowvec(g2, modv(i, 0, 5))
            with p.scope():
                Ar = p.sb([128, D], F32); Br = p.sb([128, D], F32); gr_ = p.sb([128, D], F32)
                rowvec(gr_, I["norm_ffn"][i])
                rowvec(Br, modv(i, 0, 3))
                rowvec(Ar, modv(i, 0, 4))
                p.stt("dve", Ar[:], Ar[:], 1.0, gr_[:], ALU.add, ALU.mult, R=[Ar, gr_], W=[Ar])
                r32 = p.sb([128, 8, NE], F32); rhi = p.sb([128, 8, NE], BF16); rlo = p.sb([128, 8, NE], BF16)
                p.dma("sp", r32[:], I["router"][i].rearrange("(k p) e -> p k e", p=128), W=[r32])
                p.cp("dve", rhi[:], r32[:], R=[r32], W=[rhi])
                p.tt("dve", r32[:], r32[:], rhi[:], ALU.subtract, R=[r32, rhi], W=[r32])
                p.cp("dve", rlo[:], r32[:], R=[r32], W=[rlo])
                xr = p.ring(2, [128, D], F32); junk = p.sb([128, D], BF16)
                ssr = p.ring(2, [128, 8], F32)
                hr = p.ring(2, [128, D], F32); hir = p.ring(2, [128, D], BF16); lor = p.ring(2, [128, D], BF16)
                psT = p.ring(4, [128, D], BF16, psum=True)
                hiTr = p.ring(2, [128, D], BF16); loTr = p.ring(2, [128, D], BF16)
                psL = p.ring(2, [128, 512], F32, psum=True)
                er = p.ring(2, [128, NE], F32)
                def mA(j):
                    t0 = j * 128
                    xt = xr.next()
                    p.dma("sp", xt[:], S["xl"][t0:t0 + 128, :], W=[xt])
                    ss = ssr.next()
                    p.memset("pool", ss[:], 0.0, W=[ss])
                    p.act(junk[:], xt[:], AF.Square, accum=ss[:, 0:1], R=[xt], W=[junk, ss])
                    p.ts("dve", ss[:, 1:2], ss[:, 0:1], 1.0 / D, EPS, ALU.mult, ALU.add, R=[ss], W=[ss])
                    p.rsqrt(ss[:, 1:2], ss)
                    h = hr.next()
                    p.stt("dve", h[:], xt[:], ss[:, 1:2], Ar[:], ALU.mult, ALU.mult, R=[xt, ss, Ar], W=[h])
                    p.tt("dve", h[:], h[:], Br[:], ALU.add, R=[h, Br], W=[h])
                    hi = hir.next(); lo = lor.next()
                    p.cp("act", hi[:], h[:], R=[h], W=[hi])
                    p.dma("pool", S["h2"][t0:t0 + 128, :], hi[:], R=[hi])
                    p.tt("dve", lo[:], h[:], hi[:], ALU.subtract, R=[h, hi], W=[lo])
                    return (j, ss, hi, lo)

                def mB(st):
                    j, ss, hi, lo = st
                    pTh = psT.next(); pTl = psT.next()
                    for k in range(8):
                        p.tr(pTh[:, k * 128:(k + 1) * 128], hi[:, k * 128:(k + 1) * 128], CB("ident"), R=[hi, cbT], W=[pTh])
                    for k in range(8):
                        p.tr(pTl[:, k * 128:(k + 1) * 128], lo[:, k * 128:(k + 1) * 128], CB("ident"), R=[lo, cbT], W=[pTl])
                    hiT = hiTr.next(); loT = loTr.next()
                    p.cp("act", hiT[:], pTh[:], R=[pTh], W=[hiT])
                    p.cp("dve", loT[:], pTl[:], R=[pTl], W=[loT])
                    pl = psL.next()
                    n = 0
                    for (a, b) in ((hiT, rhi), (loT, rhi), (hiT, rlo)):
                        for k in range(8):
                            p.mm(pl[:, 0:NE], a[:, k * 128:(k + 1) * 128], b[:, k, :], start=(n == 0), stop=(n == 23), R=[a, b], W=[pl])
                            n += 1
                    p.op("dve", lambda e, o=ss[:, 2:3], a=pl[:, 0:NE]: e.tensor_reduce(out=o, in_=a, axis=AX.X, op=ALU.max), R=[pl], W=[ss])
                    p.ts("dve", ss[:, 3:4], ss[:, 2:3], -1.0, None, ALU.mult, R=[ss], W=[ss])
                    ex = er.next()
                    p.act(ex[:], pl[:, 0:NE], AF.Exp, bias=ss[:, 3:4], accum=ss[:, 4:5], R=[pl, ss], W=[ex, ss])
                    p.op("dve", lambda e, o=ss[:, 5:6], a=ss[:, 4:5]: e.reciprocal(out=o, in_=a), R=[ss], W=[ss])
                    p.ts("dve", affall[:, j, :], ex[:], ss[:, 5:6], None, ALU.mult, R=[ex, ss], W=[affall])

                pend = None
                for j in range(NT):
                    st = mA(j)
                    if pend is not None:
                        mB(pend)
                    pend = st
                mB(pend)
            with p.scope():
                NB_ = NT * NE
                lo = p.sb([128, NE], F32); hi = p.sb([128, NE], F32); mid = p.sb([128, NE], F32)
                sel = p.sb([128, NE], F32); d1 = p.sb([128, NE], F32)
                cmp_ = p.sb([128, NT, NE], BF16); cntp = p.sb([128, NE], F32)
                psc = p.ring(2, [128, 512], F32, psum=True)
                p.memset("dve", lo[:], 0.0, W=[lo])
                p.memset("dve", hi[:], 2.0, W=[hi])
                for itn in range(32):
                    p.tt("dve", mid[:], lo[:], hi[:], ALU.add, R=[lo, hi], W=[mid])
                    p.ts("dve", mid[:], mid[:], 0.5, None, ALU.mult, R=[mid], W=[mid])
                    p.tt("dve", cmp_[:], affall[:], mid[:, :].unsqueeze(1).to_broadcast([128, NT, NE]), ALU.is_ge, R=[affall, mid], W=[cmp_])
                    p.op("dve", lambda e: e.tensor_reduce(out=cntp[:], in_=cmp_[:].rearrange("p j e -> p e j"), axis=AX.X, op=ALU.add),
                         R=[cmp_], W=[cntp])
                    pc = psc.next()
                    p.mm(pc[:, 0:NE], CF("ones"), cntp[:], R=[cfT, cntp], W=[pc])
                    p.ts("dve", sel[:], pc[:, 0:NE], float(CAP) - 0.5, None, ALU.is_ge, R=[pc], W=[sel])
                    p.tt("dve", d1[:], mid[:], lo[:], ALU.subtract, R=[mid, lo], W=[d1])
                    p.tt("dve", d1[:], d1[:], sel[:], ALU.mult, R=[d1, sel], W=[d1])
                    p.tt("dve", lo[:], lo[:], d1[:], ALU.add, R=[lo, d1], W=[lo])
                    p.tt("dve", d1[:], hi[:], mid[:], ALU.subtract, R=[hi, mid], W=[d1])
                    p.tt("dve", d1[:], d1[:], sel[:], ALU.mult, R=[d1, sel], W=[d1])
                    p.tt("dve", hi[:], mid[:], d1[:], ALU.add, R=[mid, d1], W=[hi])
                selm = p.sb([128, NT, NE], F32); ca = p.sb([128, NT, NE], F32); cb_ = p.sb([128, NT, NE], F32)
                p.tt("dve", selm[:], affall[:], lo[:, :].unsqueeze(1).to_broadcast([128, NT, NE]), ALU.is_ge, R=[affall, lo], W=[selm])
                p.cp("dve", ca[:], selm[:], R=[selm], W=[ca])
                cur, oth = ca, cb_
                s = 1
                while s < NT:
                    p.cp("pool", oth[:, 0:s, :], cur[:, 0:s, :], R=[cur], W=[oth])
                    p.tt("dve", oth[:, s:, :], cur[:, s:, :], cur[:, 0:NT - s, :], ALU.add, R=[cur], W=[oth])
                    cur, oth = oth, cur
                    s *= 2
                totb = p.sb([128, NE], BF16)
                p.cp("dve", totb[:], cur[:, NT - 1, :], R=[cur], W=[totb])
                pp_ = psc.next()
                p.mm(pp_[:, 0:NE], CB("lstrict"), totb[:], R=[cbT, totb], W=[pp_])
                pref = p.sb([128, NE], F32)
                p.cp("dve", pref[:], pp_[:, 0:NE], R=[pp_], W=[pref])
                pos = oth
                p.tt("dve", pos[:], cur[:], selm[:], ALU.subtract, R=[cur, selm], W=[pos])
                p.tt("dve", pos[:], pos[:], pref[:, :].unsqueeze(1).to_broadcast([128, NT, NE]), ALU.add, R=[pos, pref], W=[pos])
                posi = p.sb([128, NT, NE], I32); pdi = p.sb([128, NT, NE], I32); pmi = p.sb([128, NT, NE], I32)
                pdf = p.sb([128, NT, NE], F32); pmf = p.sb([128, NT, NE], F32)
                p.cp("dve", posi[:], pos[:], R=[pos], W=[posi])
                p.op("dve", lambda e: e.tensor_single_scalar(out=pdi[:], in_=posi[:], scalar=7, op=ALU.arith_shift_right), R=[posi], W=[pdi])
                p.op("dve", lambda e: e.tensor_single_scalar(out=pmi[:], in_=posi[:], scalar=127, op=ALU.bitwise_and), R=[posi], W=[pmi])
                p.cp("dve", pdf[:], pdi[:], R=[pdi], W=[pdf])
                p.cp("dve", pmf[:], pmi[:], R=[pmi], W=[pmf])
                p.stt("dve", pmf[:], pmf[:], 1.0, selm[:], ALU.add, ALU.mult, R=[pmf, selm], W=[pmf])
                p.ts("dve", pmf[:], pmf[:], -1.0, None, ALU.add, R=[pmf], W=[pmf])
                val4 = p.sb([128, NT, NE, 4], F32)
                ahi = p.sb([128, NT, NE], BF16)
                p.cp("pool", val4[:, :, :, 0], cfT[:, OF["misc"]:OF["misc"] + 1].unsqueeze(2).to_broadcast([128, NT, NE]), R=[cfT], W=[val4])
                jrow = cfT[:, OF["iota"]:OF["iota"] + NT]
                p.cp("pool", val4[:, :, :, 1], jrow.unsqueeze(2).to_broadcast([128, NT, NE]), R=[cfT], W=[val4])
                p.cp("dve", ahi[:], affall[:], R=[affall], W=[ahi])
                p.cp("dve", val4[:, :, :, 2], ahi[:], R=[ahi], W=[val4])
                p.tt("dve", val4[:, :, :, 3], affall[:], ahi[:], ALU.subtract, R=[affall, ahi], W=[val4])
                iota8 = cfT[:, OF["misc"] + 8:OF["misc"] + 16]
                JC = 8
                Mh = p.sb([128, JC, NE, 8], F32)
                Rr = p.ring(2, [128, NT, 128], BF16)
                pacc = psc.next()
                rhs_all = [p.sb([128, JC, NE, 8, 4], BF16) for _ in range((NT + JC - 1) // JC)]
                for jc in range(0, NT, JC):
                    n = min(JC, NT - jc)
                    p.tt("dve", Mh[:, 0:n], pdf[:, jc:jc + n, :].unsqueeze(3).to_broadcast([128, n, NE, 8]),
                         iota8.unsqueeze(1).unsqueeze(1).to_broadcast([128, n, NE, 8]), ALU.is_equal, R=[pdf, cfT], W=[Mh])
                    ra = rhs_all[jc // JC]
                    p.tt("pool", ra[:, 0:n], Mh[:, 0:n].unsqueeze(4).to_broadcast([128, n, NE, 8, 4]),
                         val4[:, jc:jc + n].unsqueeze(3).to_broadcast([128, n, NE, 8, 4]), ALU.mult, R=[Mh, val4], W=[ra])
                for e_ in range(NE):
                    Rm = Rr.next()
                    p.tt("dve", Rm[:], CF("iota").unsqueeze(1).to_broadcast([128, NT, 128]),
                         pmf[:, :, e_:e_ + 1].to_broadcast([128, NT, 128]), ALU.is_equal, R=[cfT, pmf], W=[Rm])
                    for j in range(NT):
                        ra = rhs_all[j // JC]
                        p.mm(pacc[:, e_ * 32:(e_ + 1) * 32], Rm[:, j, :], ra[:, j % JC, e_].rearrange("p m g -> p (m g)"),
                             start=(j == 0), stop=(j == NT - 1), R=[Rm, ra], W=[pacc])
                accs = p.sb([128, 512], F32)
                p.cp("dve", accs[:], pacc[:, 0:512], R=[pacc], W=[accs])
                accv = accs[:, 0:512].rearrange("p (e m g) -> p e m g", e=NE, m=8)
                lf = p.sb([128, NE, NM], F32)
                p.stt("dve", lf[:], accv[:, :, 0:NM, 1], 128.0, accv[:, :, 0:NM, 0], ALU.mult, ALU.add, R=[accs], W=[lf])
                p.cp("dve", idx[:], lf[:], R=[lf], W=[idx])
                p.tt("dve", gate[:], accv[:, :, 0:NM, 2], accv[:, :, 0:NM, 3], ALU.add, R=[accs], W=[gate])
            with p.scope():
                wgr = p.ring(2, [128, 8, 512], BF16, name="wg"); wur = p.ring(2, [128, 8, 512], BF16, name="wu")
                wdr = p.ring(2, [128, 4, D], BF16, name="wd")
                xgr = p.ring(2 * NM + 2, [128, D], BF16)
                xgTr = p.ring(2, [128, 8, CAP], BF16, name="xgT")
                hidr = p.ring(2, [128, 4, CAP], BF16, name="hid")
                psT = p.ring(2, [128, D], BF16, psum=True)
                psG = p.ring(4, [128, 512], F32, psum=True)
                psY = p.ring(2, [128, 512], F32, psum=True)
                sgr = p.ring(2, [128, 512], F32)
                yr = p.ring(2, [128, D], F32)
                xacc = Tile(None)
                SH = min(512, CAP)
                def issue_loads(e_):
                    wg = wgr.next(); wu = wur.next(); wd = wdr.next()
                    p.dma("pool", wg[:], I["w_gate"][i, e_].rearrange("(k p) f -> p k f", p=128), W=[wg])
                    p.dma("pool", wu[:], I["w_up"][i, e_].rearrange("(k p) f -> p k f", p=128), W=[wu])
                    p.dma("pool", wd[:], I["w_down"][i, e_].rearrange("(k p) f -> p k f", p=128), W=[wd])
                    xgs = []
                    for m in range(NM):
                        xg = xgr.next()
                        p.dmaf("pool", lambda e, o=xg[:], ix=idx[:, e_, m:m + 1]: e.indirect_dma_start(
                            out=o, out_offset=None, in_=S["h2"][:, :], in_offset=bass.IndirectOffsetOnAxis(ap=ix, axis=0)),
                            R=[idx], W=[xg])
                        xgs.append(xg)
                    return wg, wu, wd, xgs

                nxt = issue_loads(0)
                for e_ in range(NE):
                    wg, wu, wd, xgs = nxt
                    if e_ + 1 < NE:
                        nxt = issue_loads(e_ + 1)
                    xgT = xgTr.next()
                    for m in range(NM):
                        xg = xgs[m]
                        pt = psT.next()
                        for k in range(8):
                            p.tr(pt[:, k * 128:(k + 1) * 128], xg[:, k * 128:(k + 1) * 128], CB("ident"), R=[xg, cbT], W=[pt])
                        p.cp("act" if m % 2 == 0 else "dve", xgT[:, :, m * 128:(m + 1) * 128], pt[:].rearrange("p (k t) -> p k t", k=8), R=[pt], W=[xgT])
                    hid = hidr.next()
                    for fc in range(4):
                        for sh in range(CAP // SH):
                            pg = psG.next(); pu = psG.next()
                            for k in range(8):
                                p.mm(pg[:, 0:SH], wg[:, k, fc * 128:(fc + 1) * 128], xgT[:, k, sh * SH:(sh + 1) * SH],
                                     start=(k == 0), stop=(k == 7), R=[wg, xgT], W=[pg])
                            for k in range(8):
                                p.mm(pu[:, 0:SH], wu[:, k, fc * 128:(fc + 1) * 128], xgT[:, k, sh * SH:(sh + 1) * SH],
                                     start=(k == 0), stop=(k == 7), R=[wu, xgT], W=[pu])
                            sg = sgr.next()
                            p.act(sg[:, 0:SH], pg[:, 0:SH], AF.Silu, R=[pg], W=[sg])
                            p.tt("dve", hid[:, fc, sh * SH:(sh + 1) * SH], sg[:, 0:SH], pu[:, 0:SH], ALU.mult, R=[sg, pu], W=[hid])
                    for m in range(NM):
                        y = yr.next()
                        for dh in range(2):
                            py = psY.next()
                            for fk in range(4):
                                p.mm(py[:], hid[:, fk, m * 128:(m + 1) * 128], wd[:, fk, dh * 512:(dh + 1) * 512],
                                     start=(fk == 0), stop=(fk == 3), R=[hid, wd], W=[py])
                            p.stt("dve", y[:, dh * 512:(dh + 1) * 512], py[:], gate[:, e_, m:m + 1], g2[:, dh * 512:(dh + 1) * 512],
                                  ALU.mult, ALU.mult, R=[py, gate, g2], W=[y])
                        p.dmaf("pool", lambda e, s_=y[:], ix=idx[:, e_, m:m + 1]: e.indirect_dma_start(
                            out=S["xl"][:, :], out_offset=bass.IndirectOffsetOnAxis(ap=ix, axis=0), in_=s_, in_offset=None,
                            compute_op=ALU.add), R=[y, idx], W=[xacc])

    def phase_final():
        with p.scope():
            gfin = p.sb([128, D], F32)
            rowvec(gfin, I["norm_final"])
            xr = p.ring(3, [128, D], F32); junk = p.sb([128, D], BF16); ssr = p.ring(3, [128, 2], F32)
            orr = p.ring(3, [128, D], F32)
            for j in range(NT):
                t0 = j * 128
                xt = xr.next()
                p.dma("sp", xt[:], S["xl"][t0:t0 + 128, :], W=[xt])
                ss = ssr.next()
                p.memset("pool", ss[:], 0.0, W=[ss])
                p.act(junk[:], xt[:], AF.Square, accum=ss[:, 0:1], R=[xt], W=[junk, ss])
                p.ts("dve", ss[:, 1:2], ss[:, 0:1], 1.0 / D, EPS, ALU.mult, ALU.add, R=[ss], W=[ss])
                p.rsqrt(ss[:, 1:2], ss)
                o = orr.next()
                p.stt("dve", o[:], xt[:], ss[:, 1:2], gfin[:], ALU.mult, ALU.mult, R=[xt, ss, gfin], W=[o])
                p.dma("pool", out[t0:t0 + 128, :], o[:], R=[o])

    phases = [
        ("hT0", lambda: phase_hT([(I["ctx"], CTX, 0, 1), (I["x"], L, CTX, 0)], 0, I["norm_mix"][0])),
        ("proj", phase_even_proj), ("delta", phase_delta), ("evout", phase_even_out),
        ("moe0", lambda: phase_moe(0)),
        ("hT1", lambda: phase_hT([(S["xl"], L, CTX, 0)], 1, I["norm_mix"][1])),
        ("odd", phase_odd), ("moe1", lambda: phase_moe(1)), ("final", phase_final),
    ]
    for nm, fn in phases:
        fn()
        if stop == nm:
            break
    p.flush()
    p.finish()
    print("bass instructions:", p.ninst, {e: p.cnt[e] for e in p.ENG})
    return nc


def prep_inputs(b, inputs, L):
    arr_f, _, arr_b, _ = make_consts()
    f = lambda a: np.ascontiguousarray(np.asarray(a, dtype=np.float32))
    c = f(inputs["c"])[b]
    cc = f(inputs["c_ctx"])
    cvec = np.concatenate([c.reshape(8, 128).T, cc.reshape(8, 128).T], axis=1)
    m = dict(
        x=f(inputs["x"])[b][:L], ctx=f(inputs["ctx"])[b], cvec=f(cvec),
        ada_w=f(inputs["ada_w"]), ada_b=f(inputs["ada_b"]),
        norm_mix=f(inputs["norm_mix"]), norm_ffn=f(inputs["norm_ffn"]), norm_final=f(inputs["norm_final"]),
        ev_w_in=f(inputs["ev_w_in"])[0],
        dn_conv=f(f(inputs["dn_conv"])[0].reshape(3, 12, 128).transpose(2, 1, 0).reshape(128, 36)),
        dn_a_log=f(inputs["dn_a_log"])[0].reshape(8), dn_dt_bias=f(inputs["dn_dt_bias"])[0].reshape(8),
        dn_norm=f(inputs["dn_norm"])[0], pool_w=f(inputs["pool_w"])[0],
        pool_scale=f(f(inputs["pool_scale"])[0].reshape(4, 128).T),
        ev_w_out=f(inputs["ev_w_out"])[0],
        sc_w_in=f(inputs["sc_w_in"])[0],
        sc_conv=f(f(inputs["sc_conv"])[0].reshape(3, 8, 128).transpose(2, 1, 0).reshape(128, 24)),
        sc_w_out=f(inputs["sc_w_out"])[0],
        router=f(inputs["router"]), w_gate=f(inputs["w_gate"]), w_up=f(inputs["w_up"]), w_down=f(inputs["w_down"]),
        cf=arr_f, cb=arr_b,
    )
    return m


_NC_CACHE = {}


def kernel(**inputs):
    x = np.asarray(inputs["x"])
    B, L, _ = x.shape
    if L not in _NC_CACHE:
        _NC_CACHE[L] = build(L)
    nc = _NC_CACHE[L]
    maps = [prep_inputs(b, inputs, L) for b in range(B)]
    n = 8
    in_maps = [maps[c % B] for c in range(n)]
    res = run_bass_kernel_spmd(nc, in_maps, core_ids=list(range(n)))
    outs = [np.asarray(res.results[b]["out"], dtype=np.float32) for b in range(B)]
    return np.stack(outs, axis=0)
```

```python
import numpy as np
import ml_dtypes
from contextlib import ExitStack
import concourse.bass as bass
import concourse.mybir as mybir
from concourse.bass_utils import run_bass_kernel_spmd

F32 = mybir.dt.float32
BF16 = mybir.dt.bfloat16
I32 = mybir.dt.int32
ALU = mybir.AluOpType
AF = mybir.ActivationFunctionType
AX = mybir.AxisListType
EPS = 1e-6
D = 1024
CTX = 256
NE = 16


class Buf:
    __slots__ = ("w", "r")

    def __init__(self):
        self.w = None
        self.r = []


class Tile:
    def __init__(self, t):
        self.t = t
        self.b = Buf()

    def __getitem__(self, k):
        return self.t[k]


class Ring:
    def __init__(self, tiles):
        self.tiles = tiles
        self.i = 0

    def next(self):
        t = self.tiles[self.i % len(self.tiles)]
        self.i += 1
        return t


class Prog:
    ENG = ("pe", "act", "dve", "pool", "sp")
    NDMA = 48
    NHW = 32

    def __init__(self, nc):
        self.nc = nc
        self.es = ExitStack()
        self.q = {e: [] for e in self.ENG}
        self.cnt = {e: 0 for e in self.ENG}
        self.seen = {e: {} for e in self.ENG}
        self.sem = {e: self.es.enter_context(nc.semaphore("pg_" + e)) for e in self.ENG}
        self.dsem = [self.es.enter_context(nc.semaphore("pg_d%d" % i)) for i in range(self.NDMA)]
        self.dcnt = [0] * self.NDMA
        self.dnext = 0
        self.dnext_sw = 0
        self.ninst = 0
        self.uid = 0

    def sb(self, shape, dt, name=None):
        self.uid += 1
        return Tile(self.es.enter_context(self.nc.sbuf_tensor("%s_%d" % (name or "sb", self.uid), list(shape), dt)))

    def ps(self, shape, dt, name=None):
        self.uid += 1
        return Tile(self.es.enter_context(self.nc.psum_tensor("%s_%d" % (name or "ps", self.uid), list(shape), dt)))

    def ring(self, n, shape, dt, psum=False, name=None):
        return Ring([(self.ps if psum else self.sb)(shape, dt, name) for _ in range(n)])

    def _semof(self, tl):
        return self.sem[tl] if isinstance(tl, str) else self.dsem[tl]

    def _deps(self, eng, reads, writes):
        deps = {}

        def add(tok):
            if tok is None:
                return
            tl, v = tok
            if tl == "pe" and eng == "pe":
                return
            if deps.get(tl, 0) < v:
                deps[tl] = v
        for b in reads:
            add(b.b.w)
        for b in writes:
            add(b.b.w)
            for t in b.b.r:
                add(t)
        waits = []
        sn = self.seen[eng]
        for tl, v in deps.items():
            if sn.get(tl, 0) < v:
                sn[tl] = v
                waits.append((tl, v))
        return waits

    def _mark(self, tok, reads, writes):
        for b in reads:
            b.b.r.append(tok)
            if len(b.b.r) > 64:
                b.b.r = b.b.r[-48:]
        for b in writes:
            b.b.w = tok
            b.b.r = []

    def op(self, eng, fn, R=(), W=()):
        waits = self._deps(eng, R, W)
        self.cnt[eng] += 1
        tok = (eng, self.cnt[eng])
        self.q[eng].append((waits, fn, (eng, 1)))
        self._mark(tok, R, W)
        self.ninst += 1
        return tok

    def dma(self, eng, out, in_, R=(), W=(), **kw):
        return self.dmaf(eng, lambda e: e.dma_start(out=out, in_=in_, **kw), R, W)

    def dmaf(self, eng, fn, R=(), W=()):
        waits = self._deps(eng, R, W)
        if eng == "pool":
            k = self.NHW + self.dnext_sw
            self.dnext_sw = (self.dnext_sw + 1) % (self.NDMA - self.NHW)
        else:
            k = self.dnext
            self.dnext = (self.dnext + 1) % self.NHW
        if self.dcnt[k] > 0 and self.seen[eng].get(k, 0) < self.dcnt[k]:
            self.seen[eng][k] = self.dcnt[k]
            waits.append((k, self.dcnt[k]))
        self.dcnt[k] += 16
        tok = (k, self.dcnt[k])
        self.q[eng].append((waits, fn, (k, 16)))
        self._mark(tok, R, W)
        self.ninst += 1
        return tok

    def mm(self, out, lhsT, rhs, start=True, stop=True, R=(), W=()):
        return self.op("pe", lambda e: e.matmul(out, lhsT=lhsT, rhs=rhs, start=start, stop=stop), R, W)

    def tr(self, out, in_, ident, R=(), W=()):
        return self.op("pe", lambda e: e.transpose(out, in_, ident), R, W)

    def act(self, out, in_, func, bias=None, scale=1.0, accum=None, R=(), W=()):
        def f(e):
            kw = {}
            if bias is not None:
                kw["bias"] = bias
            if accum is not None:
                kw["accum_out"] = accum
            return e.activation(out=out, in_=in_, func=func, scale=scale, **kw)
        return self.op("act", f, R, W)

    def ts(self, eng, out, in0, s1, s2, op0, op1=None, R=(), W=()):
        def f(e):
            if op1 is None:
                return e.tensor_scalar(out=out, in0=in0, scalar1=s1, scalar2=None, op0=op0)
            return e.tensor_scalar(out=out, in0=in0, scalar1=s1, scalar2=s2, op0=op0, op1=op1)
        return self.op(eng, f, R, W)

    def tt(self, eng, out, in0, in1, op, R=(), W=()):
        return self.op(eng, lambda e: e.tensor_tensor(out=out, in0=in0, in1=in1, op=op), R, W)

    def stt(self, eng, out, in0, scalar, in1, op0, op1, R=(), W=()):
        return self.op(eng, lambda e: e.scalar_tensor_tensor(out=out, in0=in0, scalar=scalar, in1=in1, op0=op0, op1=op1), R, W)

    def cp(self, eng, out, in_, R=(), W=()):
        if eng == "act":
            return self.op("act", lambda e: e.copy(out=out, in_=in_), R, W)
        return self.op(eng, lambda e: e.tensor_copy(out=out, in_=in_), R, W)

    def rsqrt(self, ap, T):
        self.op("act", lambda e: e.activation(out=ap, in_=ap, func=AF.Sqrt), [T], [T])
        self.op("dve", lambda e: e.reciprocal(out=ap, in_=ap), [T], [T])

    def memset(self, eng, ap, val, W=()):
        return self.op(eng, lambda e: e.memset(ap, val), (), W)

    def barrier(self):
        for e in self.ENG:
            waits = []
            sn = self.seen[e]
            for f in self.ENG:
                if f != e and sn.get(f, 0) < self.cnt[f]:
                    sn[f] = self.cnt[f]
                    waits.append((f, self.cnt[f]))
            for k in range(self.NDMA):
                if self.dcnt[k] > 0 and sn.get(k, 0) < self.dcnt[k]:
                    sn[k] = self.dcnt[k]
                    waits.append((k, self.dcnt[k]))
            if waits:
                self.q[e].append((waits, None, None))

    def scope(self):
        prog = self

        class _S:
            def __enter__(s2):
                s2.outer = prog.es
                prog.es = ExitStack()
                return prog

            def __exit__(s2, *a):
                if a[0] is None:
                    prog.flush()
                prog.es.close()
                prog.es = s2.outer
                return False
        return _S()

    def flush(self):
        nc = self.nc
        self.barrier()
        with nc.Block() as block:
            def run(engname):
                def body(eng):
                    for waits, fn, inc in self.q[engname]:
                        for tl, v in waits:
                            eng.wait_ge(self._semof(tl), v)
                        if fn is not None:
                            fn(eng).then_inc(self._semof(inc[0]), inc[1])
                return body
            block.tensor(run("pe"))
            block.scalar(run("act"))
            block.vector(run("dve"))
            block.gpsimd(run("pool"))
            block.sync(run("sp"))
        self.q = {e: [] for e in self.ENG}

    def finish(self):
        self.es.close()


def interleave(gens):
    gens = list(gens)
    while gens:
        nxt = []
        for g in gens:
            try:
                next(g)
                nxt.append(g)
            except StopIteration:
                pass
        gens = nxt


def make_consts():
    cf = {}
    i = np.arange(128)
    cf["ones"] = np.ones((128, 128), np.float32)
    cf["ucf"] = (i[:, None] <= i[None, :]).astype(np.float32)
    cf["ucb"] = (i[:, None] >= i[None, :]).astype(np.float32)
    cf["msf"] = (i[None, :] > i[:, None]).astype(np.float32)
    cf["msb"] = (i[None, :] < i[:, None]).astype(np.float32)
    cf["mif"] = (i[None, :] >= i[:, None]).astype(np.float32)
    cf["mib"] = (i[None, :] <= i[:, None]).astype(np.float32)
    cf["iota"] = np.broadcast_to(i[None, :].astype(np.float32), (128, 128)).copy()
    cf["identf"] = np.eye(128, dtype=np.float32)
    misc = np.zeros((128, 128), np.float32)
    misc[:, 0] = i
    misc[:, 8:16] = np.arange(8)[None, :]
    cf["misc"] = misc
    inv = np.zeros((128, 4 * 128), np.float32)
    pm = np.zeros((4, 128, 128), np.float32)
    for g, w in enumerate((2, 4, 8, 16)):
        for t in range(128):
            seg = t // 64
            tl = t % 64
            lo = min(max(tl - w // 2, 0), 64)
            hi = min(max(tl + w - w // 2, 0), 64)
            cnt = hi - lo
            pm[g, seg * 64 + lo: seg * 64 + hi, t] += 1.0
            pm[g, t, t] -= cnt
            inv[:, g * 128 + t] = 1.0 / cnt
    cf["invcnt"] = inv
    lvl_names = []
    for lv in range(7):
        b = 1 << lv
        jj, ii = i[:, None], i[None, :]
        mf = ((jj // (2 * b)) == (ii // (2 * b))) & ((jj % (2 * b)) < b) & ((ii % (2 * b)) >= b)
        cf["nmf%d" % lv] = -(mf.astype(np.float32))
        cf["nmb%d" % lv] = -(mf.T.astype(np.float32))
        lvl_names += ["nmf%d" % lv, "nmb%d" % lv]
    names_f = ["ones", "ucf", "ucb", "msf", "msb", "mif", "mib", "iota", "identf", "misc"] + lvl_names
    arr_f = np.concatenate([cf[n] for n in names_f] + [inv], axis=1).astype(np.float32)
    off_f = {n: k * 128 for k, n in enumerate(names_f)}
    off_f["invcnt"] = len(names_f) * 128
    cb = [np.eye(128, dtype=np.float32), np.ones((128, 128), np.float32),
          (i[:, None] < i[None, :]).astype(np.float32)] + [pm[g] for g in range(4)]
    arr_b = np.concatenate(cb, axis=1).astype(ml_dtypes.bfloat16)
    off_b = {"ident": 0, "ones": 128, "lstrict": 256, "pm": 384}
    return arr_f, off_f, arr_b, off_b


def build(L, debug_outs=(), stop=None):
    assert L % 256 == 0
    NT = L // 128
    LT = CTX + L
    NCH = LT // 128
    CAP = 2 * L // NE
    NM = CAP // 128
    assert CAP % 128 == 0
    arr_f, OF, arr_b, OB = make_consts()
    nc = bass.Bass("TRN2", target_bir_lowering=False)

    def din(name, shape, dt=F32):
        return nc.dram_tensor(name, list(shape), dt, kind="ExternalInput").ap()

    def dscr(name, shape, dt=F32):
        kind = "ExternalOutput" if name in debug_outs else "Internal"
        return nc.dram_tensor(name, list(shape), dt, kind=kind).ap()

    I = dict(
        x=din("x", [L, D]), ctx=din("ctx", [CTX, D]), cvec=din("cvec", [128, 16]),
        ada_w=din("ada_w", [2, D, 6 * D]), ada_b=din("ada_b", [2, 6 * D]),
        norm_mix=din("norm_mix", [2, D]), norm_ffn=din("norm_ffn", [2, D]), norm_final=din("norm_final", [D]),
        ev_w_in=din("ev_w_in", [D, 2576]), dn_conv=din("dn_conv", [128, 36]),
        dn_a_log=din("dn_a_log", [8]), dn_dt_bias=din("dn_dt_bias", [8]), dn_norm=din("dn_norm", [128]),
        pool_w=din("pool_w", [4, 128, 128]), pool_scale=din("pool_scale", [128, 4]),
        ev_w_out=din("ev_w_out", [D, D]),
        sc_w_in=din("sc_w_in", [D, 3 * D]), sc_conv=din("sc_conv", [128, 24]), sc_w_out=din("sc_w_out", [D, D]),
        router=din("router", [2, D, NE]), w_gate=din("w_gate", [2, NE, D, 512]),
        w_up=din("w_up", [2, NE, D, 512]), w_down=din("w_down", [2, NE, 512, D]),
        cf=din("cf", list(arr_f.shape)), cb=din("cb", list(arr_b.shape), BF16),
    )
    out = nc.dram_tensor("out", [L, D], F32, kind="ExternalOutput").ap()
    S = dict(
        mod=dscr("mod", [2, 2, 6 * D]),
        hT=dscr("hT", [8, 128, LT], BF16),
        qT=dscr("qT", [4, 128, LT], BF16), kT=dscr("kT", [4, 128, LT], BF16),
        ktok=dscr("ktok", [LT, 512], BF16), vtok=dscr("vtok", [LT, 512], BF16),
        gates=dscr("gates", [LT, 16]), zs=dscr("zs", [L, 512], BF16),
        mixT=dscr("mixT", [8, 128, L], BF16),
        of=dscr("of", [L, 512]), ob=dscr("ob", [L, 512]),
        xl=dscr("xl", [L, D]), h2=dscr("h2", [L, D], BF16),
    )
    p = Prog(nc)
    cfT = p.sb([128, arr_f.shape[1]], F32, "cf")
    cbT = p.sb([128, arr_b.shape[1]], BF16, "cb")
    p.dma("sp", cfT[:], I["cf"], W=[cfT])
    p.dma("sp", cbT[:], I["cb"], W=[cbT])

    def CF(n, w=128):
        return cfT[:, OF[n]:OF[n] + w]

    def CB(n, w=128, o=0):
        return cbT[:, OB[n] + o:OB[n] + o + w]

    def colvec(dst, src1d, eng="sp"):
        p.dma(eng, dst[:], src1d.rearrange("(k p) -> p k", p=128), W=[dst], allow_slow_non_contiguous=True)

    def rowvec(dst, src1d, eng="sp"):
        p.dma(eng, dst[:], src1d.partition_broadcast(128), W=[dst])

    with p.scope():
        cv = p.sb([128, 16], F32)
        sc = p.sb([128, 16], F32)
        p.dma("sp", cv[:], I["cvec"], W=[cv])
        p.act(sc[:], cv[:], AF.Silu, R=[cv], W=[sc])
        awr = p.ring(2, [128, 8, 512], F32)
        psr = p.ring(2, [128, 512], F32, psum=True)
        for i in range(2):
            bias = p.sb([2, 6 * D], F32)
            modrow = p.sb([2, 6 * D], F32)
            for r in range(2):
                p.dma("act", bias[r:r + 1, :], I["ada_b"][i:i + 1, :], W=[bias])
            for n in range(12):
                a = awr.next()
                p.dma("sp", a[:], I["ada_w"][i][:, n * 512:(n + 1) * 512].rearrange("(k p) c -> p k c", p=128), W=[a])
                ps = psr.next()
                for k in range(8):
                    p.mm(ps[0:2, :], sc[:, k::8], a[:, k, :], start=(k == 0), stop=(k == 7), R=[sc, a], W=[ps])
                p.tt("dve", modrow[0:2, n * 512:(n + 1) * 512], ps[0:2, :], bias[0:2, n * 512:(n + 1) * 512], ALU.add,
                     R=[ps, bias], W=[modrow])
            p.dma("sp", S["mod"][i], modrow[0:2, :], R=[modrow])

    def modv(i, r, j):
        return S["mod"][i, r, j * D:(j + 1) * D]

    def phase_hT(srcs, i, normvec):
        with p.scope():
            xr = p.ring(3, [128, D], F32)
            junk = p.sb([128, D], BF16)
            ssr = p.ring(3, [128, 2], F32)
            xnr = p.ring(2, [128, D], BF16)
            pTr = p.ring(2, [128, D], BF16, psum=True)
            tmpr = p.ring(2, [128, 8, 128], F32)
            hsr = p.ring(2, [128, 8, 256], BF16)
            for (src, ntok, col0, r) in srcs:
                gcol = p.sb([128, 8], F32)
                shc = p.sb([128, 8], F32)
                scc = p.sb([128, 8], F32)
                A = p.sb([128, 8], F32)
                colvec(gcol, normvec)
                colvec(shc, modv(i, r, 0))
                colvec(scc, modv(i, r, 1))
                p.stt("dve", A[:], scc[:], 1.0, gcol[:], ALU.add, ALU.mult, R=[scc, gcol], W=[A])
                def hA(t0):
                    xt = xr.next()
                    p.dma("sp", xt[:], src[t0:t0 + 128, :], W=[xt])
                    ss = ssr.next()
                    p.memset("pool", ss[:], 0.0, W=[ss])
                    p.act(junk[:], xt[:], AF.Square, accum=ss[:, 0:1], R=[xt], W=[junk, ss])
                    p.ts("dve", ss[:, 1:2], ss[:, 0:1], 1.0 / D, EPS, ALU.mult, ALU.add, R=[ss], W=[ss])
                    p.rsqrt(ss[:, 1:2], ss)
                    xn = xnr.next()
                    p.act(xn[:], xt[:], AF.Copy, scale=ss[:, 1:2], R=[xt, ss], W=[xn])
                    return xn

                def hB(xn, hs, t2):
                    pT = pTr.next()
                    for k in range(8):
                        p.tr(pT[:, k * 128:(k + 1) * 128], xn[:, k * 128:(k + 1) * 128], CB("ident"), R=[xn, cbT], W=[pT])
                    tmp = tmpr.next()
                    p.tt("dve", tmp[:], pT[:].rearrange("p (k t) -> p k t", k=8),
                         A[:, :].unsqueeze(2).to_broadcast([128, 8, 128]), ALU.mult, R=[pT, A], W=[tmp])
                    p.tt("pool", hs[:, :, t2 * 128:(t2 + 1) * 128], tmp[:],
                         shc[:, :].unsqueeze(2).to_broadcast([128, 8, 128]), ALU.add, R=[tmp, shc], W=[hs])

                ntile = ntok // 128
                pend = None
                hs = None
                for j in range(ntile + 1):
                    xn = hA(j * 128) if j < ntile else None
                    if pend is not None:
                        jj, pxn = pend
                        if jj % 2 == 0:
                            hs = hsr.next()
                        hB(pxn, hs, jj % 2)
                        if jj % 2 == 1:
                            c0 = col0 + (jj // 2) * 256
                            p.dma("pool", S["hT"][:, :, c0:c0 + 256].rearrange("k p t -> p k t"), hs[:], R=[hs])
                    pend = (j, xn)

    def load_window(hw, col0, ntok, blk):
        a = col0 + blk * 256
        lo = a - 1 if blk > 0 else a
        hi = a + 257 if (blk + 1) * 256 < ntok else a + 256
        d0 = 0 if blk > 0 else 1
        if blk == 0:
            p.memset("pool", hw[:, :, 0:1], 0.0, W=[hw])
        if (blk + 1) * 256 >= ntok:
            p.memset("pool", hw[:, :, 257:258], 0.0, W=[hw])
        p.dma("sp", hw[:, :, d0:d0 + (hi - lo)], S["hT"][:, :, lo:hi].rearrange("k p t -> p k t"), W=[hw])

    def conv3(ps, wcol, c, acc, psT, accT):
        p.act(acc[:], ps[:, 1:257], AF.Copy, scale=wcol[:, c, 1:2], R=[psT, wcol], W=[accT])
        p.stt("dve", acc[:], ps[:, 0:256], wcol[:, c, 0:1], acc[:], ALU.mult, ALU.add, R=[psT, wcol, accT], W=[accT])
        p.stt("dve", acc[:], ps[:, 2:258], wcol[:, c, 2:3], acc[:], ALU.mult, ALU.add, R=[psT, wcol, accT], W=[accT])

    def phase_even_proj():
        with p.scope():
            w = p.sb([128, 8, 2576], BF16, "wev")
            for k in range(8):
                p.dma("pool", w[:, k, :], I["ev_w_in"][k * 128:(k + 1) * 128, :], W=[w])
            cw = p.sb([128, 12, 3], F32)
            p.dma("sp", cw[:], I["dn_conv"].rearrange("p (c t) -> p c t", t=3), W=[cw])
            pw = p.sb([128, 4, 128], BF16)
            p.dma("pool", pw[:], I["pool_w"].rearrange("g c d -> c g d"), W=[pw])
            pscale = p.sb([128, 4], F32)
            p.dma("sp", pscale[:], I["pool_scale"], W=[pscale])
            dtb = p.sb([128, 8], F32)
            nega = p.sb([128, 8], F32)
            rowvec(dtb, I["dn_dt_bias"])
            rowvec(nega, I["dn_a_log"])
            p.act(nega[:], nega[:], AF.Exp, R=[nega], W=[nega])
            p.ts("dve", nega[:], nega[:], -1.0, None, ALU.mult, R=[nega], W=[nega])
            hwr = p.ring(2, [128, 8, 258], BF16)
            psW = p.ring(3, [128, 512], F32, psum=True)
            psT = p.ring(2, [128, 1024], BF16, psum=True)
            psS = p.ring(2, [128, 512], F32, psum=True)
            accr = p.ring(3, [128, 256], F32)
            sr = p.ring(3, [128, 256], F32)
            sqr = p.ring(2, [128, 256], BF16)
            rinvr = p.ring(2, [128, 256], F32)
            qnr = p.ring(3, [128, 256], BF16)
            tokr = p.ring(3, [128, 2, 128], BF16)
            zr = p.ring(2, [128, 512], BF16)
            ur = p.ring(2, [128, 512], BF16)
            g4r = p.ring(2, [128, 512], BF16)
            m4r = p.ring(2, [128, 4, 128], BF16)
            gtr = p.ring(2, [128, 48], F32)
            for (col0, ntok, lat) in ((0, CTX, False), (CTX, L, True)):
                for blk in range(ntok // 256):
                    hw = hwr.next()
                    load_window(hw, col0, ntok, blk)
                    c0 = col0 + blk * 256
                    def stageA(c):
                        ps = psW.next()
                        for k in range(8):
                            p.mm(ps[:, 0:258], w[:, k, c * 128:(c + 1) * 128], hw[:, k, :], start=(k == 0), stop=(k == 7),
                                 R=[w, hw], W=[ps])
                        acc = accr.next()
                        conv3(ps, cw, c, acc, ps, acc)
                        st = dict(c=c)
                        if c < 8:
                            s_ = sr.next()
                            p.act(s_[:], acc[:], AF.Silu, R=[acc], W=[s_])
                            sq = sqr.next()
                            p.tt("pool", sq[:], s_[:], s_[:], ALU.mult, R=[s_], W=[sq])
                            st.update(s=s_, sq=sq)
                        else:
                            qn = qnr.next()
                            p.act(qn[:], acc[:], AF.Silu, R=[acc], W=[qn])
                            st.update(qn=qn)
                        return st

                    def stageB(st):
                        c = st["c"]
                        h = c % 4
                        if c < 8:
                            s_ = st["s"]; sq = st["sq"]
                            pss = psS.next()
                            p.mm(pss[:, 0:256], CB("ones"), sq[:], R=[sq, cbT], W=[pss])
                            rinv = rinvr.next()
                            p.ts("dve", rinv[:], pss[:, 0:256], EPS, None, ALU.add, R=[pss], W=[rinv])
                            p.rsqrt(rinv[:], rinv)
                            qn = qnr.next()
                            p.stt("dve", qn[:], s_[:], (128.0 ** -0.5) if c < 4 else 1.0, rinv[:], ALU.mult, ALU.mult,
                                  R=[s_, rinv], W=[qn])
                            dst = S["qT"] if c < 4 else S["kT"]
                            p.dma("pool", dst[h, :, c0:c0 + 256], qn[:], R=[qn])
                            src_tok = qn if c >= 4 else None
                            dtok = S["ktok"]
                        else:
                            src_tok = st["qn"]
                            dtok = S["vtok"]
                        if src_tok is not None:
                            pt = psT.next()
                            for t2 in range(2):
                                p.tr(pt[:, t2 * 128:(t2 + 1) * 128], src_tok[:, t2 * 128:(t2 + 1) * 128], CB("ident"),
                                     R=[src_tok, cbT], W=[pt])
                            tk = tokr.next()
                            p.cp("act", tk[:], pt[:, 0:256].rearrange("p (a b) -> p a b", a=2), R=[pt], W=[tk])
                            p.dma("pool", dtok[c0:c0 + 256, h * 128:(h + 1) * 128].rearrange("(a p) d -> p a d", p=128), tk[:], R=[tk])

                    pend = None
                    for c in range(12):
                        st = stageA(c)
                        if pend is not None:
                            stageB(pend)
                        pend = st
                    stageB(pend)
                    for t2 in range(2):
                        t0 = c0 + t2 * 128
                        lhs = lambda k: hw[:, k, 1 + t2 * 128:1 + (t2 + 1) * 128]
                        psg = psS.next()
                        for k in range(8):
                            p.mm(psg[:, 0:16], lhs(k), w[:, k, 2048:2064], start=(k == 0), stop=(k == 7), R=[w, hw], W=[psg])
                        if lat:
                            psz = psW.next()
                            for k in range(8):
                                p.mm(psz[:], lhs(k), w[:, k, 1536:2048], start=(k == 0), stop=(k == 7), R=[w, hw], W=[psz])
                            psp = psW.next()
                            for k in range(8):
                                p.mm(psp[:], lhs(k), w[:, k, 2064:2576], start=(k == 0), stop=(k == 7), R=[w, hw], W=[psp])
                        ps = psg
                        g = gtr.next()
                        p.act(g[:, 0:8], ps[:, 0:8], AF.Sigmoid, R=[ps], W=[g])
                        p.tt("dve", g[:, 16:24], ps[:, 8:16], dtb[:], ALU.add, R=[ps, dtb], W=[g])
                        p.ts("dve", g[:, 24:32], g[:, 16:24], 30.0, None, ALU.min, R=[g], W=[g])
                        p.act(g[:, 24:32], g[:, 24:32], AF.Exp, R=[g], W=[g])
                        p.act(g[:, 24:32], g[:, 24:32], AF.Ln, bias=1.0, R=[g], W=[g])
                        p.ts("dve", g[:, 32:40], g[:, 16:24], -30.0, 0.0, ALU.add, ALU.max, R=[g], W=[g])
                        p.tt("dve", g[:, 24:32], g[:, 24:32], g[:, 32:40], ALU.add, R=[g], W=[g])
                        p.tt("dve", g[:, 8:16], g[:, 24:32], nega[:], ALU.mult, R=[g, nega], W=[g])
                        p.dma("pool", S["gates"][t0:t0 + 128, :], g[:, 0:16], R=[g])
                        if not lat:
                            continue
                        tl = t0 - CTX
                        z = zr.next()
                        p.act(z[:], psz[:], AF.Silu, R=[psz], W=[z])
                        p.dma("pool", S["zs"][tl:tl + 128, :], z[:], R=[z])
                        u = ur.next()
                        p.cp("act", u[:], psp[:], R=[psp], W=[u])
                        ps1 = psS.next()
                        for gi in range(4):
                            p.mm(ps1[:, gi * 128:(gi + 1) * 128], u[:, gi * 128:(gi + 1) * 128], CB("pm", 128, gi * 128), R=[u, cbT], W=[ps1])
                        gT4 = g4r.next()
                        p.tt("dve", gT4[:], ps1[:], cfT[:, OF["invcnt"]:OF["invcnt"] + 512], ALU.mult, R=[ps1, cfT], W=[gT4])
                        ps2 = psS.next()
                        for gi in range(4):
                            p.mm(ps2[:, gi * 128:(gi + 1) * 128], pw[:, gi, :], gT4[:, gi * 128:(gi + 1) * 128], R=[pw, gT4], W=[ps2])
                        mo4 = m4r.next()
                        p.tt("dve", mo4[:], ps2[:].rearrange("p (g t) -> p g t", g=4),
                             pscale[:, :].unsqueeze(2).to_broadcast([128, 4, 128]), ALU.mult, R=[ps2, pscale], W=[mo4])
                        p.dma("pool", S["mixT"][4:8, :, tl:tl + 128].rearrange("k p t -> p k t"), mo4[:], R=[mo4])

    def phase_delta():
        with p.scope():
            psPd = {0: p.ring(2, [128, 4, 128], F32, psum=True, name="psP0"),
                    1: p.ring(2, [128, 4, 128], F32, psum=True, name="psP1")}
            psSc = {0: p.ring(2, [128, 4, 128], F32, psum=True, name="psS0"),
                    1: p.ring(2, [128, 4, 128], F32, psum=True, name="psS1")}
            NS = 3
            names_bf = ["qT", "kT", "ktok", "vtok", "P", "qkd", "qd", "kd", "B", "BT"]
            ins = {}
            for d in range(2):
                ins[d] = dict(
                    g=p.ring(NS, [128, 16], F32), sm=p.ring(NS, [128, 32], F32),
                    X=p.ring(2, [128, 4, 128], F32), E=p.ring(2, [128, 4, 128], F32),
                    Es=p.ring(2, [128, 4, 128], F32), Ei=p.ring(2, [128, 4, 128], F32),
                    lu=p.ring(2, [128, 4, 128], F32), eg=p.ring(2, [128, 4, 128], F32),
                    nR=p.ring(2, [128, 4, 128], BF16), w=p.ring(2, [128, 4, 128], BF16),
                    osb=p.ring(2, [128, 4, 128], F32),
                    S32=p.sb([128, 4, 128], F32), Sbf=p.ring(2, [128, 4, 128], BF16),
                )
                ins[d]["B32"] = p.ring(2, [128, 4, 128], F32)
                ins[d]["GT"] = p.ring(2, [128, 4, 128], BF16)
                ins[d]["Xb"] = p.ring(2, [128, 4, 128], BF16)
                ins[d]["tmp"] = p.ring(4, [128, 4, 128], BF16)
                for nm in names_bf:
                    ins[d][nm] = p.ring(NS if nm not in ("B", "BT", "B2", "B2T") else 2, [128, 4, 128], BF16)
            order = {0: list(range(NCH)), 1: [1, 0] + list(range(NCH - 1, 1, -1))}
            prepped = {0: {}, 1: {}}

            def prep(d, it):
                R_ = ins[d]
                psP = psPd[d]
                c = order[d][it]
                t0 = c * 128
                ucum = CF("ucf") if d == 0 else CF("ucb")
                ms = CF("msf") if d == 0 else CF("msb")
                mi = CF("mif") if d == 0 else CF("mib")
                lastcol = 127 if d == 0 else 0
                qT = R_["qT"].next(); kT = R_["kT"].next(); ktok = R_["ktok"].next(); vtok = R_["vtok"].next()
                g = R_["g"].next(); sm = R_["sm"].next()
                p.dma("act", qT[:], S["qT"][:, :, t0:t0 + 128].rearrange("h p t -> p h t"), W=[qT])
                p.dma("act", kT[:], S["kT"][:, :, t0:t0 + 128].rearrange("h p t -> p h t"), W=[kT])
                p.dma("act", ktok[:], S["ktok"][t0:t0 + 128, :].rearrange("p (h d) -> p h d", h=4), W=[ktok])
                p.dma("act", vtok[:], S["vtok"][t0:t0 + 128, :].rearrange("p (h d) -> p h d", h=4), W=[vtok])
                p.dma("act", g[:], S["gates"][t0:t0 + 128, :], W=[g])
                ld = g[:, 8 + 4 * d:12 + 4 * d]
                beta = g[:, 4 * d:4 * d + 4]
                yield
                ps = psP.next()
                p.mm(ps[:, 0, 0:4], ucum, ld, R=[cfT, g], W=[ps])
                p.cp("dve", sm[:, 0:4], ps[:, 0, 0:4], R=[ps], W=[sm])
                lu = R_["lu"].next()
                p.tt("dve", lu[:], ucum.unsqueeze(1).to_broadcast([128, 4, 128]),
                     ld.unsqueeze(2).to_broadcast([128, 4, 128]), ALU.mult, R=[cfT, g], W=[lu])
                pg = psP.next()
                for h in range(4):
                    p.mm(pg[:, h, :], CF("ones"), lu[:, h, :], R=[cfT, lu], W=[pg])
                yield
                p.cp("dve", sm[:, 4:8], pg[:, :, lastcol], R=[pg], W=[sm])
                X = R_["X"].next()
                p.tt("dve", X[:], pg[:], sm[:, 0:4].unsqueeze(2).to_broadcast([128, 4, 128]), ALU.subtract, R=[pg, sm], W=[X])
                E = R_["E"].next()
                p.act(X[:], X[:], AF.Relu, scale=-1.0, R=[X], W=[X])
                p.act(E[:], X[:], AF.Exp, scale=-1.0, R=[X], W=[E])
                eg = R_["eg"].next()
                p.act(eg[:], pg[:], AF.Exp, R=[pg], W=[eg])
                p.tt("dve", sm[:, 8:12], sm[:, 4:8], sm[:, 0:4], ALU.subtract, R=[sm], W=[sm])
                p.act(sm[:, 8:12], sm[:, 8:12], AF.Exp, R=[sm], W=[sm])
                p.act(sm[:, 12:16], sm[:, 0:4], AF.Exp, R=[sm], W=[sm])
                p.ts("dve", sm[:, 16:20], beta, -1.0, None, ALU.mult, R=[g], W=[sm])
                p.act(sm[:, 20:24], sm[:, 4:8], AF.Exp, R=[sm], W=[sm])
                yield
                Es = R_["Es"].next(); Ei = R_["Ei"].next()
                p.tt("pool", Es[:], E[:], ms.unsqueeze(1).to_broadcast([128, 4, 128]), ALU.mult, R=[E, cfT], W=[Es])
                p.tt("pool", Ei[:], E[:], mi.unsqueeze(1).to_broadcast([128, 4, 128]), ALU.mult, R=[E, cfT], W=[Ei])
                qd = R_["qd"].next()
                p.tt("dve", qd[:], qT[:], eg[:], ALU.mult, R=[qT, eg], W=[qd])
                kd = R_["kd"].next()
                for h in range(4):
                    p.act(kd[:, h, :], ktok[:, h, :], AF.Copy, scale=sm[:, 8 + h:9 + h], R=[ktok, sm], W=[kd])
                pk = psP.next()
                for h in range(4):
                    p.mm(pk[:, h, :], kT[:, h, :], kT[:, h, :], R=[kT], W=[pk])
                pq = psP.next()
                for h in range(4):
                    p.mm(pq[:, h, :], kT[:, h, :], qT[:, h, :], R=[kT, qT], W=[pq])
                yield
                B = R_["B"].next(); BT = R_["BT"].next(); B32 = R_["B32"].next()
                for h in range(4):
                    p.stt("dve", B32[:, h, :], pk[:, h, :], beta[:, h:h + 1], Es[:, h, :], ALU.mult, ALU.mult, R=[pk, Es, g], W=[B32])
                p.cp("act", B[:], B32[:], R=[B32], W=[B])
                qkd = R_["qkd"].next()
                p.tt("dve", qkd[:], pq[:], Ei[:], ALU.mult, R=[pq, Ei], W=[qkd])
                pb = psP.next()
                for h in range(4):
                    p.tr(pb[:, h, :], B32[:, h, :], CF("identf"), R=[B32, cfT], W=[pb])
                yield
                p.cp("act", BT[:], pb[:], R=[pb], W=[BT])
                nmn = "nmf%d" if d == 0 else "nmb%d"
                nmt = "nmb%d" if d == 0 else "nmf%d"
                bc4 = lambda ap: ap.unsqueeze(1).to_broadcast([128, 4, 128])
                G = R_["P"].next(); GT = R_["GT"].next()
                tmp = R_["tmp"].next(); tmpT = R_["tmp"].next()
                p.tt("dve", tmp[:], B[:], bc4(CF(nmn % 0)), ALU.mult, R=[B, cfT], W=[tmp])
                p.tt("dve", G[:], tmp[:], bc4(CF("identf")), ALU.add, R=[tmp, cfT], W=[G])
                p.tt("pool", tmpT[:], BT[:], bc4(CF(nmt % 0)), ALU.mult, R=[BT, cfT], W=[tmpT])
                p.tt("pool", GT[:], tmpT[:], bc4(CF("identf")), ALU.add, R=[tmpT, cfT], W=[GT])
                yield
                for lev in range(1, 7):
                    last = (lev == 6)
                    px = psP.next()
                    for h in range(4):
                        p.mm(px[:, h, :], BT[:, h, :], G[:, h, :], R=[BT, G], W=[px])
                    yield
                    Xb = R_["Xb"].next()
                    p.cp("act", Xb[:], px[:], R=[px], W=[Xb])
                    py = psP.next()
                    for h in range(4):
                        p.mm(py[:, h, :], GT[:, h, :], Xb[:, h, :], R=[GT, Xb], W=[py])
                    if not last:
                        pyt = psP.next()
                        for h in range(4):
                            p.mm(pyt[:, h, :], Xb[:, h, :], GT[:, h, :], R=[GT, Xb], W=[pyt])
                    yield
                    tmp = R_["tmp"].next()
                    p.tt("dve", tmp[:], py[:], bc4(CF(nmn % lev)), ALU.mult, R=[py, cfT], W=[tmp])
                    p.tt("dve", G[:], G[:], tmp[:], ALU.add, R=[G, tmp], W=[G])
                    if not last:
                        tmpT = R_["tmp"].next()
                        p.tt("dve", tmpT[:], pyt[:], bc4(CF(nmt % lev)), ALU.mult, R=[pyt, cfT], W=[tmpT])
                        p.tt("pool", GT[:], GT[:], tmpT[:], ALU.add, R=[GT, tmpT], W=[GT])
                    yield
                P = G
                prepped[d][it] = dict(c=c, kT=kT, vtok=vtok, sm=sm, P=P, qkd=qkd, qd=qd, kd=kd)

            state = {}

            def scan(d, it):
                R_ = ins[d]
                pr = prepped[d].pop(it)
                c = pr["c"]; sm = pr["sm"]
                S32 = R_["S32"]
                Sbf = state[d]
                pks = psSc[d].next()
                for h in range(4):
                    p.mm(pks[:, h, :], pr["kT"][:, h, :], Sbf[:, h, :], R=[pr["kT"], Sbf], W=[pks])
                yield
                nR = R_["nR"].next()
                for h in range(4):
                    p.stt("dve", nR[:, h, :], pks[:, h, :], sm[:, 12 + h:13 + h], pr["vtok"][:, h, :], ALU.mult, ALU.subtract,
                          R=[pks, sm, pr["vtok"]], W=[nR])
                pw_ = psSc[d].next()
                for h in range(4):
                    p.mm(pw_[:, h, :], pr["P"][:, h, :], nR[:, h, :], R=[pr["P"], nR], W=[pw_])
                yield
                w = R_["w"].next()
                for h in range(4):
                    p.act(w[:, h, :], pw_[:, h, :], AF.Copy, scale=sm[:, 16 + h:17 + h], R=[pw_, sm], W=[w])
                if c >= 2:
                    po = psSc[d].next()
                    for h in range(4):
                        p.mm(po[:, h, :], pr["qd"][:, h, :], Sbf[:, h, :], start=True, stop=False, R=[pr["qd"], Sbf], W=[po])
                        p.mm(po[:, h, :], pr["qkd"][:, h, :], w[:, h, :], start=False, stop=True, R=[pr["qkd"], w], W=[po])
                pds = psSc[d].next()
                for h in range(4):
                    p.mm(pds[:, h, :], pr["kd"][:, h, :], w[:, h, :], R=[pr["kd"], w], W=[pds])
                yield
                for h in range(4):
                    p.stt("dve", S32[:, h, :], S32[:, h, :], sm[:, 20 + h:21 + h], pds[:, h, :], ALU.mult, ALU.add,
                          R=[S32, sm, pds], W=[S32])
                nS = R_["Sbf"].next()
                p.cp("act", nS[:], S32[:], R=[S32], W=[nS])
                state[d] = nS
                if c >= 2:
                    osb = R_["osb"].next()
                    p.cp("act", osb[:], po[:], R=[po], W=[osb])
                    tl = (c - 2) * 128
                    dst = S["of"] if d == 0 else S["ob"]
                    p.dma("sp", dst[tl:tl + 128, :].rearrange("p (h d) -> p h d", h=4), osb[:], R=[osb])
                yield

            for d in range(2):
                p.memset("pool", ins[d]["S32"][:], 0.0, W=[ins[d]["S32"]])
                s0 = ins[d]["Sbf"].next()
                p.memset("pool", s0[:], 0.0, W=[s0])
                state[d] = s0
            interleave([prep(0, 0), prep(1, 0)])
            import os
            nit = int(os.environ.get("DELTA_ITERS", NCH))
            for it in range(nit):
                gens = [scan(0, it), scan(1, it)]
                if it + 1 < NCH:
                    gens += [prep(0, it + 1), prep(1, it + 1)]
                interleave(gens)

    def phase_even_out():
        with p.scope():
            wo = p.sb([128, 8, D], BF16, "wo")
            for k in range(8):
                p.dma("pool", wo[:, k, :], I["ev_w_out"][k * 128:(k + 1) * 128, :], W=[wo])
            onorm = p.sb([128, 128], F32)
            rowvec(onorm, I["dn_norm"])
            g1 = p.sb([128, D], F32)
            rowvec(g1, modv(0, 0, 2))
            ofr = p.ring(2, [128, 4, 128], F32); obr = p.ring(2, [128, 4, 128], F32)
            zr = p.ring(2, [128, 4, 128], BF16); xr = p.ring(3, [128, D], F32)
            plr = p.ring(3, [128, 4, 128], BF16)
            junk = p.sb([128, 128], F32)
            ssr = p.ring(2, [128, 8], F32)
            omr = p.ring(3, [128, 4, 128], BF16)
            mxr = p.ring(2, [128, 4, 128], BF16)
            psB = p.ring(2, [128, 8, 128], BF16, psum=True)
            psY = p.ring(4, [128, 512], F32, psum=True)
            x1r = p.ring(2, [128, D], F32)
            def eA(j):
                t0 = j * 128
                of = ofr.next(); ob = obr.next(); z = zr.next(); xt = xr.next(); pl = plr.next()
                p.dma("sp", of[:], S["of"][t0:t0 + 128, :].rearrange("p (h d) -> p h d", h=4), W=[of])
                p.dma("sp", ob[:], S["ob"][t0:t0 + 128, :].rearrange("p (h d) -> p h d", h=4), W=[ob])
                p.dma("sp", z[:], S["zs"][t0:t0 + 128, :].rearrange("p (h d) -> p h d", h=4), W=[z])
                p.dma("sp", xt[:], I["x"][t0:t0 + 128, :], W=[xt])
                p.dma("sp", pl[:], S["mixT"][4:8, :, t0:t0 + 128].rearrange("k p t -> p k t"), W=[pl])
                p.tt("pool", of[:], of[:], ob[:], ALU.add, R=[of, ob], W=[of])
                ss = ssr.next()
                p.memset("pool", ss[:], 0.0, W=[ss])
                for h in range(4):
                    p.act(junk[:], of[:, h, :], AF.Square, accum=ss[:, h:h + 1], R=[of], W=[junk, ss])
                p.ts("dve", ss[:, 4:8], ss[:, 0:4], 1.0 / 128, EPS, ALU.mult, ALU.add, R=[ss], W=[ss])
                p.rsqrt(ss[:, 4:8], ss)
                p.tt("dve", of[:], of[:], ss[:, 4:8].unsqueeze(2).to_broadcast([128, 4, 128]), ALU.mult, R=[of, ss], W=[of])
                p.tt("pool", of[:], of[:], onorm[:, :].unsqueeze(1).to_broadcast([128, 4, 128]), ALU.mult, R=[of, onorm], W=[of])
                om = omr.next()
                p.tt("dve", om[:], of[:], z[:], ALU.mult, R=[of, z], W=[om])
                return (t0, om, pl, xt)

            def eB(st):
                t0, om, pl, xt = st
                pb = psB.next()
                for h in range(4):
                    p.tr(pb[:, h, :], om[:, h, :], CB("ident"), R=[om, cbT], W=[pb])
                mx = mxr.next()
                p.cp("act", mx[:], pb[:, 0:4, :], R=[pb], W=[mx])
                x1 = x1r.next()
                for nh in range(2):
                    py = psY.next()
                    for k in range(8):
                        lhs = mx[:, k, :] if k < 4 else pl[:, k - 4, :]
                        p.mm(py[:], lhs, wo[:, k, nh * 512:(nh + 1) * 512], start=(k == 0), stop=(k == 7), R=[mx, pl, wo], W=[py])
                    p.tt("dve", x1[:, nh * 512:(nh + 1) * 512], py[:], g1[:, nh * 512:(nh + 1) * 512], ALU.mult, R=[py, g1], W=[x1])
                p.tt("pool", x1[:], x1[:], xt[:], ALU.add, R=[x1, xt], W=[x1])
                p.dma("pool", S["xl"][t0:t0 + 128, :], x1[:], R=[x1])

            pend = None
            for j in range(NT):
                st = eA(j)
                if pend is not None:
                    eB(pend)
                pend = st
            eB(pend)

    def phase_odd():
        with p.scope():
            w = p.sb([128, 8, 3 * D], BF16, "wsc")
            for k in range(8):
                p.dma("pool", w[:, k, :], I["sc_w_in"][k * 128:(k + 1) * 128, :], W=[w])
            wo = p.sb([128, 8, D], BF16, "wsco")
            for k in range(8):
                p.dma("pool", wo[:, k, :], I["sc_w_out"][k * 128:(k + 1) * 128, :], W=[wo])
            cw = p.sb([128, 8, 3], F32)
            p.dma("sp", cw[:], I["sc_conv"].rearrange("p (c t) -> p c t", t=3), W=[cw])
            g1 = p.sb([128, D], F32)
            rowvec(g1, modv(1, 0, 2))
            hwr = p.ring(2, [128, 8, 258], BF16)
            psW = p.ring(5, [128, 512], F32, psum=True)
            psY = p.ring(3, [128, 512], F32, psum=True)
            gcr = p.ring(2, [128, 258], F32)
            ur = p.ring(2, [128, 258], F32)
            accr = p.ring(2, [128, 256], F32)
            mixr = p.ring(2, [128, 8, 256], BF16)
            xr = p.ring(2, [128, D], F32)
            x1r = p.ring(2, [128, D], F32)
            for blk in range(L // 256):
                hw = hwr.next()
                load_window(hw, CTX, L, blk)
                mix = mixr.next()
                for c in range(8):
                    pss = []
                    for part in range(3):
                        ps = psW.next()
                        col = part * D + c * 128
                        for k in range(8):
                            p.mm(ps[:, 0:258], w[:, k, col:col + 128], hw[:, k, :], start=(k == 0), stop=(k == 7), R=[w, hw], W=[ps])
                        pss.append(ps)
                    gc = gcr.next()
                    p.cp("act", gc[:], pss[1][:, 0:258], R=[pss[1]], W=[gc])
                    u = ur.next()
                    p.tt("dve", u[:], gc[:], pss[2][:, 0:258], ALU.mult, R=[gc, pss[2]], W=[u])
                    acc = accr.next()
                    p.ts("pool", acc[:], u[:, 1:257], cw[:, c, 1:2], None, ALU.mult, R=[u, cw], W=[acc])
                    p.stt("dve", acc[:], u[:, 0:256], cw[:, c, 0:1], acc[:], ALU.mult, ALU.add, R=[u, cw, acc], W=[acc])
                    p.stt("dve", acc[:], u[:, 2:258], cw[:, c, 2:3], acc[:], ALU.mult, ALU.add, R=[u, cw, acc], W=[acc])
                    p.tt("dve", mix[:, c, :], acc[:], pss[0][:, 1:257], ALU.mult, R=[acc, pss[0]], W=[mix])
                for t2 in range(2):
                    t0 = blk * 256 + t2 * 128
                    xt = xr.next()
                    p.dma("act", xt[:], S["xl"][t0:t0 + 128, :], W=[xt])
                    x1 = x1r.next()
                    for nh in range(2):
                        py = psY.next()
                        for k in range(8):
                            p.mm(py[:], mix[:, k, t2 * 128:(t2 + 1) * 128], wo[:, k, nh * 512:(nh + 1) * 512],
                                 start=(k == 0), stop=(k == 7), R=[mix, wo], W=[py])
                        p.tt("dve", x1[:, nh * 512:(nh + 1) * 512], py[:], g1[:, nh * 512:(nh + 1) * 512], ALU.mult, R=[py, g1], W=[x1])
                    p.tt("pool", x1[:], x1[:], xt[:], ALU.add, R=[x1, xt], W=[x1])
                    p.dma("pool", S["xl"][t0:t0 + 128, :], x1[:], R=[x1, xt])

    def phase_moe(i):
        with p.scope():
            affall = p.sb([128, NT, NE], F32, "affall")
            idx = p.sb([128, NE, NM], I32, "idx")
            gate = p.sb([128, NE, NM], F32, "gate")
            g2 = p.sb([128, D], F32, "g2")
            rowvec(g2, modv(i, 0, 5))
            with p.scope():
                Ar = p.sb([128, D], F32); Br = p.sb([128, D], F32); gr_ = p.sb([128, D], F32)
                rowvec(gr_, I["norm_ffn"][i])
                rowvec(Br, modv(i, 0, 3))
                rowvec(Ar, modv(i, 0, 4))
                p.stt("dve", Ar[:], Ar[:], 1.0, gr_[:], ALU.add, ALU.mult, R=[Ar, gr_], W=[Ar])
                r32 = p.sb([128, 8, NE], F32); rhi = p.sb([128, 8, NE], BF16); rlo = p.sb([128, 8, NE], BF16)
                p.dma("sp", r32[:], I["router"][i].rearrange("(k p) e -> p k e", p=128), W=[r32])
                p.cp("dve", rhi[:], r32[:], R=[r32], W=[rhi])
                p.tt("dve", r32[:], r32[:], rhi[:], ALU.subtract, R=[r32, rhi], W=[r32])
                p.cp("dve", rlo[:], r32[:], R=[r32], W=[rlo])
                xr = p.ring(2, [128, D], F32); junk = p.sb([128, D], BF16)
                ssr = p.ring(2, [128, 8], F32)
                hr = p.ring(2, [128, D], F32); hir = p.ring(2, [128, D], BF16); lor = p.ring(2, [128, D], BF16)
                psT = p.ring(4, [128, D], BF16, psum=True)
                hiTr = p.ring(2, [128, D], BF16); loTr = p.ring(2, [128, D], BF16)
                psL = p.ring(2, [128, 512], F32, psum=True)
                er = p.ring(2, [128, NE], F32)
                def mA(j):
                    t0 = j * 128
                    xt = xr.next()
                    p.dma("sp", xt[:], S["xl"][t0:t0 + 128, :], W=[xt])
                    ss = ssr.next()
                    p.memset("pool", ss[:], 0.0, W=[ss])
                    p.act(junk[:], xt[:], AF.Square, accum=ss[:, 0:1], R=[xt], W=[junk, ss])
                    p.ts("dve", ss[:, 1:2], ss[:, 0:1], 1.0 / D, EPS, ALU.mult, ALU.add, R=[ss], W=[ss])
                    p.rsqrt(ss[:, 1:2], ss)
                    h = hr.next()
                    p.stt("dve", h[:], xt[:], ss[:, 1:2], Ar[:], ALU.mult, ALU.mult, R=[xt, ss, Ar], W=[h])
                    p.tt("dve", h[:], h[:], Br[:], ALU.add, R=[h, Br], W=[h])
                    hi = hir.next(); lo = lor.next()
                    p.cp("act", hi[:], h[:], R=[h], W=[hi])
                    p.dma("pool", S["h2"][t0:t0 + 128, :], hi[:], R=[hi])
                    p.tt("dve", lo[:], h[:], hi[:], ALU.subtract, R=[h, hi], W=[lo])
                    return (j, ss, hi, lo)

                def mB(st):
                    j, ss, hi, lo = st
                    pTh = psT.next(); pTl = psT.next()
                    for k in range(8):
                        p.tr(pTh[:, k * 128:(k + 1) * 128], hi[:, k * 128:(k + 1) * 128], CB("ident"), R=[hi, cbT], W=[pTh])
                    for k in range(8):
                        p.tr(pTl[:, k * 128:(k + 1) * 128], lo[:, k * 128:(k + 1) * 128], CB("ident"), R=[lo, cbT], W=[pTl])
                    hiT = hiTr.next(); loT = loTr.next()
                    p.cp("act", hiT[:], pTh[:], R=[pTh], W=[hiT])
                    p.cp("dve", loT[:], pTl[:], R=[pTl], W=[loT])
                    pl = psL.next()
                    n = 0
                    for (a, b) in ((hiT, rhi), (loT, rhi), (hiT, rlo)):
                        for k in range(8):
                            p.mm(pl[:, 0:NE], a[:, k * 128:(k + 1) * 128], b[:, k, :], start=(n == 0), stop=(n == 23), R=[a, b], W=[pl])
                            n += 1
                    p.op("dve", lambda e, o=ss[:, 2:3], a=pl[:, 0:NE]: e.tensor_reduce(out=o, in_=a, axis=AX.X, op=ALU.max), R=[pl], W=[ss])
                    p.ts("dve", ss[:, 3:4], ss[:, 2:3], -1.0, None, ALU.mult, R=[ss], W=[ss])
                    ex = er.next()
                    p.act(ex[:], pl[:, 0:NE], AF.Exp, bias=ss[:, 3:4], accum=ss[:, 4:5], R=[pl, ss], W=[ex, ss])
                    p.op("dve", lambda e, o=ss[:, 5:6], a=ss[:, 4:5]: e.reciprocal(out=o, in_=a), R=[ss], W=[ss])
                    p.ts("dve", affall[:, j, :], ex[:], ss[:, 5:6], None, ALU.mult, R=[ex, ss], W=[affall])

                pend = None
                for j in range(NT):
                    st = mA(j)
                    if pend is not None:
                        mB(pend)
                    pend = st
                mB(pend)
            with p.scope():
                NB_ = NT * NE
                lo = p.sb([128, NE], F32); hi = p.sb([128, NE], F32); mid = p.sb([128, NE], F32)
                sel = p.sb([128, NE], F32); d1 = p.sb([128, NE], F32)
                cmp_ = p.sb([128, NT, NE], BF16); cntp = p.sb([128, NE], F32)
                psc = p.ring(2, [128, 512], F32, psum=True)
                p.memset("dve", lo[:], 0.0, W=[lo])
                p.memset("dve", hi[:], 2.0, W=[hi])
                for itn in range(29):
                    p.tt("dve", mid[:], lo[:], hi[:], ALU.add, R=[lo, hi], W=[mid])
                    p.ts("dve", mid[:], mid[:], 0.5, None, ALU.mult, R=[mid], W=[mid])
                    p.tt("dve", cmp_[:], affall[:], mid[:, :].unsqueeze(1).to_broadcast([128, NT, NE]), ALU.is_ge, R=[affall, mid], W=[cmp_])
                    p.op("dve", lambda e: e.tensor_reduce(out=cntp[:], in_=cmp_[:].rearrange("p j e -> p e j"), axis=AX.X, op=ALU.add),
                         R=[cmp_], W=[cntp])
                    pc = psc.next()
                    p.mm(pc[:, 0:NE], CF("ones"), cntp[:], R=[cfT, cntp], W=[pc])
                    p.ts("dve", sel[:], pc[:, 0:NE], float(CAP) - 0.5, None, ALU.is_ge, R=[pc], W=[sel])
                    p.tt("dve", d1[:], mid[:], lo[:], ALU.subtract, R=[mid, lo], W=[d1])
                    p.tt("dve", d1[:], d1[:], sel[:], ALU.mult, R=[d1, sel], W=[d1])
                    p.tt("dve", lo[:], lo[:], d1[:], ALU.add, R=[lo, d1], W=[lo])
                    p.tt("dve", d1[:], hi[:], mid[:], ALU.subtract, R=[hi, mid], W=[d1])
                    p.tt("dve", d1[:], d1[:], sel[:], ALU.mult, R=[d1, sel], W=[d1])
                    p.tt("dve", hi[:], mid[:], d1[:], ALU.add, R=[mid, d1], W=[hi])
                selm = p.sb([128, NT, NE], F32); ca = p.sb([128, NT, NE], F32); cb_ = p.sb([128, NT, NE], F32)
                p.tt("dve", selm[:], affall[:], lo[:, :].unsqueeze(1).to_broadcast([128, NT, NE]), ALU.is_ge, R=[affall, lo], W=[selm])
                p.cp("dve", ca[:], selm[:], R=[selm], W=[ca])
                cur, oth = ca, cb_
                s = 1
                while s < NT:
                    p.cp("pool", oth[:, 0:s, :], cur[:, 0:s, :], R=[cur], W=[oth])
                    p.tt("dve", oth[:, s:, :], cur[:, s:, :], cur[:, 0:NT - s, :], ALU.add, R=[cur], W=[oth])
                    cur, oth = oth, cur
                    s *= 2
                totb = p.sb([128, NE], BF16)
                p.cp("dve", totb[:], cur[:, NT - 1, :], R=[cur], W=[totb])
                pp_ = psc.next()
                p.mm(pp_[:, 0:NE], CB("lstrict"), totb[:], R=[cbT, totb], W=[pp_])
                pref = p.sb([128, NE], F32)
                p.cp("dve", pref[:], pp_[:, 0:NE], R=[pp_], W=[pref])
                pos = oth
                p.tt("dve", pos[:], cur[:], selm[:], ALU.subtract, R=[cur, selm], W=[pos])
                p.tt("dve", pos[:], pos[:], pref[:, :].unsqueeze(1).to_broadcast([128, NT, NE]), ALU.add, R=[pos, pref], W=[pos])
                posi = p.sb([128, NT, NE], I32); pdi = p.sb([128, NT, NE], I32); pmi = p.sb([128, NT, NE], I32)
                pdf = p.sb([128, NT, NE], F32); pmf = p.sb([128, NT, NE], F32)
                p.cp("dve", posi[:], pos[:], R=[pos], W=[posi])
                p.op("dve", lambda e: e.tensor_single_scalar(out=pdi[:], in_=posi[:], scalar=7, op=ALU.arith_shift_right), R=[posi], W=[pdi])
                p.op("dve", lambda e: e.tensor_single_scalar(out=pmi[:], in_=posi[:], scalar=127, op=ALU.bitwise_and), R=[posi], W=[pmi])
                p.cp("dve", pdf[:], pdi[:], R=[pdi], W=[pdf])
                p.cp("dve", pmf[:], pmi[:], R=[pmi], W=[pmf])
                p.stt("dve", pmf[:], pmf[:], 1.0, selm[:], ALU.add, ALU.mult, R=[pmf, selm], W=[pmf])
                p.ts("dve", pmf[:], pmf[:], -1.0, None, ALU.add, R=[pmf], W=[pmf])
                val4 = p.sb([128, NT, NE, 4], F32)
                ahi = p.sb([128, NT, NE], BF16)
                p.cp("pool", val4[:, :, :, 0], cfT[:, OF["misc"]:OF["misc"] + 1].unsqueeze(2).to_broadcast([128, NT, NE]), R=[cfT], W=[val4])
                jrow = cfT[:, OF["iota"]:OF["iota"] + NT]
                p.cp("pool", val4[:, :, :, 1], jrow.unsqueeze(2).to_broadcast([128, NT, NE]), R=[cfT], W=[val4])
                p.cp("dve", ahi[:], affall[:], R=[affall], W=[ahi])
                p.cp("dve", val4[:, :, :, 2], ahi[:], R=[ahi], W=[val4])
                p.tt("dve", val4[:, :, :, 3], affall[:], ahi[:], ALU.subtract, R=[affall, ahi], W=[val4])
                iota8 = cfT[:, OF["misc"] + 8:OF["misc"] + 16]
                JC = 8
                Mh = p.sb([128, JC, NE, 8], F32)
                Rr = p.ring(2, [128, NT, 128], BF16)
                pacc = psc.next()
                rhs_all = [p.sb([128, JC, NE, 8, 4], BF16) for _ in range((NT + JC - 1) // JC)]
                for jc in range(0, NT, JC):
                    n = min(JC, NT - jc)
                    p.tt("dve", Mh[:, 0:n], pdf[:, jc:jc + n, :].unsqueeze(3).to_broadcast([128, n, NE, 8]),
                         iota8.unsqueeze(1).unsqueeze(1).to_broadcast([128, n, NE, 8]), ALU.is_equal, R=[pdf, cfT], W=[Mh])
                    ra = rhs_all[jc // JC]
                    p.tt("pool", ra[:, 0:n], Mh[:, 0:n].unsqueeze(4).to_broadcast([128, n, NE, 8, 4]),
                         val4[:, jc:jc + n].unsqueeze(3).to_broadcast([128, n, NE, 8, 4]), ALU.mult, R=[Mh, val4], W=[ra])
                for e_ in range(NE):
                    Rm = Rr.next()
                    p.tt("dve", Rm[:], CF("iota").unsqueeze(1).to_broadcast([128, NT, 128]),
                         pmf[:, :, e_:e_ + 1].to_broadcast([128, NT, 128]), ALU.is_equal, R=[cfT, pmf], W=[Rm])
                    for j in range(NT):
                        ra = rhs_all[j // JC]
                        p.mm(pacc[:, e_ * 32:(e_ + 1) * 32], Rm[:, j, :], ra[:, j % JC, e_].rearrange("p m g -> p (m g)"),
                             start=(j == 0), stop=(j == NT - 1), R=[Rm, ra], W=[pacc])
                accs = p.sb([128, 512], F32)
                p.cp("dve", accs[:], pacc[:, 0:512], R=[pacc], W=[accs])
                accv = accs[:, 0:512].rearrange("p (e m g) -> p e m g", e=NE, m=8)
                lf = p.sb([128, NE, NM], F32)
                p.stt("dve", lf[:], accv[:, :, 0:NM, 1], 128.0, accv[:, :, 0:NM, 0], ALU.mult, ALU.add, R=[accs], W=[lf])
                p.cp("dve", idx[:], lf[:], R=[lf], W=[idx])
                p.tt("dve", gate[:], accv[:, :, 0:NM, 2], accv[:, :, 0:NM, 3], ALU.add, R=[accs], W=[gate])
            with p.scope():
                wgr = p.ring(2, [128, 8, 512], BF16, name="wg"); wur = p.ring(2, [128, 8, 512], BF16, name="wu")
                wdr = p.ring(2, [128, 4, D], BF16, name="wd")
                xgr = p.ring(2 * NM + 2, [128, D], BF16)
                xgTr = p.ring(2, [128, 8, CAP], BF16, name="xgT")
                hidr = p.ring(2, [128, 4, CAP], BF16, name="hid")
                psT = p.ring(2, [128, D], BF16, psum=True)
                psG = p.ring(4, [128, 512], F32, psum=True)
                psY = p.ring(2, [128, 512], F32, psum=True)
                sgr = p.ring(2, [128, 512], F32)
                yr = p.ring(2, [128, D], F32)
                xacc = Tile(None)
                SH = min(512, CAP)
                def issue_loads(e_):
                    wg = wgr.next(); wu = wur.next(); wd = wdr.next()
                    p.dma("pool", wg[:], I["w_gate"][i, e_].rearrange("(k p) f -> p k f", p=128), W=[wg])
                    p.dma("pool", wu[:], I["w_up"][i, e_].rearrange("(k p) f -> p k f", p=128), W=[wu])
                    p.dma("pool", wd[:], I["w_down"][i, e_].rearrange("(k p) f -> p k f", p=128), W=[wd])
                    xgs = []
                    for m in range(NM):
                        xg = xgr.next()
                        p.dmaf("pool", lambda e, o=xg[:], ix=idx[:, e_, m:m + 1]: e.indirect_dma_start(
                            out=o, out_offset=None, in_=S["h2"][:, :], in_offset=bass.IndirectOffsetOnAxis(ap=ix, axis=0)),
                            R=[idx], W=[xg])
                        xgs.append(xg)
                    return wg, wu, wd, xgs

                nxt = issue_loads(0)
                for e_ in range(NE):
                    wg, wu, wd, xgs = nxt
                    if e_ + 1 < NE:
                        nxt = issue_loads(e_ + 1)
                    xgT = xgTr.next()
                    for m in range(NM):
                        xg = xgs[m]
                        pt = psT.next()
                        for k in range(8):
                            p.tr(pt[:, k * 128:(k + 1) * 128], xg[:, k * 128:(k + 1) * 128], CB("ident"), R=[xg, cbT], W=[pt])
                        p.cp("act" if m % 2 == 0 else "dve", xgT[:, :, m * 128:(m + 1) * 128], pt[:].rearrange("p (k t) -> p k t", k=8), R=[pt], W=[xgT])
                    hid = hidr.next()
                    for fc in range(4):
                        for sh in range(CAP // SH):
                            pg = psG.next(); pu = psG.next()
                            for k in range(8):
                                p.mm(pg[:, 0:SH], wg[:, k, fc * 128:(fc + 1) * 128], xgT[:, k, sh * SH:(sh + 1) * SH],
                                     start=(k == 0), stop=(k == 7), R=[wg, xgT], W=[pg])
                            for k in range(8):
                                p.mm(pu[:, 0:SH], wu[:, k, fc * 128:(fc + 1) * 128], xgT[:, k, sh * SH:(sh + 1) * SH],
                                     start=(k == 0), stop=(k == 7), R=[wu, xgT], W=[pu])
                            sg = sgr.next()
                            p.act(sg[:, 0:SH], pg[:, 0:SH], AF.Silu, R=[pg], W=[sg])
                            p.tt("dve", hid[:, fc, sh * SH:(sh + 1) * SH], sg[:, 0:SH], pu[:, 0:SH], ALU.mult, R=[sg, pu], W=[hid])
                    for m in range(NM):
                        y = yr.next()
                        for dh in range(2):
                            py = psY.next()
                            for fk in range(4):
                                p.mm(py[:], hid[:, fk, m * 128:(m + 1) * 128], wd[:, fk, dh * 512:(dh + 1) * 512],
                                     start=(fk == 0), stop=(fk == 3), R=[hid, wd], W=[py])
                            p.stt("dve", y[:, dh * 512:(dh + 1) * 512], py[:], gate[:, e_, m:m + 1], g2[:, dh * 512:(dh + 1) * 512],
                                  ALU.mult, ALU.mult, R=[py, gate, g2], W=[y])
                        p.dmaf("pool", lambda e, s_=y[:], ix=idx[:, e_, m:m + 1]: e.indirect_dma_start(
                            out=S["xl"][:, :], out_offset=bass.IndirectOffsetOnAxis(ap=ix, axis=0), in_=s_, in_offset=None,
                            compute_op=ALU.add), R=[y, idx], W=[xacc])

    def phase_final():
        with p.scope():
            gfin = p.sb([128, D], F32)
            rowvec(gfin, I["norm_final"])
            xr = p.ring(3, [128, D], F32); junk = p.sb([128, D], BF16); ssr = p.ring(3, [128, 2], F32)
            orr = p.ring(3, [128, D], F32)
            for j in range(NT):
                t0 = j * 128
                xt = xr.next()
                p.dma("sp", xt[:], S["xl"][t0:t0 + 128, :], W=[xt])
                ss = ssr.next()
                p.memset("pool", ss[:], 0.0, W=[ss])
                p.act(junk[:], xt[:], AF.Square, accum=ss[:, 0:1], R=[xt], W=[junk, ss])
                p.ts("dve", ss[:, 1:2], ss[:, 0:1], 1.0 / D, EPS, ALU.mult, ALU.add, R=[ss], W=[ss])
                p.rsqrt(ss[:, 1:2], ss)
                o = orr.next()
                p.stt("dve", o[:], xt[:], ss[:, 1:2], gfin[:], ALU.mult, ALU.mult, R=[xt, ss, gfin], W=[o])
                p.dma("act", out[t0:t0 + 128, :], o[:], R=[o])

    phases = [
        ("hT0", lambda: phase_hT([(I["ctx"], CTX, 0, 1), (I["x"], L, CTX, 0)], 0, I["norm_mix"][0])),
        ("proj", phase_even_proj), ("delta", phase_delta), ("evout", phase_even_out),
        ("moe0", lambda: phase_moe(0)),
        ("hT1", lambda: phase_hT([(S["xl"], L, CTX, 0)], 1, I["norm_mix"][1])),
        ("odd", phase_odd), ("moe1", lambda: phase_moe(1)), ("final", phase_final),
    ]
    for nm, fn in phases:
        fn()
        if stop == nm:
            break
    p.flush()
    p.finish()
    print("bass instructions:", p.ninst, {e: p.cnt[e] for e in p.ENG})
    return nc


def prep_inputs(b, inputs, L):
    arr_f, _, arr_b, _ = make_consts()
    f = lambda a: np.ascontiguousarray(np.asarray(a, dtype=np.float32))
    c = f(inputs["c"])[b]
    cc = f(inputs["c_ctx"])
    cvec = np.concatenate([c.reshape(8, 128).T, cc.reshape(8, 128).T], axis=1)
    m = dict(
        x=f(inputs["x"])[b][:L], ctx=f(inputs["ctx"])[b], cvec=f(cvec),
        ada_w=f(inputs["ada_w"]), ada_b=f(inputs["ada_b"]),
        norm_mix=f(inputs["norm_mix"]), norm_ffn=f(inputs["norm_ffn"]), norm_final=f(inputs["norm_final"]),
        ev_w_in=f(inputs["ev_w_in"])[0],
        dn_conv=f(f(inputs["dn_conv"])[0].reshape(3, 12, 128).transpose(2, 1, 0).reshape(128, 36)),
        dn_a_log=f(inputs["dn_a_log"])[0].reshape(8), dn_dt_bias=f(inputs["dn_dt_bias"])[0].reshape(8),
        dn_norm=f(inputs["dn_norm"])[0], pool_w=f(inputs["pool_w"])[0],
        pool_scale=f(f(inputs["pool_scale"])[0].reshape(4, 128).T),
        ev_w_out=f(inputs["ev_w_out"])[0],
        sc_w_in=f(inputs["sc_w_in"])[0],
        sc_conv=f(f(inputs["sc_conv"])[0].reshape(3, 8, 128).transpose(2, 1, 0).reshape(128, 24)),
        sc_w_out=f(inputs["sc_w_out"])[0],
        router=f(inputs["router"]), w_gate=f(inputs["w_gate"]), w_up=f(inputs["w_up"]), w_down=f(inputs["w_down"]),
        cf=arr_f, cb=arr_b,
    )
    return m


_NC_CACHE = {}


def kernel(**inputs):
    x = np.asarray(inputs["x"])
    B, L, _ = x.shape
    if L not in _NC_CACHE:
        _NC_CACHE[L] = build(L)
    nc = _NC_CACHE[L]
    maps = [prep_inputs(b, inputs, L) for b in range(B)]
    n = 8
    in_maps = [maps[c % B] for c in range(n)]
    res = run_bass_kernel_spmd(nc, in_maps, core_ids=list(range(n)))
    outs = [np.asarray(res.results[b]["out"], dtype=np.float32) for b in range(B)]
    return np.stack(outs, axis=0)
```
